# Optimizing a Trainium2 kernel written in Bass

```python
import jax, jax.numpy as jnp
from jax import lax
import numpy as np

D_MODEL = 1024
BATCH = 4
SEQ = 8192
DEPTH = 1

N_MEM = 256
MLA_HEADS = 8
MLA_Q_RANK = 256
MLA_KV_RANK = 128
MLA_NOPE_DIM = 64
MLA_ROPE_DIM = 32
MLA_V_DIM = 64
MLA_QK_DIM = MLA_NOPE_DIM + MLA_ROPE_DIM
DSA_HEADS = 8
DSA_KV_HEADS = 2
DSA_HEAD_DIM = 64
IDX_HEADS = 8
IDX_DIM = 64
PARTIAL_ROT_DIM = DSA_HEAD_DIM // 4
INDEX_TOPK_MAX = 256
MEM_HEADS = 4
MEM_HEAD_DIM = 128
D_FF = ((8 * D_MODEL // 3 + 255) // 256) * 256
ROPE_THETA = 500000.0
EPS = 1e-6
QBLOCK = 128
MIX_WIDTH = MLA_HEADS * MLA_V_DIM + DSA_HEADS * DSA_HEAD_DIM
IN_SPLIT_SIZES = (MLA_Q_RANK, MLA_KV_RANK, MLA_ROPE_DIM,
                  DSA_HEADS * DSA_HEAD_DIM, DSA_KV_HEADS * DSA_HEAD_DIM, DSA_KV_HEADS * DSA_HEAD_DIM,
                  IDX_HEADS * IDX_DIM, IDX_DIM, IDX_HEADS)
IN_COLS = sum(IN_SPLIT_SIZES)

kernel_name = 'hybrid_mla_dsa_parallel_heads'


def rmsnorm(x, g):
    x32 = x.astype(jnp.float32)
    y = x32 * lax.rsqrt(jnp.mean(x32 * x32, axis=-1, keepdims=True) + EPS)
    return (y * g.astype(jnp.float32)).astype(x.dtype)


def rope_tables(positions, rot_dim):
    inv_freq = ROPE_THETA ** (-jnp.arange(0, rot_dim, 2, dtype=jnp.float32) / rot_dim)
    ang = positions.astype(jnp.float32)[..., None] * inv_freq
    return jnp.cos(ang)[:, :, None, :], jnp.sin(ang)[:, :, None, :]


def rotate(x, cos, sin):
    x1, x2 = jnp.split(x.astype(jnp.float32), 2, axis=-1)
    return jnp.concatenate([x1 * cos - x2 * sin, x2 * cos + x1 * sin], axis=-1).astype(x.dtype)


def partial_rope(x, cos, sin, rot_dim):
    return jnp.concatenate([rotate(x[..., :rot_dim], cos, sin), x[..., rot_dim:]], axis=-1)


def to_blocks(a, nb):
    return a.reshape((a.shape[0], nb, QBLOCK) + a.shape[2:]).swapaxes(0, 1)


def causal_block_attention(q, k, v, scale):
    B, S, H, _ = q.shape
    nb = S // QBLOCK
    k32 = k.astype(jnp.float32)
    key_pos = jnp.arange(S)

    def one_block(args):
        q_blk, start = args
        q_pos = start + jnp.arange(QBLOCK)
        s = jnp.einsum('bqhd,bshd->bhqs', q_blk.astype(jnp.float32), k32) * scale
        s = jnp.where((key_pos[None, :] <= q_pos[:, None])[None, None], s, -jnp.inf)
        p = jax.nn.softmax(s, axis=-1)
        return jnp.einsum('bhqs,bshd->bqhd', p.astype(v.dtype), v)

    out = lax.map(one_block, (to_blocks(q, nb), jnp.arange(nb) * QBLOCK))
    return out.swapaxes(0, 1).reshape(B, S, H, v.shape[-1])


def dsa_sparse_attention(q, k, v, q_idx, k_idx, w_idx, scale, top_k):
    B, S, H, d = q.shape
    G = k.shape[2]
    R = H // G
    nb = S // QBLOCK
    k_idx32 = k_idx.astype(jnp.float32)
    key_pos = jnp.arange(S)
    gather = jax.vmap(lambda table, idx: table[idx])

    def one_block(args):
        q_blk, qi_blk, wi_blk, start = args
        q_pos = start + jnp.arange(QBLOCK)
        rel = jax.nn.relu(jnp.einsum('bqhd,bsd->bqhs', qi_blk.astype(jnp.float32), k_idx32) * IDX_DIM ** -0.5)
        score = jnp.einsum('bqhs,bqh->bqs', rel, wi_blk.astype(jnp.float32))
        score = jnp.where((key_pos[None, :] <= q_pos[:, None])[None], score, -jnp.inf)
        _, sel = lax.top_k(score, top_k)
        valid = sel <= q_pos[None, :, None]
        k_sel = gather(k, sel)
        v_sel = gather(v, sel)
        qg = q_blk.reshape(B, QBLOCK, G, R, d)
        s = jnp.einsum('bqgrd,bqkgd->bqgrk', qg.astype(jnp.float32), k_sel.astype(jnp.float32)) * scale
        s = jnp.where(valid[:, :, None, None, :], s, -jnp.inf)
        p = jax.nn.softmax(s, axis=-1)
        o = jnp.einsum('bqgrk,bqkgd->bqgrd', p.astype(v.dtype), v_sel)
        return o.reshape(B, QBLOCK, H, d)

    out = lax.map(one_block, (to_blocks(q, nb), to_blocks(q_idx, nb), to_blocks(w_idx, nb),
                              jnp.arange(nb) * QBLOCK))
    return out.swapaxes(0, 1).reshape(B, S, H, d)


def hybrid_mixer(h, cos_m, sin_m, cos_p, sin_p, top_k, w_in, mla_q_a_norm, mla_w_uq, mla_kv_a_norm,
                 mla_w_ukv, mla_q_norm, mla_k_norm, dsa_q_norm, dsa_k_norm, idx_k_norm, w_out):
    B, S, _ = h.shape
    offsets = np.cumsum(IN_SPLIT_SIZES)[:-1].tolist()
    c_q, c_kv, k_pe, q_d, k_d, v_d, q_i, k_i, w_i = jnp.split(h @ w_in, offsets, axis=-1)

    q_m = (rmsnorm(c_q, mla_q_a_norm) @ mla_w_uq).reshape(B, S, MLA_HEADS, MLA_QK_DIM)
    kv_m = (rmsnorm(c_kv, mla_kv_a_norm) @ mla_w_ukv).reshape(B, S, MLA_HEADS, MLA_NOPE_DIM + MLA_V_DIM)
    k_nope, v_m = kv_m[..., :MLA_NOPE_DIM], kv_m[..., MLA_NOPE_DIM:]
    k_m = jnp.concatenate([k_nope, jnp.broadcast_to(k_pe[:, :, None, :], (B, S, MLA_HEADS, MLA_ROPE_DIM))], axis=-1)
    q_m = rmsnorm(q_m, mla_q_norm)
    k_m = rmsnorm(k_m, mla_k_norm)
    q_m = jnp.concatenate([q_m[..., :MLA_NOPE_DIM], rotate(q_m[..., MLA_NOPE_DIM:], cos_m, sin_m)], axis=-1)
    k_m = jnp.concatenate([k_m[..., :MLA_NOPE_DIM], rotate(k_m[..., MLA_NOPE_DIM:], cos_m, sin_m)], axis=-1)
    o_mla = causal_block_attention(q_m, k_m, v_m, MLA_QK_DIM ** -0.5)

    q_d = partial_rope(rmsnorm(q_d.reshape(B, S, DSA_HEADS, DSA_HEAD_DIM), dsa_q_norm), cos_p, sin_p, PARTIAL_ROT_DIM)
    k_d = partial_rope(rmsnorm(k_d.reshape(B, S, DSA_KV_HEADS, DSA_HEAD_DIM), dsa_k_norm), cos_p, sin_p, PARTIAL_ROT_DIM)
    v_d = v_d.reshape(B, S, DSA_KV_HEADS, DSA_HEAD_DIM)
    q_i = partial_rope(q_i.reshape(B, S, IDX_HEADS, IDX_DIM), cos_p, sin_p, PARTIAL_ROT_DIM)
    k_i = partial_rope(rmsnorm(k_i, idx_k_norm)[:, :, None, :], cos_p, sin_p, PARTIAL_ROT_DIM)[:, :, 0, :]
    o_dsa = dsa_sparse_attention(q_d, k_d, v_d, q_i, k_i, w_i * IDX_HEADS ** -0.5, DSA_HEAD_DIM ** -0.5, top_k)

    mixed = jnp.concatenate([o_mla.reshape(B, S, -1), o_dsa.reshape(B, S, -1)], axis=-1)
    return mixed @ w_out


def memory_cross_attention(h, mem_n, w_q, w_k, w_v, q_norm, k_norm, w_o):
    B, S, _ = h.shape
    N = mem_n.shape[1]
    q = rmsnorm((h @ w_q).reshape(B, S, MEM_HEADS, MEM_HEAD_DIM), q_norm)
    k = rmsnorm((mem_n @ w_k).reshape(B, N, MEM_HEADS, MEM_HEAD_DIM), k_norm)
    v = (mem_n @ w_v).reshape(B, N, MEM_HEADS, MEM_HEAD_DIM)
    s = jnp.einsum('bshd,bnhd->bhsn', q.astype(jnp.float32), k.astype(jnp.float32)) * MEM_HEAD_DIM ** -0.5
    p = jax.nn.softmax(s, axis=-1)
    o = jnp.einsum('bhsn,bnhd->bshd', p.astype(v.dtype), v)
    return o.reshape(B, S, MEM_HEADS * MEM_HEAD_DIM) @ w_o


def swiglu_ffn(h, w_gate, w_up, w_down):
    return (jax.nn.silu(h @ w_gate) * (h @ w_up)) @ w_down


def setup_inputs(seed: int = 0) -> dict:
    key = jax.random.key(seed)
    ks = jax.random.split(key, 32)
    f32 = jnp.float32

    def w(k, shape, fan_in):
        return jax.random.normal(k, (DEPTH,) + shape, f32) * fan_in ** -0.5

    def g(k, n):
        return 1.0 + 0.1 * jax.random.normal(k, (DEPTH, n), f32)

    mem_w = MEM_HEADS * MEM_HEAD_DIM
    return {
        'x': jax.random.normal(ks[0], (BATCH, SEQ, D_MODEL), f32),
        'mem': jax.random.normal(ks[1], (BATCH, N_MEM, D_MODEL), f32),
        'positions': (jax.random.randint(ks[2], (BATCH, 1), 0, 1024) + jnp.arange(SEQ)[None, :]).astype(jnp.int32),
        'norm_mix': g(ks[3], D_MODEL),
        'w_in': w(ks[4], (D_MODEL, IN_COLS), D_MODEL),
        'mla_q_a_norm': g(ks[5], MLA_Q_RANK),
        'mla_w_uq': w(ks[6], (MLA_Q_RANK, MLA_HEADS * MLA_QK_DIM), MLA_Q_RANK),
        'mla_kv_a_norm': g(ks[7], MLA_KV_RANK),
        'mla_w_ukv': w(ks[8], (MLA_KV_RANK, MLA_HEADS * (MLA_NOPE_DIM + MLA_V_DIM)), MLA_KV_RANK),
        'mla_q_norm': g(ks[9], MLA_QK_DIM),
        'mla_k_norm': g(ks[10], MLA_QK_DIM),
        'dsa_q_norm': g(ks[11], DSA_HEAD_DIM),
        'dsa_k_norm': g(ks[12], DSA_HEAD_DIM),
        'idx_k_norm': g(ks[13], IDX_DIM),
        'w_out': w(ks[14], (MIX_WIDTH, D_MODEL), MIX_WIDTH),
        'norm_mem_x': g(ks[15], D_MODEL),
        'norm_mem_kv': g(ks[16], D_MODEL),
        'mem_w_q': w(ks[17], (D_MODEL, mem_w), D_MODEL),
        'mem_w_k': w(ks[18], (D_MODEL, mem_w), D_MODEL),
        'mem_w_v': w(ks[19], (D_MODEL, mem_w), D_MODEL),
        'mem_q_norm': g(ks[20], MEM_HEAD_DIM),
        'mem_k_norm': g(ks[21], MEM_HEAD_DIM),
        'mem_w_o': w(ks[22], (mem_w, D_MODEL), mem_w),
        'norm_ffn': g(ks[23], D_MODEL),
        'ffn_w_gate': w(ks[24], (D_MODEL, D_FF), D_MODEL),
        'ffn_w_up': w(ks[25], (D_MODEL, D_FF), D_MODEL),
        'ffn_w_down': w(ks[26], (D_FF, D_MODEL), D_FF),
    }


def reference(x, mem, positions, norm_mix, w_in, mla_q_a_norm, mla_w_uq, mla_kv_a_norm, mla_w_ukv,
              mla_q_norm, mla_k_norm, dsa_q_norm, dsa_k_norm, idx_k_norm, w_out, norm_mem_x, norm_mem_kv,
              mem_w_q, mem_w_k, mem_w_v, mem_q_norm, mem_k_norm, mem_w_o, norm_ffn, ffn_w_gate, ffn_w_up,
              ffn_w_down):
    S = x.shape[1]
    top_k = min(INDEX_TOPK_MAX, S // 4)
    cos_m, sin_m = rope_tables(positions, MLA_ROPE_DIM)
    cos_p, sin_p = rope_tables(positions, PARTIAL_ROT_DIM)
    for l in range(DEPTH):
        h = rmsnorm(x, norm_mix[l])
        x = x + hybrid_mixer(h, cos_m, sin_m, cos_p, sin_p, top_k, w_in[l], mla_q_a_norm[l], mla_w_uq[l],
                             mla_kv_a_norm[l], mla_w_ukv[l], mla_q_norm[l], mla_k_norm[l], dsa_q_norm[l],
                             dsa_k_norm[l], idx_k_norm[l], w_out[l])
        x = x + memory_cross_attention(rmsnorm(x, norm_mem_x[l]), rmsnorm(mem, norm_mem_kv[l]), mem_w_q[l],
                                       mem_w_k[l], mem_w_v[l], mem_q_norm[l], mem_k_norm[l], mem_w_o[l])
        x = x + swiglu_ffn(rmsnorm(x, norm_ffn[l]), ffn_w_gate[l], ffn_w_up[l], ffn_w_down[l])
    return x
```

```python
import math
from contextlib import ExitStack

import numpy as np
import concourse.bass as bass
import concourse.mybir as mybir
from concourse.bass_utils import run_bass_kernel_spmd

F32 = mybir.dt.float32
BF16 = mybir.dt.bfloat16
I32 = mybir.dt.int32
AF = mybir.ActivationFunctionType
ALU = mybir.AluOpType
AX = mybir.AxisListType

D = 1024
NMEM = 256
DFF = 2816
EPS = 1e-6
TOPK = 256
BISECT_ITERS = 20
NEG = -1.0e30
O_CQ, O_CKV, O_KPE, O_QD, O_KD, O_VD, O_QI, O_KI, O_WI = 0, 256, 384, 416, 928, 1056, 1184, 1696, 1760
THETA = 500000.0


class Sched:
    def __init__(self, nc):
        self.nc = nc
        self.engs = {"pe": nc.tensor, "act": nc.scalar, "dve": nc.vector, "pool": nc.gpsimd, "sp": nc.sync}
        self.sems = {}
        self.cnt = {}
        self.seen = {k: {} for k in self.engs}
        self.lastw = {}
        self.readers = {}
        self._stack = []
        self.ninst = 0
        self.nwait = 0
        for k in ["pe", "act", "dve", "pool"]:
            self.sems[k] = self.sem("s_" + k)
            self.cnt[k] = 0
        self.dsems = [[self.sem("d%d" % i), 0] for i in range(24)]
        self.dnext = 0
        self.out_events = []

    def sem(self, name):
        cm = self.nc.semaphore(name)
        s = cm.__enter__()
        self._stack.append(cm)
        return s

    def _wait(self, e, ev):
        if ev is None:
            return
        s, v = ev
        if e == "pe" and s is self.sems["pe"]:
            return
        key = id(s)
        if self.seen[e].get(key, 0) >= v:
            return
        self.engs[e].wait_ge(s, v)
        self.seen[e][key] = v
        self.nwait += 1

    def deps(self, e, reads, writes):
        for r in reads:
            self._wait(e, self.lastw.get(r))
        for w in writes:
            self._wait(e, self.lastw.get(w))
            for ev in self.readers.get(w, {}).values():
                self._wait(e, ev)

    def record(self, ev, reads, writes):
        for r in reads:
            d = self.readers.setdefault(r, {})
            d[id(ev[0])] = ev
        for w in writes:
            self.lastw[w] = ev
            self.readers[w] = {}

    def op(self, e, fn, reads=(), writes=()):
        self.deps(e, reads, writes)
        inst = fn(self.engs[e])
        self.cnt[e] += 1
        inst.then_inc(self.sems[e], 1)
        ev = (self.sems[e], self.cnt[e])
        self.record(ev, reads, writes)
        self.ninst += 1
        return ev

    def dma(self, out, in_, reads=(), writes=(), is_out=False):
        q = "sp"
        ds = self.dsems[self.dnext % len(self.dsems)]
        self.dnext += 1
        if ds[1] > 0:
            self._wait(q, (ds[0], ds[1]))
        self.deps(q, reads, writes)
        inst = self.engs[q].dma_start(out=out, in_=in_)
        ds[1] += 16
        inst.then_inc(ds[0], 16)
        ev = (ds[0], ds[1])
        self.record(ev, reads, writes)
        self.ninst += 1
        if is_out:
            self.out_events.append(ev)
        return ev

    def barrier(self):
        evs = [(self.sems[k], self.cnt[k]) for k in ["pe", "act", "dve", "pool"] if self.cnt[k] > 0]
        evs += [(d[0], d[1]) for d in self.dsems if d[1] > 0]
        for e in ["pe", "act", "dve", "pool", "sp"]:
            for ev in evs:
                self._wait(e, ev)

    def finish(self):
        for ev in self.out_events:
            self._wait("sp", ev)
        for d in self.dsems:
            if d[1] > 0:
                self._wait("sp", (d[0], d[1]))
        for k in ["pe", "act", "dve", "pool"]:
            if self.cnt[k] > 0:
                self._wait("sp", (self.sems[k], self.cnt[k]))
        for cm in reversed(self._stack):
            cm.__exit__(None, None, None)


def build(S, NH, dbg=False):
    T_OWN = S // 2
    T_H = T_OWN // NH
    N = min(512, T_H)
    NB = N // 128
    NCH_ALL = S // N
    NCH_H = T_H // N
    NKB = S // 128
    nc = bass.Bass("TRN2", target_bir_lowering=False)

    def dram(name, shape, dt=F32, kind="ExternalInput"):
        return nc.dram_tensor(name, shape, dt, kind=kind).ap()

    xT_all = dram("xT_all", [D, S])
    xT_own = dram("xT_own", [D, T_OWN])
    pos_all = dram("pos_all", [1, S], I32)
    pos_own = dram("pos_own", [1, T_OWN], I32)
    memT = dram("memT", [D, NMEM])
    w_in = dram("w_in", [D, 1768])
    w_uq = dram("w_uq", [256, 768])
    w_ukv = dram("w_ukv", [128, 1024])
    w_out = dram("w_out", [D, D])
    m_wq = dram("m_wq", [D, 512])
    m_wk = dram("m_wk", [D, 512])
    m_wv = dram("m_wv", [D, 512])
    m_wo = dram("m_wo", [512, D])
    f_wg = dram("f_wg", [D, DFF])
    f_wu = dram("f_wu", [D, DFF])
    f_wd = dram("f_wd", [DFF, D])
    gm_d = dram("gm", [128, 42])
    cb_d = dram("cb", [128, 6 * 128])
    cf_d = dram("cf", [128, 66])
    mI_d = dram("mI", [128, 256])
    mT_d = dram("mT", [128, 2 * NB * N])
    outT = dram("outT", [D, T_OWN], kind="ExternalOutput")
    if dbg:
        dbg_mixed = dram("dbg_mixed", [D, T_OWN], kind="ExternalOutput")

    Sc = Sched(nc)
    uid = [0]

    def nm(p):
        uid[0] += 1
        return "%s_%d" % (p, uid[0])

    def sb(es, shape, dt, name="t"):
        return es.enter_context(nc.sbuf_tensor(nm(name), shape, dt))

    def ps(es, shape, dt, name="p"):
        return es.enter_context(nc.psum_tensor(nm(name), shape, dt))

    op = Sc.op

    from contextlib import contextmanager

    @contextmanager
    def scope():
        with ExitStack() as es_:
            yield es_
        Sc.barrier()

    def mm(out, lhsT, rhs, start, stop, reads, writes):
        return op("pe", lambda e: e.matmul(out, lhsT=lhsT, rhs=rhs, start=start, stop=stop), reads, writes)

    def act(out, in_, func, reads, writes, scale=None, bias=None):
        kw = {}
        if scale is not None:
            kw["scale"] = scale
        if bias is not None:
            kw["bias"] = bias
        return op("act", lambda e: e.activation(out=out, in_=in_, func=func, **kw), reads, writes)

    def ts(eng, out, in0, s1, op0, reads, writes, s2=None, op1=None, accum=None):
        kw = {}
        if op1 is not None:
            kw["op1"] = op1
        if accum is not None:
            kw["accum_out"] = accum
        return op(eng, lambda e: e.tensor_scalar(out=out, in0=in0, scalar1=s1, scalar2=s2, op0=op0, **kw), reads, writes)

    def tt(eng, out, in0, in1, o, reads, writes):
        return op(eng, lambda e: e.tensor_tensor(out=out, in0=in0, in1=in1, op=o), reads, writes)

    def stt(out, in0, scalar, in1, op0, op1, reads, writes):
        return op("dve", lambda e: e.scalar_tensor_tensor(out=out, in0=in0, scalar=scalar, in1=in1, op0=op0, op1=op1), reads, writes)

    def cp(eng, out, in_, reads, writes):
        if eng == "act":
            return act(out, in_, AF.Copy, reads, writes)
        return op(eng, lambda e: e.tensor_copy(out=out, in_=in_), reads, writes)

    with ExitStack() as top:
        gm = sb(top, [128, 42], F32, "gm")
        cf = sb(top, [128, 66], F32, "cf")
        cbf = sb(top, [128, 6 * 128], BF16, "cbf")
        gs = sb(top, [128, 4], F32, "gs")
        epsc = sb(top, [128, 1], F32, "epsc")
        mI = sb(top, [128, 256], F32, "mI")
        mixD = sb(top, [128, 4, T_H], BF16, "mixD")
        with scope() as es:
            st = sb(es, [128, 6 * 128], F32, "cst")
            Sc.dma(st[:], cb_d[:, :], writes=["cst"])
            cp("dve", cbf[:], st[:], ["cst"], ["cbf"])
        Sc.dma(gm[:], gm_d[:, :], writes=["gm"])
        Sc.dma(cf[:], cf_d[:, :], writes=["cf"])
        Sc.dma(mI[:], mI_d[:, :], writes=["mI"])
        op("dve", lambda e: e.memset(epsc[:], EPS), (), ["epsc"])
        ts("dve", gs[:, 0:1], gm[:, 35:36], 96 ** -0.5, ALU.mult, ["gm"], ["gs"])
        ts("dve", gs[:, 1:2], gm[:, 37:38], 64 ** -0.5, ALU.mult, ["gm"], ["gs"])
        ts("dve", gs[:, 2:3], gm[:, 40:41], 128 ** -0.5, ALU.mult, ["gm"], ["gs"])
        ones128 = cbf[:, 0:128]
        B64 = cbf[:, 128:256]
        O96 = cbf[:, 256:384]
        P_part = cbf[:, 384:512]
        P_mla = cbf[:, 512:640]
        ident = cbf[:, 640:768]
        sel = cf[:, 0:64]
        invf_part = cf[:, 64:65]
        invf_mla = cf[:, 65:66]
        CONST = ["gm", "cf", "cbf", "gs", "epsc"]

        PB = [ps(top, [128, 512], F32, "pb%d" % i) for i in range(7)]
        PT = ps(top, [128, 1024], BF16, "pt")
        PBN = ["pb%d" % i for i in range(7)]

        def load_w(es, dst, src, rows, cols, c0, dcol0, name):
            kc = rows // 128
            stg = sb(es, [128, kc, cols], F32, "wst")
            r = nm("wst")
            Sc.dma(stg[:], src[:, c0:c0 + cols].rearrange("(k p) c -> p k c", p=128), writes=[r])
            cp("pool", dst[:, :, dcol0:dcol0 + cols], stg[:], [r], [name])

        def rstd_from_ps(pss, psname, rows, n, dim, out, outname, tmp, tmpname):
            act(tmp[0:rows, 0:n], pss[0:rows, 0:n], AF.Ln, [psname, "epsc"], [tmpname], scale=1.0 / dim, bias=epsc[0:rows, :])
            act(out[0:rows, 0:n], tmp[0:rows, 0:n], AF.Exp, [tmpname], [outname], scale=-0.5)

        def norm_chunk(xs, xsname, n, gcol0, h, hname, sq, sqname, bank, tmp, tmpname, rs, rsname):
            act(sq[:, :, 0:n], xs[:, :, 0:n], AF.Square, [xsname], [sqname])
            for k in range(8):
                mm(PB[bank][:, 0:n], ones128, sq[:, k, 0:n], k == 0, k == 7, ["cbf", sqname], [PBN[bank]])
            rstd_from_ps(PB[bank], PBN[bank], 128, n, float(D), rs, rsname, tmp, tmpname)
            for k in range(8):
                stt(h[:, k, 0:n], xs[:, k, 0:n], gm[:, gcol0 + k:gcol0 + k + 1], rs[:, 0:n], ALU.mult, ALU.mult,
                    [xsname, "gm", rsname], [hname])

        def rope_tmp(es, n):
            return {"pi": sb(es, [128, n], I32, "rp_pi"), "pf": sb(es, [128, n], F32, "rp_pf"), "a": sb(es, [128, n], F32, "rp_a"),
                    "u": sb(es, [128, n], F32, "rp_u"), "ki": sb(es, [128, n], I32, "rp_ki"), "r": sb(es, [128, n], F32, "rp_r"),
                    "t": sb(es, [128, n], F32, "rp_t"), "k": nm("rp")}

        def rope_tables(RT, pos_d, t0, n, invf, rows, Ct, Ctn, St, Stn, cview=None, sview=None):
            pi, pf, a, u, ki, r, t, k = RT["pi"], RT["pf"], RT["a"], RT["u"], RT["ki"], RT["r"], RT["t"], RT["k"]
            R = slice(0, rows)
            Sc.dma(pi[:], pos_d[0:1, t0:t0 + n].to_broadcast([128, n]), writes=[k + "pi"])
            cp("pool", pf[R, :], pi[R, :], [k + "pi"], [k + "pf"])
            ts("pool", pf[R, :], pf[R, :], invf[R, :], ALU.mult, [k + "pf", "cf"], [k + "pf"])
            for shift, dst, dstn, view in ((0.0, St, Stn, sview), (math.pi / 2, Ct, Ctn, cview)):
                ts("pool", a[R, :], pf[R, :], shift, ALU.add, [k + "pf"], [k + "a"])
                ts("pool", u[R, :], a[R, :], 1.0 / (2 * math.pi), ALU.mult, [k + "a"], [k + "u"])
                cp("pool", ki[R, :], u[R, :], [k + "u"], [k + "ki"])
                cp("pool", u[R, :], ki[R, :], [k + "ki"], [k + "u"])
                ts("pool", u[R, :], u[R, :], -2 * math.pi, ALU.mult, [k + "u"], [k + "u"])
                tt("pool", r[R, :], u[R, :], a[R, :], ALU.add, [k + "u", k + "a"], [k + "r"])
                ts("pool", t[R, :], r[R, :], math.pi, ALU.is_gt, [k + "r"], [k + "t"], s2=-2 * math.pi, op1=ALU.mult)
                tt("pool", r[R, :], r[R, :], t[R, :], ALU.add, [k + "r", k + "t"], [k + "r"])
                ts("pool", t[R, :], r[R, :], -math.pi, ALU.is_lt, [k + "r"], [k + "t"], s2=2 * math.pi, op1=ALU.mult)
                tt("pool", r[R, :], r[R, :], t[R, :], ALU.add, [k + "r", k + "t"], [k + "r"])
                ts("pool", r[R, :], r[R, :], math.pi, ALU.min, [k + "r"], [k + "r"], s2=-math.pi, op1=ALU.max)
                act(dst[R, :] if view is None else view, r[R, :], AF.Sin, [k + "r"], [dstn])

        def head_norm_rope(src, srcname, rows, n, blk, dim, gcol, Ct, Ctn, St, Stn, Pm, out, outname, bank, W, wn):
            R = slice(0, rows)
            if blk is not None:
                act(W["sq"][R, 0:n], src, AF.Square, [srcname], [wn + "sq"])
                mm(PB[bank][R, 0:n], blk[R, R], W["sq"][R, 0:n], True, True, ["cbf", wn + "sq"], [PBN[bank]])
                rstd_from_ps(PB[bank], PBN[bank], rows, n, float(dim), W["rs"], wn + "rs", W["tmp"], wn + "tmp")
                dst = W["xn"][R, 0:n] if Ct is not None else out
                dn = wn + "xn" if Ct is not None else outname
                stt(dst, src, gcol, W["rs"][R, 0:n], ALU.mult, ALU.mult, [srcname, "gm", "gs", wn + "rs"], [dn])
            else:
                cp("act", W["xn"][R, 0:n], src, [srcname], [wn + "xn"])
            if Ct is not None:
                mm(PB[bank][R, 0:n], Pm[R, R], W["xn"][R, 0:n], True, True, ["cbf", wn + "xn"], [PBN[bank]])
                tt("pool", W["t1"][R, 0:n], W["xn"][R, 0:n], Ct, ALU.mult, [wn + "xn", Ctn], [wn + "t1"])
                tt("dve", W["t2"][R, 0:n], PB[bank][R, 0:n], St, ALU.mult, [PBN[bank], Stn], [wn + "t2"])
                i0, i1 = W["t1"][R, 0:n], W["t2"][R, 0:n]
                if isinstance(out, tuple):
                    tt("dve", out[0], W["t1"][0:64, 0:n], W["t2"][0:64, 0:n], ALU.add, [wn + "t1", wn + "t2"], [outname])
                    tt("dve", out[1], W["t1"][64:128, 0:n], W["t2"][64:128, 0:n], ALU.add, [wn + "t1", wn + "t2"], [outname])
                    return
                if len(out.shape) == 3:
                    i0 = i0.rearrange("p (b q) -> p b q", q=128)
                    i1 = i1.rearrange("p (b q) -> p b q", q=128)
                tt("dve", out, i0, i1, ALU.add, [wn + "t1", wn + "t2"], [outname])

        def chunk_pipeline(es, nch, src_d, pos_d, tokoff, invf, rows, gcol0, pref, stage2, want_rope=True):
            xs = sb(es, [128, 8, N], F32, pref + "xs")
            hh_ = [sb(es, [128, 8, N], BF16, pref + "h%d" % i) for i in range(2)]
            sq = sb(es, [128, 8, N], BF16, pref + "sq")
            rs1 = sb(es, [128, N], F32, pref + "rs1")
            tmp1 = sb(es, [128, N], F32, pref + "tmp1")
            if want_rope:
                Cts = [sb(es, [128, N], F32, pref + "C%d" % i) for i in range(2)]
                Sts = [sb(es, [128, N], F32, pref + "S%d" % i) for i in range(2)]
                RT = rope_tmp(es, N)

            def s1a(c):
                i = c % 2
                t0 = tokoff + c * N
                Sc.dma(xs[:], src_d[:, t0:t0 + N].rearrange("(k p) n -> p k n", p=128), writes=[pref + "xs"])
                if want_rope:
                    rope_tables(RT, pos_d, t0, N, invf, rows, Cts[i], pref + "C%d" % i, Sts[i], pref + "S%d" % i)
                act(sq[:, :, 0:N], xs[:, :, 0:N], AF.Square, [pref + "xs"], [pref + "sq"])
                for k in range(8):
                    mm(PB[0][:, 0:N], ones128, sq[:, k, 0:N], k == 0, k == 7, ["cbf", pref + "sq"], [PBN[0]])
                rstd_from_ps(PB[0], PBN[0], 128, N, float(D), rs1, pref + "rs1", tmp1, pref + "tmp1")

            def s1b(c):
                i = c % 2
                for k in range(8):
                    stt(hh_[i][:, k, 0:N], xs[:, k, 0:N], gm[:, gcol0 + k:gcol0 + k + 1], rs1[:, 0:N], ALU.mult, ALU.mult,
                        [pref + "xs", "gm", pref + "rs1"], [pref + "h%d" % i])
            s1a(0)
            s1b(0)
            for c in range(nch):
                if c + 1 < nch:
                    s1a(c + 1)
                i = c % 2
                if want_rope:
                    stage2(c, hh_[i], pref + "h%d" % i, Cts[i], pref + "C%d" % i, Sts[i], pref + "S%d" % i)
                else:
                    stage2(c, hh_[i], pref + "h%d" % i, None, None, None, None)
                if c + 1 < nch:
                    s1b(c + 1)

        def work_tiles(es, n, pref):
            W = {"sq": sb(es, [128, n], BF16, pref + "sq"), "rs": sb(es, [128, n], F32, pref + "rs"),
                 "tmp": sb(es, [128, n], F32, pref + "tmp"), "xn": sb(es, [128, n], BF16, pref + "xn"),
                 "t1": sb(es, [128, n], F32, pref + "t1"), "t2": sb(es, [128, n], F32, pref + "t2")}
            return W

        def attn_epilogue(psO, psOn, bankB, n, W, wn, writer):
            cp("act", W["osb"][0:65, 0:n], psO[0:65, 0:n], [psOn], [wn + "osb"])
            mm(PB[bankB][0:64, 0:n], sel[0:65, 0:64], W["osb"][0:65, 0:n], True, True, ["cf", wn + "osb"], [PBN[bankB]])
            op("dve", lambda e: e.reciprocal(out=W["rinv"][0:64, 0:n], in_=PB[bankB][0:64, 0:n]), [PBN[bankB]], [wn + "rinv"])
            writer(W["osb"], W["rinv"])

        KmT = sb(top, [128, 4, NMEM], BF16, "KmT")
        Vm = sb(top, [128, 2, 512], BF16, "Vm")
        with scope() as es:
            wk = sb(es, [128, 8, 512], BF16, "wk")
            wv = sb(es, [128, 8, 512], BF16, "wv")
            with scope() as e2:
                load_w(e2, wk, m_wk, D, 512, 0, 0, "wk")
                load_w(e2, wv, m_wv, D, 512, 0, 0, "wv")
            ms = sb(es, [128, 8, NMEM], F32, "ms")
            mh = sb(es, [128, 8, NMEM], BF16, "mh")
            msq = sb(es, [128, 8, NMEM], BF16, "msq")
            W = work_tiles(es, NMEM, "mw")
            Sc.dma(ms[:], memT.rearrange("(k p) n -> p k n", p=128), writes=["ms"])
            norm_chunk(ms, "ms", NMEM, 16, mh, "mh", msq, "msq", 0, W["tmp"], "mwtmp", W["rs"], "mwrs")
            for hh in range(4):
                for k in range(8):
                    mm(PB[1][:, 0:NMEM], wk[:, k, hh * 128:(hh + 1) * 128], mh[:, k, :], k == 0, k == 7, ["wk", "mh"], ["pb1"])
                head_norm_rope(PB[1][:, 0:NMEM], "pb1", 128, NMEM, ones128, 128, gm[:, 41:42], None, None, None, None, None,
                               KmT[:, hh, :], "KmT", 2, W, "mw")
            for blk in range(2):
                for k in range(8):
                    mm(PB[3][:, 0:512], mh[:, k, blk * 128:(blk + 1) * 128], wv[:, k, :], k == 0, k == 7, ["mh", "wv"], ["pb3"])
                cp("act", Vm[:, blk, :], PB[3][:, 0:512], ["pb3"], ["Vm"])

        for hf in range(NH):
            tok0 = hf * T_H
            with scope() as pd:
                NBH = T_H // 128
                qdT = sb(pd, [128, NBH, 4, 128], BF16, "qdT")
                qiT = sb(pd, [128, NBH, 4, 128], BF16, "qiT")
                wabs = sb(pd, [128, NBH, 8], F32, "wabs")
                wsgn = sb(pd, [128, NBH, 8], F32, "wsgn")
                with scope() as es:
                    Wq = sb(es, [128, 8, 1032], BF16, "Wq")
                    for c4 in range(4):
                        with scope() as e2:
                            load_w(e2, Wq, w_in, D, 64, O_QD + c4 * 64, c4 * 128, "Wq")
                            load_w(e2, Wq, w_in, D, 64, O_QD + (4 + c4) * 64, c4 * 128 + 64, "Wq")
                    for q4 in range(2):
                        with scope() as e2:
                            load_w(e2, Wq, w_in, D, 256, O_QI + q4 * 256, 512 + q4 * 256, "Wq")
                    with scope() as e2:
                        load_w(e2, Wq, w_in, D, 8, O_WI, 1024, "Wq")
                    W = work_tiles(es, N, "bw")
                    wtok = sb(es, [128, NB, 8], F32, "wtok")

                    def q_stage2(c, h, hn, Ct, Ctn, St, Stn):
                        b0 = c * NB
                        for c4 in range(4):
                            for k in range(8):
                                mm(PB[1][:, 0:N], Wq[:, k, c4 * 128:(c4 + 1) * 128], h[:, k, :], k == 0, k == 7, ["Wq", hn], ["pb1"])
                            head_norm_rope(PB[1][:, 0:N], "pb1", 128, N, B64, 64, gs[:, 1:2], Ct[:, :], Ctn, St[:, :], Stn, P_part,
                                           qdT[:, b0:b0 + NB, c4, :], "qdT", 2, W, "bw")
                        for c4 in range(4):
                            for k in range(8):
                                mm(PB[3][:, 0:N], Wq[:, k, 512 + c4 * 128:512 + (c4 + 1) * 128], h[:, k, :], k == 0, k == 7, ["Wq", hn], ["pb3"])
                            head_norm_rope(PB[3][:, 0:N], "pb3", 128, N, None, 64, None, Ct[:, :], Ctn, St[:, :], Stn, P_part,
                                           qiT[:, b0:b0 + NB, c4, :], "qiT", 4, W, "bw")
                        for b in range(NB):
                            for k in range(8):
                                mm(PB[5][:, 0:8], h[:, k, b * 128:(b + 1) * 128], Wq[:, k, 1024:1032], k == 0, k == 7, [hn, "Wq"], ["pb5"])
                            cp("act", wtok[:, b, :], PB[5][:, 0:8], ["pb5"], ["wtok"])
                        act(wabs[:, b0:b0 + NB, :], wtok[:], AF.Abs, ["wtok"], ["wabs"], scale=(8 ** -0.5) * (64 ** -0.5))
                        ts("dve", wsgn[:, b0:b0 + NB, :], wtok[:], 0.0, ALU.is_ge, ["wtok"], ["wsgn"], s2=2.0, op1=ALU.mult)
                        ts("dve", wsgn[:, b0:b0 + NB, :], wsgn[:, b0:b0 + NB, :], -1.0, ALU.add, ["wsgn"], ["wsgn"])
                    chunk_pipeline(es, NCH_H, xT_own, pos_own, tok0, invf_part, 128, 0, "q", q_stage2)
                kdT = sb(pd, [128, S], BF16, "kdT")
                kiT_lo = sb(pd, [128, S], BF16, "kiTlo")
                kiT_hi = sb(pd, [128, S], BF16, "kiThi")
                op("pool", lambda e: e.memset(kiT_lo[64:128, :], 0.0), (), ["kiT"])
                op("pool", lambda e: e.memset(kiT_hi[0:64, :], 0.0), (), ["kiT"])
                vd = sb(pd, [128, NKB, 2, 65], BF16, "vd")
                op("pool", lambda e: e.memset(vd[:, :, :, 64:65], 1.0), (), ["vd"])
                with scope() as es:
                    Wk = sb(es, [128, 8, 384], BF16, "Wk1")
                    with scope() as e2:
                        load_w(e2, Wk, w_in, D, 128, O_KD, 0, "Wk1")
                        load_w(e2, Wk, w_in, D, 128, O_VD, 128, "Wk1")
                        load_w(e2, Wk, w_in, D, 64, O_KI, 256, "Wk1")
                        load_w(e2, Wk, w_in, D, 64, O_KI, 320, "Wk1")
                    W = work_tiles(es, N, "aw")

                    def a1_stage2(c, h, hn, Ct, Ctn, St, Stn):
                        t0 = c * N
                        for k in range(8):
                            mm(PB[1][:, 0:N], Wk[:, k, 0:128], h[:, k, :], k == 0, k == 7, ["Wk1", hn], ["pb1"])
                        head_norm_rope(PB[1][:, 0:N], "pb1", 128, N, B64, 64, gm[:, 38:39], Ct[:, :], Ctn, St[:, :], Stn, P_part,
                                       kdT[:, t0:t0 + N], "kdT", 2, W, "aw")
                        for k in range(8):
                            mm(PB[3][:, 0:N], Wk[:, k, 256:384], h[:, k, :], k == 0, k == 7, ["Wk1", hn], ["pb3"])
                        head_norm_rope(PB[3][:, 0:N], "pb3", 128, N, B64, 64, gm[:, 39:40], Ct[:, :], Ctn, St[:, :], Stn, P_part,
                                       (kiT_lo[0:64, t0:t0 + N], kiT_hi[64:128, t0:t0 + N]), "kiT", 4, W, "aw")
                        for b in range(N // 128):
                            kb = (t0 // 128) + b
                            for k in range(8):
                                mm(PB[5][:, 0:128], h[:, k, b * 128:(b + 1) * 128], Wk[:, k, 128:256], k == 0, k == 7, [hn, "Wk1"], ["pb5"])
                            cp("act", vd[:, kb, :, 0:64], PB[5][:, 0:128].rearrange("p (g d) -> p g d", g=2), ["pb5"], ["vd"])
                    chunk_pipeline(es, NCH_ALL, xT_all, pos_all, 0, invf_part, 128, 0, "a", a1_stage2)
                with scope() as es:
                    W = {}
                    W["osb"] = sb(es, [128, 512], F32, "bwosb")
                    W["rinv"] = sb(es, [128, 512], F32, "bwrinv")
                    Dg = sb(es, [128, 8, 128], BF16, "Dg")
                    Isb = sb(es, [128, S], F32, "Isb")
                    junk = sb(es, [128, S], mybir.dt.uint8, "junk")
                    Mall = sb(es, [128, S], BF16, "Mall")
                    Rh = [sb(es, [128, 512], BF16, "Rh%d" % i) for i in range(3)]
                    MT = [sb(es, [128, 4, 128], BF16, "MT%d" % i) for i in range(2)]
                    Eb = [sb(es, [128, 512], BF16, "Eb%d" % i) for i in range(3)]
                    bs = sb(es, [128, 8], F32, "bs")
                    qb0 = tok0 // 128

                    def stage_idx(b):
                        ext = 256 * (qb0 + b + 1)
                        for hh in range(8):
                            ts("dve", Dg[:, hh, :], ident, wsgn[:, b, hh:hh + 1], ALU.mult, ["cbf", "wsgn"], ["Dg"])
                        k0 = 0
                        while k0 < ext:
                            kw = min(512, ext - k0)

                            def ymm(hh, k0=k0, kw=kw):
                                kt = kiT_lo if hh % 2 == 0 else kiT_hi
                                mm(PB[hh % 2][:, 0:kw], qiT[:, b, hh // 2, :], kt[:, k0:k0 + kw],
                                   True, True, ["qiT", "kiT"], [PBN[hh % 2]])
                            ymm(0)
                            for hh in range(8):
                                if hh + 1 < 8:
                                    ymm(hh + 1)
                                r = Rh[hh % 3]
                                rn = "Rh%d" % (hh % 3)
                                if hh % 2 == 0:
                                    act(r[:, 0:kw], PB[hh % 2][:, 0:kw], AF.Relu, [PBN[hh % 2], "wabs"], [rn], scale=wabs[:, b, hh:hh + 1])
                                else:
                                    ts("dve", r[:, 0:kw], PB[hh % 2][:, 0:kw], 0.0, ALU.max, [PBN[hh % 2], "wabs"], [rn],
                                       s2=wabs[:, b, hh:hh + 1], op1=ALU.mult)
                                mm(PB[2][:, 0:kw], Dg[:, hh, :], r[:, 0:kw], hh == 0, hh == 7, ["Dg", rn], ["pb2"])
                            cp("act", Isb[:, k0:k0 + kw], PB[2][:, 0:kw], ["pb2"], ["Isb"])
                            k0 += kw

                    def stage_thr(b):
                        ext = 256 * (qb0 + b + 1)
                        op("dve", lambda e: e.tensor_reduce(out=bs[:, 5:6], in_=Isb[:, 0:ext], axis=AX.X, op=ALU.max, apply_absolute_value=True),
                           ["Isb"], ["bs"])
                        tt("dve", Isb[:, ext - 256:ext], Isb[:, ext - 256:ext], mI[:, :], ALU.add, ["Isb", "mI"], ["Isb"])
                        ts("dve", bs[:, 0:1], bs[:, 5:6], -1.0, ALU.mult, ["bs"], ["bs"], s2=-1.0, op1=ALU.add)
                        ts("dve", bs[:, 1:2], bs[:, 5:6], 2.0, ALU.mult, ["bs"], ["bs"], s2=2.0, op1=ALU.add)
                        for it in range(BISECT_ITERS):
                            cst = 2.0 ** -(it + 1)
                            stt(bs[:, 2:3], bs[:, 1:2], cst, bs[:, 0:1], ALU.mult, ALU.add, ["bs"], ["bs"])
                            ts("dve", junk[:, 0:ext], Isb[:, 0:ext], bs[:, 2:3], ALU.is_ge, ["Isb", "bs"], ["junk", "bs"],
                               op1=ALU.add, accum=bs[:, 3:4])
                            ts("dve", bs[:, 4:5], bs[:, 3:4], TOPK - 0.5, ALU.is_ge, ["bs"], ["bs"], s2=cst, op1=ALU.mult)
                            stt(bs[:, 0:1], bs[:, 4:5], bs[:, 1:2], bs[:, 0:1], ALU.mult, ALU.add, ["bs"], ["bs"])

                    def stage_mall(b):
                        ext = 256 * (qb0 + b + 1)
                        ts("dve", Mall[:, 0:ext], Isb[:, 0:ext], bs[:, 0:1], ALU.is_lt, ["Isb", "bs"], ["Mall"], s2=-30000.0, op1=ALU.mult)

                    def stage_attn(b):
                        ext = 256 * (qb0 + b + 1)
                        nkb = ext // 128
                        ngrp = (nkb + 3) // 4

                        def prep(gi):
                            kc = gi * 4
                            nb4 = min(4, nkb - kc)
                            mt = MT[gi % 2]
                            mtn = "MT%d" % (gi % 2)
                            for j in range(nb4):
                                op("pe", lambda e: e.transpose(out=PT[:, j * 128:(j + 1) * 128], in_=Mall[:, (kc + j) * 128:(kc + j + 1) * 128], identity=ident),
                                   ["Mall", "cbf"], ["pt"])
                            cp("act", mt[:, 0:nb4, :], PT[:, 0:nb4 * 128].rearrange("p (a q) -> p a q", a=nb4), ["pt"], [mtn])

                        units = [(kb, g) for kb in range(nkb) for g in range(2)]

                        def emit_S(u):
                            kb, g = units[u]
                            G = slice(g * 64, (g + 1) * 64)
                            bk = 3 + (u % 2)
                            mt = MT[(kb // 4) % 2]
                            mtn = "MT%d" % ((kb // 4) % 2)
                            mm(PB[bk][:, 0:512], kdT[G, kb * 128:(kb + 1) * 128], qdT[G, b, :, :].rearrange("p a q -> p (a q)"),
                               True, False, ["kdT", "qdT"], [PBN[bk]])
                            mm(PB[bk][:, 0:512].rearrange("p (a q) -> p a q", a=4), ident, mt[:, (kb % 4):(kb % 4) + 1, :].to_broadcast([128, 4, 128]),
                               False, True, ["cbf", mtn], [PBN[bk]])

                        def emit_rest(u):
                            kb, g = units[u]
                            bk = 3 + (u % 2)
                            u2 = u % 3
                            act(Eb[u2][:, :], PB[bk][:, 0:512], AF.Exp, [PBN[bk]], ["Eb%d" % u2])
                            mm(PB[5 + g][0:65, 0:512], vd[:, kb, g, :], Eb[u2][:, :], kb == 0, kb == nkb - 1, ["vd", "Eb%d" % u2], [PBN[5 + g]])

                        prep(0)
                        if ngrp > 1:
                            prep(1)
                        emit_S(0)
                        for u in range(len(units)):
                            if u + 1 < len(units):
                                emit_S(u + 1)
                            emit_rest(u)
                            kb, g = units[u]
                            if g == 1 and kb % 4 == 3 and (kb // 4) + 2 < ngrp:
                                prep(kb // 4 + 2)
                        for g in range(2):
                            def writer(osb, rinv, g=g, b=b):
                                for a in range(4):
                                    hd = g * 4 + a
                                    col = b * 128
                                    tt("dve", mixD[(hd % 2) * 64:(hd % 2) * 64 + 64, hd // 2, col:col + 128],
                                       osb[0:64, a * 128:(a + 1) * 128], rinv[0:64, a * 128:(a + 1) * 128], ALU.mult,
                                       ["bwosb", "bwrinv"], ["mixD"])
                            attn_epilogue(PB[5 + g], PBN[5 + g], 3, 512, W, "bw", writer)

                    stage_idx(0)
                    stage_thr(0)
                    stage_mall(0)
                    for b in range(NBH):
                        if b + 1 < NBH:
                            stage_idx(b + 1)
                            stage_thr(b + 1)
                        stage_attn(b)
                        if b + 1 < NBH:
                            stage_mall(b + 1)

            with scope() as pmd:
                mixM = sb(pmd, [128, 4, T_H], BF16, "mixM")
                mT = sb(pmd, [128, 2 * NB, N], BF16, "mT")
                with scope() as es:
                    st2 = sb(es, [128, 2 * NB * N], F32, "cst2")
                    Sc.dma(st2[:], mT_d[:, :], writes=["cst2"])
                    cp("dve", mT[:].rearrange("p a n -> p (a n)"), st2[:], ["cst2"], ["mT"])
                with scope() as pm:
                    ckvnT = sb(pm, [128, S], BF16, "ckvnT")
                    kpeX = sb(pm, [128, S], BF16, "kpeX")
                    cqnT = sb(pm, [128, 2, T_H], BF16, "cqnT")
                    tabC = sb(pm, [128, T_H], BF16, "tabC")
                    tabS = sb(pm, [128, T_H], BF16, "tabS")
                    with scope() as es:
                        Wk = sb(es, [128, 8, 256], BF16, "Wk2")
                        with scope() as e2:
                            load_w(e2, Wk, w_in, D, 128, O_CKV, 0, "Wk2")
                            load_w(e2, Wk, w_in, D, 32, O_KPE, 128, "Wk2")
                            load_w(e2, Wk, w_in, D, 32, O_KPE, 192, "Wk2")
                        W = work_tiles(es, N, "cw")
                        op("pool", lambda e: e.memset(Wk[:, :, 160:192], 0.0), (), ["Wk2"])
                        op("pool", lambda e: e.memset(Wk[:, :, 224:256], 0.0), (), ["Wk2"])

                        def a2_stage2(c, h, hn, Ct, Ctn, St, Stn):
                            t0 = c * N
                            for k in range(8):
                                mm(PB[1][:, 0:N], Wk[:, k, 0:128], h[:, k, :], k == 0, k == 7, ["Wk2", hn], ["pb1"])
                            head_norm_rope(PB[1][:, 0:N], "pb1", 128, N, ones128, 128, gm[:, 34:35], None, None, None, None, None,
                                           ckvnT[:, t0:t0 + N], "ckvnT", 2, W, "cw")
                            for k in range(8):
                                mm(PB[3][:, 0:N], Wk[:, k, 128:256], h[:, k, :], k == 0, k == 7, ["Wk2", hn], ["pb3"])
                            act(kpeX[0:32, t0:t0 + N], PB[3][0:32, 0:N], AF.Square, ["pb3"], ["kpeX"])
                            R = slice(64, 96)
                            ts("dve", W["xn"][R, 0:N], PB[3][R, 0:N], gm[R, 36:37], ALU.mult, ["pb3", "gm"], ["cwxn"])
                            op("pool", lambda e: e.memset(W["xn"][0:64, 0:N], 0.0), (), ["cwxn"])
                            mm(PB[4][0:96, 0:N], P_mla[0:96, 0:96], W["xn"][0:96, 0:N], True, True, ["cbf", "cwxn"], ["pb4"])
                            tt("pool", W["t1"][R, 0:N], W["xn"][R, 0:N], Ct[R, :], ALU.mult, ["cwxn", Ctn], ["cwt1"])
                            tt("dve", W["t2"][R, 0:N], PB[4][R, 0:N], St[R, :], ALU.mult, ["pb4", Stn], ["cwt2"])
                            tt("dve", kpeX[R, t0:t0 + N], W["t1"][R, 0:N], W["t2"][R, 0:N], ALU.add, ["cwt1", "cwt2"], ["kpeX"])
                        chunk_pipeline(es, NCH_ALL, xT_all, pos_all, 0, invf_mla, 96, 0, "c", a2_stage2)
                    with scope() as es:
                        Wc = sb(es, [128, 8, 256], BF16, "Wc")
                        with scope() as e2:
                            load_w(e2, Wc, w_in, D, 256, O_CQ, 0, "Wc")
                        W = work_tiles(es, N, "dw")
                        cqr = sb(es, [128, 2, N], F32, "cqr")
                        cqs = sb(es, [128, 2, N], BF16, "cqs")

                        def l_stage2(c, h, hn, Cf, Cfn, Sf, Sfn):
                            cp("pool", tabC[0:96, c * N:(c + 1) * N], Cf[0:96, :], [Cfn], ["tabC"])
                            cp("pool", tabS[0:96, c * N:(c + 1) * N], Sf[0:96, :], [Sfn], ["tabS"])
                            for r2 in range(2):
                                for k in range(8):
                                    mm(PB[1][:, 0:N], Wc[:, k, r2 * 128:(r2 + 1) * 128], h[:, k, :], k == 0, k == 7, ["Wc", hn], ["pb1"])
                                cp("act", cqr[:, r2, :], PB[1][:, 0:N], ["pb1"], ["cqr"])
                            act(cqs[:, :, :], cqr[:, :, :], AF.Square, ["cqr"], ["cqs"])
                            for r2 in range(2):
                                mm(PB[2][:, 0:N], ones128, cqs[:, r2, :], r2 == 0, r2 == 1, ["cbf", "cqs"], ["pb2"])
                            rstd_from_ps(PB[2], "pb2", 128, N, 256.0, W["rs"], "dwrs", W["tmp"], "dwtmp")
                            for r2 in range(2):
                                stt(cqnT[:, r2, c * N:(c + 1) * N], cqr[:, r2, :], gm[:, 32 + r2:33 + r2], W["rs"][:, 0:N], ALU.mult, ALU.mult,
                                    ["cqr", "gm", "dwrs"], ["cqnT"])
                        chunk_pipeline(es, NCH_H, xT_own, pos_own, tok0, invf_mla, 96, 0, "l", l_stage2)
                    with scope() as es:
                        wuq = sb(es, [128, 2, 768], BF16, "wuq")
                        wukv = sb(es, [128, 1, 1024], BF16, "wukv")
                        with scope() as e2:
                            load_w(e2, wuq, w_uq, 256, 768, 0, 0, "wuq")
                            load_w(e2, wukv, w_ukv, 128, 1024, 0, 0, "wukv")
                        KhT = sb(es, [128, S], BF16, "KhT")
                        Vh = sb(es, [128, NKB, 65], BF16, "Vh")
                        QhT = sb(es, [128, T_H], BF16, "QhT")
                        W = work_tiles(es, N, "ew")
                        W["osb"] = sb(es, [128, 512], F32, "ewosb")
                        W["rinv"] = sb(es, [128, 512], F32, "ewrinv")
                        Eb = [sb(es, [128, N], BF16, "mEb%d" % i) for i in range(3)]
                        op("pool", lambda e: e.memset(Vh[:, :, 64:65], 1.0), (), ["Vh"])
                        for hd in range(8):
                            for c in range(NCH_ALL):
                                t0 = c * N
                                mm(PB[0][0:64, 0:N], wukv[:, 0, hd * 128:hd * 128 + 64], ckvnT[:, t0:t0 + N], True, True, ["wukv", "ckvnT"], ["pb0"])
                                act(W["sq"][0:64, 0:N], PB[0][0:64, 0:N], AF.Square, ["pb0"], ["ewsq"])
                                mm(PB[1][0:96, 0:N], O96[0:64, 0:96], W["sq"][0:64, 0:N], True, False, ["cbf", "ewsq"], ["pb1"])
                                mm(PB[1][0:96, 0:N], O96[0:32, 0:96], kpeX[0:32, t0:t0 + N], False, True, ["cbf", "kpeX"], ["pb1"])
                                rstd_from_ps(PB[1], "pb1", 96, N, 96.0, W["rs"], "ewrs", W["tmp"], "ewtmp")
                                stt(KhT[0:64, t0:t0 + N], PB[0][0:64, 0:N], gm[0:64, 36:37], W["rs"][0:64, 0:N], ALU.mult, ALU.mult,
                                    ["pb0", "gm", "ewrs"], ["KhT"])
                                tt("pool", KhT[64:96, t0:t0 + N], kpeX[64:96, t0:t0 + N], W["rs"][64:96, 0:N], ALU.mult, ["kpeX", "ewrs"], ["KhT"])
                            for kb0 in range(0, NKB, 8):
                                for j in range(8):
                                    kb = kb0 + j
                                    mm(PB[2][:, j * 64:(j + 1) * 64], ckvnT[:, kb * 128:(kb + 1) * 128], wukv[:, 0, hd * 128 + 64:hd * 128 + 128],
                                       True, True, ["ckvnT", "wukv"], ["pb2"])
                                cp("act", Vh[:, kb0:kb0 + 8, 0:64], PB[2][:, 0:512].rearrange("p (a d) -> p a d", a=8), ["pb2"], ["Vh"])
                            for c in range(NCH_H):
                                for r2 in range(2):
                                    mm(PB[0][0:96, 0:N], wuq[:, r2, hd * 96:(hd + 1) * 96], cqnT[:, r2, c * N:(c + 1) * N], r2 == 0, r2 == 1,
                                       ["wuq", "cqnT"], ["pb0"])
                                head_norm_rope(PB[0][0:96, 0:N], "pb0", 96, N, O96, 96, gs[0:96, 0:1], tabC[0:96, c * N:(c + 1) * N], "tabC",
                                               tabS[0:96, c * N:(c + 1) * N], "tabS", P_mla, QhT[0:96, c * N:(c + 1) * N], "QhT", 1, W, "ew")
                            for c in range(NCH_H):
                                j = (tok0 // N) + c
                                ext = 2 * NB * (j + 1)
                                bo = 3 + (c % 2)
                                def emit_S(kb, c=c, j=j):
                                    bs_ = 5 + (kb % 2)
                                    dg = kb >= 2 * NB * j
                                    mm(PB[bs_][:, 0:N], KhT[0:96, kb * 128:(kb + 1) * 128], QhT[0:96, c * N:(c + 1) * N], True, not dg, ["KhT", "QhT"], [PBN[bs_]])
                                    if dg:
                                        mm(PB[bs_][:, 0:N], ident, mT[:, kb - 2 * NB * j, :], False, True, ["cbf", "mT"], [PBN[bs_]])

                                def emit_rest(kb, ext=ext, bo=bo):
                                    bs_ = 5 + (kb % 2)
                                    e_ = Eb[kb % 3]
                                    en = "mEb%d" % (kb % 3)
                                    act(e_[:, :], PB[bs_][:, 0:N], AF.Exp, [PBN[bs_]], [en])
                                    mm(PB[bo][0:65, 0:N], Vh[:, kb, :], e_[:, :], kb == 0, kb == ext - 1, ["Vh", en], [PBN[bo]])
                                emit_S(0)
                                for kb in range(ext):
                                    if kb + 1 < ext:
                                        emit_S(kb + 1)
                                    emit_rest(kb)

                                def writer(osb, rinv, hd=hd, c=c):
                                    tt("dve", mixM[(hd % 2) * 64:(hd % 2) * 64 + 64, hd // 2, c * N:(c + 1) * N], osb[0:64, 0:N], rinv[0:64, 0:N],
                                       ALU.mult, ["ewosb", "ewrinv"], ["mixM"])
                                attn_epilogue(PB[bo], PBN[bo], 2, N, W, "ew", writer)

                if dbg:
                    with scope() as es:
                        mf = sb(es, [128, 8, T_H], F32, "mf")
                        cp("dve", mf[:, 0:4, :], mixM[:], ["mixM"], ["mf"])
                        cp("dve", mf[:, 4:8, :], mixD[:], ["mixD"], ["mf"])
                        Sc.dma(dbg_mixed[:, tok0:tok0 + T_H].rearrange("(k p) n -> p k n", p=128), mf[:], reads=["mf"], is_out=True)

                with scope() as es:
                    wo_ = sb(es, [128, 8, D], BF16, "wo_")
                    wq_ = sb(es, [128, 8, 512], BF16, "wq_")
                    wmo = sb(es, [128, 4, D], BF16, "wmo")
                    with scope() as e2:
                        for q4 in range(4):
                            with scope() as e3:
                                load_w(e3, wo_, w_out, D, 256, q4 * 256, q4 * 256, "wo_")
                        load_w(e2, wq_, m_wq, D, 512, 0, 0, "wq_")
                    with scope() as e2:
                        load_w(e2, wmo, m_wo, 512, D, 0, 0, "wmo")
                    x1 = sb(es, [128, 8, N], F32, "x1")
                    h = sb(es, [128, 8, N], BF16, "h4")
                    W = work_tiles(es, N, "fw")
                    om = sb(es, [128, 4, N], BF16, "om")
                    Eb = [sb(es, [128, N], BF16, "cEb%d" % i) for i in range(2)]
                    actT = sb(es, [128, DFF // 128, N], BF16, "actT")
                    stg = [sb(es, [128, 4096], F32, "fstg%d" % i) for i in range(2)]
                    wsl = [sb(es, [128, 4096], BF16, "fws%d" % i) for i in range(3)]
                    sg = sb(es, [128, N], F32, "sg")
                    nld = [0]

                    def load_slab(src_ap, a_, b_):
                        i = nld[0]
                        nld[0] += 1
                        sn = "fstg%d" % (i % 2)
                        wn_ = "fws%d" % (i % 3)
                        s_ = stg[i % 2][:, 0:a_ * b_].rearrange("p (a b) -> p a b", a=a_)
                        w_ = wsl[i % 3][:, 0:a_ * b_].rearrange("p (a b) -> p a b", a=a_)
                        Sc.dma(s_, src_ap, writes=[sn])
                        cp("pool" if i % 2 == 0 else "dve", w_, s_, [sn], [wn_])
                        return w_, wn_

                    sq = actT[:, 0:8, :]
                    for c in range(NCH_H):
                        lt0 = tok0 + c * N
                        Sc.dma(x1[:], xT_own[:, lt0:lt0 + N].rearrange("(k p) n -> p k n", p=128), writes=["x1"])
                        for m in range(8):
                            bk = m % 2
                            for k in range(8):
                                mm(PB[bk][:, 0:N], wo_[:, k, m * 128:(m + 1) * 128], (mixM if k < 4 else mixD)[:, k % 4, c * N:(c + 1) * N], k == 0, k == 7, ["wo_", "mixM", "mixD"], [PBN[bk]])
                            tt("dve", x1[:, m, :], x1[:, m, :], PB[bk][:, 0:N], ALU.add, ["x1", PBN[bk]], ["x1"])
                        norm_chunk(x1, "x1", N, 8, h, "h4", sq, "actT", 2, W["tmp"], "fwtmp", W["rs"], "fwrs")
                        for hh in range(4):
                            for k in range(8):
                                mm(PB[3][:, 0:N], wq_[:, k, hh * 128:(hh + 1) * 128], h[:, k, :], k == 0, k == 7, ["wq_", "h4"], ["pb3"])
                            head_norm_rope(PB[3][:, 0:N], "pb3", 128, N, ones128, 128, gs[:, 2:3], None, None, None, None, None,
                                           W["xn"][:, 0:N], "fwxn2", 4, W, "fw")
                            for blk in range(2):
                                mm(PB[5][:, 0:N], KmT[:, hh, blk * 128:(blk + 1) * 128], W["xn"][:, 0:N], True, True, ["KmT", "fwxn2"], ["pb5"])
                                act(Eb[blk][:, :], PB[5][:, 0:N], AF.Exp, ["pb5"], ["cEb%d" % blk])
                            for blk in range(2):
                                mm(PB[6][:, 0:N], Vm[:, blk, hh * 128:(hh + 1) * 128], Eb[blk][:, :], blk == 0, blk == 1, ["Vm", "cEb%d" % blk], ["pb6"])
                            for blk in range(2):
                                mm(PB[4][:, 0:N], ones128, Eb[blk][:, :], blk == 0, blk == 1, ["cbf", "cEb%d" % blk], ["pb4"])
                            op("dve", lambda e: e.reciprocal(out=W["t1"][:, 0:N], in_=PB[4][:, 0:N]), ["pb4"], ["fwt1"])
                            tt("dve", om[:, hh, :], PB[6][:, 0:N], W["t1"][:, 0:N], ALU.mult, ["pb6", "fwt1"], ["om"])
                        for m in range(8):
                            bk = m % 2
                            for hh in range(4):
                                mm(PB[bk][:, 0:N], wmo[:, hh, m * 128:(m + 1) * 128], om[:, hh, :], hh == 0, hh == 3, ["wmo", "om"], [PBN[bk]])
                            tt("dve", x1[:, m, :], x1[:, m, :], PB[bk][:, 0:N], ALU.add, ["x1", PBN[bk]], ["x1"])
                        norm_chunk(x1, "x1", N, 24, h, "h4", sq, "actT", 2, W["tmp"], "fwtmp", W["rs"], "fwrs")
                        slabs = [(c0, min(512, DFF - c0)) for c0 in range(0, DFF, 512)]
                        for (c0, cw) in slabs:
                            wg, wgn = load_slab(f_wg[:, c0:c0 + cw].rearrange("(k p) c -> p k c", p=128), 8, cw)
                            wu, wun = load_slab(f_wu[:, c0:c0 + cw].rearrange("(k p) c -> p k c", p=128), 8, cw)
                            for f in range(cw // 128):
                                ff = c0 // 128 + f
                                bg = 3 + (ff % 2)
                                bu = 5 + (ff % 2)
                                for k in range(8):
                                    mm(PB[bg][:, 0:N], wg[:, k, f * 128:(f + 1) * 128], h[:, k, :], k == 0, k == 7, [wgn, "h4"], [PBN[bg]])
                                for k in range(8):
                                    mm(PB[bu][:, 0:N], wu[:, k, f * 128:(f + 1) * 128], h[:, k, :], k == 0, k == 7, [wun, "h4"], [PBN[bu]])
                                act(sg[:, :], PB[bg][:, 0:N], AF.Silu, [PBN[bg]], ["sg"])
                                tt("dve", actT[:, ff, :], sg[:, :], PB[bu][:, 0:N], ALU.mult, ["sg", PBN[bu]], ["actT"])
                        for grp in ((0, 1, 2, 3), (4, 5, 6, 7)):
                            rslabs = [(r0, min(512, DFF - r0)) for r0 in range(0, DFF, 512)]
                            for (r0, rw) in rslabs:
                                nf = rw // 128
                                wd, wdn = load_slab(f_wd[r0:r0 + rw, grp[0] * 128:grp[0] * 128 + 512].rearrange("(f p) c -> p f c", p=128), nf, 512)
                                for f in range(nf):
                                    ff = r0 // 128 + f
                                    for mi, m in enumerate(grp):
                                        mm(PB[mi][:, 0:N], wd[:, f, mi * 128:(mi + 1) * 128], actT[:, ff, :], ff == 0, ff == DFF // 128 - 1, [wdn, "actT"], [PBN[mi]])
                            for mi, m in enumerate(grp):
                                tt("dve", x1[:, m, :], x1[:, m, :], PB[mi][:, 0:N], ALU.add, ["x1", PBN[mi]], ["x1"])
                        Sc.dma(outT[:, lt0:lt0 + N].rearrange("(k p) n -> p k n", p=128), x1[:], reads=["x1"], is_out=True)
        Sc.finish()
    return nc, Sc


def _consts(S, NH, half):
    T_OWN = S // 2
    T_H = T_OWN // NH
    N = min(512, T_H)
    NB = N // 128
    cb = np.zeros((128, 6 * 128), np.float32)
    cb[:, 0:128] = 1.0
    cb[0:64, 128:192] = 1.0
    cb[64:128, 192:256] = 1.0
    cb[0:96, 256:352] = 1.0
    Pp = np.zeros((128, 128), np.float32)
    for base in (0, 64):
        for i in range(8):
            Pp[base + 8 + i, base + i] = -1.0
            Pp[base + i, base + 8 + i] = 1.0
    cb[:, 384:512] = Pp
    Pm = np.zeros((128, 128), np.float32)
    for i in range(16):
        Pm[80 + i, 64 + i] = -1.0
        Pm[64 + i, 80 + i] = 1.0
    cb[:, 512:640] = Pm
    cb[:, 640:768] = np.eye(128, dtype=np.float32)
    cf = np.zeros((128, 66), np.float32)
    cf[64, 0:64] = 1.0
    f8 = (THETA ** (-np.arange(0, 16, 2, dtype=np.float32) / np.float32(16))).astype(np.float32)
    f16 = (THETA ** (-np.arange(0, 32, 2, dtype=np.float32) / np.float32(32))).astype(np.float32)
    for base in (0, 64):
        cf[base:base + 8, 64] = f8
        cf[base + 8:base + 16, 64] = f8
    cf[64:80, 65] = f16
    cf[80:96, 65] = f16
    mI = np.zeros((128, 256), np.float32)
    q = np.arange(128)[:, None]
    k = np.arange(256)[None, :]
    qpos = half * 128 + q
    mI[:] = np.where(k <= qpos, 0.0, NEG)
    mT = np.zeros((128, 2 * NB, N), np.float32)
    p = np.arange(128)[:, None]
    for kbp in range(2 * NB):
        for m in range(NB):
            r = np.arange(128)[None, :]
            gb = 2 * m + half
            if kbp < gb:
                v = np.zeros((128, 128), np.float32)
            elif kbp == gb:
                v = np.where(p <= r, 0.0, -30000.0).astype(np.float32)
            else:
                v = np.full((128, 128), -30000.0, np.float32)
            mT[:, kbp, m * 128:(m + 1) * 128] = v
    return cb, cf, mI, mT.reshape(128, 2 * NB * N)


def _gains(inp):
    gm = np.zeros((128, 42), np.float32)

    def col8(v, c0):
        gm[:, c0:c0 + 8] = np.asarray(v, np.float32).reshape(8, 128).T
    col8(inp["norm_mix"][0], 0)
    col8(inp["norm_mem_x"][0], 8)
    col8(inp["norm_mem_kv"][0], 16)
    col8(inp["norm_ffn"][0], 24)
    gm[:, 32:34] = np.asarray(inp["mla_q_a_norm"][0], np.float32).reshape(2, 128).T
    gm[:, 34] = inp["mla_kv_a_norm"][0]
    gm[0:96, 35] = inp["mla_q_norm"][0]
    gm[0:96, 36] = inp["mla_k_norm"][0]
    gm[:, 37] = np.tile(np.asarray(inp["dsa_q_norm"][0], np.float32), 2)
    gm[:, 38] = np.tile(np.asarray(inp["dsa_k_norm"][0], np.float32), 2)
    gm[:, 39] = np.tile(np.asarray(inp["idx_k_norm"][0], np.float32), 2)
    gm[:, 40] = inp["mem_q_norm"][0]
    gm[:, 41] = inp["mem_k_norm"][0]
    return gm


_CACHE = {}


def run(inputs, S, NH, dbg=False):
    inp = {k: np.asarray(v) for k, v in inputs.items()}
    B = inp["x"].shape[0]
    ncores = 2 * B
    key = (S, NH, dbg)
    if key not in _CACHE:
        _CACHE[key] = build(S, NH, dbg)
    nc, Sc = _CACHE[key]
    gm = _gains(inp)
    nblk = S // 128
    in_maps = []
    for c in range(ncores):
        b, half = c // 2, c % 2
        own_blocks = np.arange(half, nblk, 2)
        own_idx = (own_blocks[:, None] * 128 + np.arange(128)[None, :]).reshape(-1)
        xb = np.asarray(inp["x"][b], np.float32)
        cb, cf, mI, mT = _consts(S, NH, half)
        pos = np.asarray(inp["positions"][b], np.int32)
        in_maps.append({
            "xT_all": np.ascontiguousarray(xb.T),
            "xT_own": np.ascontiguousarray(xb[own_idx].T),
            "pos_all": np.ascontiguousarray(pos.reshape(1, S)),
            "pos_own": np.ascontiguousarray(pos[own_idx].reshape(1, S // 2)),
            "memT": np.ascontiguousarray(np.asarray(inp["mem"][b], np.float32).T),
            "w_in": np.ascontiguousarray(inp["w_in"][0], dtype=np.float32),
            "w_uq": np.ascontiguousarray(inp["mla_w_uq"][0], dtype=np.float32),
            "w_ukv": np.ascontiguousarray(inp["mla_w_ukv"][0], dtype=np.float32),
            "w_out": np.ascontiguousarray(inp["w_out"][0], dtype=np.float32),
            "m_wq": np.ascontiguousarray(inp["mem_w_q"][0], dtype=np.float32),
            "m_wk": np.ascontiguousarray(inp["mem_w_k"][0], dtype=np.float32),
            "m_wv": np.ascontiguousarray(inp["mem_w_v"][0], dtype=np.float32),
            "m_wo": np.ascontiguousarray(inp["mem_w_o"][0], dtype=np.float32),
            "f_wg": np.ascontiguousarray(inp["ffn_w_gate"][0], dtype=np.float32),
            "f_wu": np.ascontiguousarray(inp["ffn_w_up"][0], dtype=np.float32),
            "f_wd": np.ascontiguousarray(inp["ffn_w_down"][0], dtype=np.float32),
            "gm": gm, "cb": cb, "cf": cf, "mI": mI, "mT": mT,
        })
    res = run_bass_kernel_spmd(nc, in_maps, core_ids=list(range(ncores)))
    out = np.zeros((B, S, D), np.float32)
    extra = {}
    for c in range(ncores):
        b, half = c // 2, c % 2
        own_blocks = np.arange(half, nblk, 2)
        own_idx = (own_blocks[:, None] * 128 + np.arange(128)[None, :]).reshape(-1)
        out[b, own_idx, :] = np.asarray(res.results[c]["outT"]).T
        if dbg:
            extra[c] = (own_idx, np.asarray(res.results[c]["dbg_mixed"]).T)
    return out, extra


def kernel(**inputs):
    out, _ = run(inputs, 8192, 2)
    return out
```

```python
import math
from contextlib import ExitStack

import numpy as np
import concourse.bass as bass
import concourse.mybir as mybir
from concourse.bass_utils import run_bass_kernel_spmd

F32 = mybir.dt.float32
BF16 = mybir.dt.bfloat16
I32 = mybir.dt.int32
AF = mybir.ActivationFunctionType
ALU = mybir.AluOpType
AX = mybir.AxisListType

D = 1024
NMEM = 256
DFF = 2816
EPS = 1e-6
TOPK = 256
BISECT_ITERS = 20
NEG = -1.0e30
O_CQ, O_CKV, O_KPE, O_QD, O_KD, O_VD, O_QI, O_KI, O_WI = 0, 256, 384, 416, 928, 1056, 1184, 1696, 1760
THETA = 500000.0


class Sched:
    def __init__(self, nc):
        self.nc = nc
        self.engs = {"pe": nc.tensor, "act": nc.scalar, "dve": nc.vector, "pool": nc.gpsimd, "sp": nc.sync}
        self.sems = {}
        self.cnt = {}
        self.seen = {k: {} for k in self.engs}
        self.lastw = {}
        self.readers = {}
        self._stack = []
        self.ninst = 0
        self.nwait = 0
        for k in ["pe", "act", "dve", "pool"]:
            self.sems[k] = self.sem("s_" + k)
            self.cnt[k] = 0
        self.dsems = [[self.sem("d%d" % i), 0] for i in range(24)]
        self.dnext = 0
        self.out_events = []

    def sem(self, name):
        cm = self.nc.semaphore(name)
        s = cm.__enter__()
        self._stack.append(cm)
        return s

    def _wait(self, e, ev):
        if ev is None:
            return
        s, v = ev
        if e == "pe" and s is self.sems["pe"]:
            return
        key = id(s)
        if self.seen[e].get(key, 0) >= v:
            return
        self.engs[e].wait_ge(s, v)
        self.seen[e][key] = v
        self.nwait += 1

    def deps(self, e, reads, writes):
        for r in reads:
            self._wait(e, self.lastw.get(r))
        for w in writes:
            self._wait(e, self.lastw.get(w))
            for ev in self.readers.get(w, {}).values():
                self._wait(e, ev)

    def record(self, ev, reads, writes):
        for r in reads:
            d = self.readers.setdefault(r, {})
            d[id(ev[0])] = ev
        for w in writes:
            self.lastw[w] = ev
            self.readers[w] = {}

    def op(self, e, fn, reads=(), writes=()):
        self.deps(e, reads, writes)
        inst = fn(self.engs[e])
        self.cnt[e] += 1
        inst.then_inc(self.sems[e], 1)
        ev = (self.sems[e], self.cnt[e])
        self.record(ev, reads, writes)
        self.ninst += 1
        return ev

    def dma(self, out, in_, reads=(), writes=(), is_out=False):
        q = "sp"
        ds = self.dsems[self.dnext % len(self.dsems)]
        self.dnext += 1
        if ds[1] > 0:
            self._wait(q, (ds[0], ds[1]))
        self.deps(q, reads, writes)
        inst = self.engs[q].dma_start(out=out, in_=in_)
        ds[1] += 16
        inst.then_inc(ds[0], 16)
        ev = (ds[0], ds[1])
        self.record(ev, reads, writes)
        self.ninst += 1
        if is_out:
            self.out_events.append(ev)
        return ev

    def barrier(self):
        evs = [(self.sems[k], self.cnt[k]) for k in ["pe", "act", "dve", "pool"] if self.cnt[k] > 0]
        evs += [(d[0], d[1]) for d in self.dsems if d[1] > 0]
        for e in ["pe", "act", "dve", "pool", "sp"]:
            for ev in evs:
                self._wait(e, ev)

    def finish(self):
        for ev in self.out_events:
            self._wait("sp", ev)
        for d in self.dsems:
            if d[1] > 0:
                self._wait("sp", (d[0], d[1]))
        for k in ["pe", "act", "dve", "pool"]:
            if self.cnt[k] > 0:
                self._wait("sp", (self.sems[k], self.cnt[k]))
        for cm in reversed(self._stack):
            cm.__exit__(None, None, None)


def build(S, NH, dbg=False):
    T_OWN = S // 2
    T_H = T_OWN // NH
    N = min(512, T_H)
    NB = N // 128
    NCH_ALL = S // N
    NCH_H = T_H // N
    NKB = S // 128
    nc = bass.Bass("TRN2", target_bir_lowering=False)

    def dram(name, shape, dt=F32, kind="ExternalInput"):
        return nc.dram_tensor(name, shape, dt, kind=kind).ap()

    xT_all = dram("xT_all", [D, S])
    xT_own = dram("xT_own", [D, T_OWN])
    pos_all = dram("pos_all", [1, S], I32)
    pos_own = dram("pos_own", [1, T_OWN], I32)
    memT = dram("memT", [D, NMEM])
    w_in = dram("w_in", [D, 1768])
    w_uq = dram("w_uq", [256, 768])
    w_ukv = dram("w_ukv", [128, 1024])
    w_out = dram("w_out", [D, D])
    m_wq = dram("m_wq", [D, 512])
    m_wk = dram("m_wk", [D, 512])
    m_wv = dram("m_wv", [D, 512])
    m_wo = dram("m_wo", [512, D])
    f_wg = dram("f_wg", [D, DFF])
    f_wu = dram("f_wu", [D, DFF])
    f_wd = dram("f_wd", [DFF, D])
    gm_d = dram("gm", [128, 42])
    cb_d = dram("cb", [128, 6 * 128])
    cf_d = dram("cf", [128, 66])
    mI_d = dram("mI", [128, 256])
    mT_d = dram("mT", [128, 2 * NB * N])
    outT = dram("outT", [D, T_OWN], kind="ExternalOutput")
    if dbg:
        dbg_mixed = dram("dbg_mixed", [D, T_OWN], kind="ExternalOutput")

    Sc = Sched(nc)
    uid = [0]

    def nm(p):
        uid[0] += 1
        return "%s_%d" % (p, uid[0])

    def sb(es, shape, dt, name="t"):
        return es.enter_context(nc.sbuf_tensor(nm(name), shape, dt))

    def ps(es, shape, dt, name="p"):
        return es.enter_context(nc.psum_tensor(nm(name), shape, dt))

    op = Sc.op

    from contextlib import contextmanager

    @contextmanager
    def scope():
        with ExitStack() as es_:
            yield es_
        Sc.barrier()

    def mm(out, lhsT, rhs, start, stop, reads, writes):
        return op("pe", lambda e: e.matmul(out, lhsT=lhsT, rhs=rhs, start=start, stop=stop), reads, writes)

    def act(out, in_, func, reads, writes, scale=None, bias=None):
        kw = {}
        if scale is not None:
            kw["scale"] = scale
        if bias is not None:
            kw["bias"] = bias
        return op("act", lambda e: e.activation(out=out, in_=in_, func=func, **kw), reads, writes)

    def ts(eng, out, in0, s1, op0, reads, writes, s2=None, op1=None, accum=None):
        kw = {}
        if op1 is not None:
            kw["op1"] = op1
        if accum is not None:
            kw["accum_out"] = accum
        return op(eng, lambda e: e.tensor_scalar(out=out, in0=in0, scalar1=s1, scalar2=s2, op0=op0, **kw), reads, writes)

    def tt(eng, out, in0, in1, o, reads, writes):
        return op(eng, lambda e: e.tensor_tensor(out=out, in0=in0, in1=in1, op=o), reads, writes)

    def stt(out, in0, scalar, in1, op0, op1, reads, writes):
        return op("dve", lambda e: e.scalar_tensor_tensor(out=out, in0=in0, scalar=scalar, in1=in1, op0=op0, op1=op1), reads, writes)

    def cp(eng, out, in_, reads, writes):
        if eng == "act":
            return act(out, in_, AF.Copy, reads, writes)
        return op(eng, lambda e: e.tensor_copy(out=out, in_=in_), reads, writes)

    with ExitStack() as top:
        gm = sb(top, [128, 42], F32, "gm")
        cf = sb(top, [128, 66], F32, "cf")
        cbf = sb(top, [128, 6 * 128], BF16, "cbf")
        gs = sb(top, [128, 4], F32, "gs")
        epsc = sb(top, [128, 1], F32, "epsc")
        shc = sb(top, [128, 2], F32, "shc")
        mI = sb(top, [128, 256], F32, "mI")
        mixD = sb(top, [128, 4, T_H], BF16, "mixD")
        with scope() as es:
            st = sb(es, [128, 6 * 128], F32, "cst")
            Sc.dma(st[:], cb_d[:, :], writes=["cst"])
            cp("dve", cbf[:], st[:], ["cst"], ["cbf"])
        Sc.dma(gm[:], gm_d[:, :], writes=["gm"])
        Sc.dma(cf[:], cf_d[:, :], writes=["cf"])
        Sc.dma(mI[:], mI_d[:, :], writes=["mI"])
        op("dve", lambda e: e.memset(epsc[:], EPS), (), ["epsc"])
        op("dve", lambda e: e.memset(shc[:, 0:1], 0.0), (), ["shc"])
        op("dve", lambda e: e.memset(shc[:, 1:2], math.pi / 2), (), ["shc"])
        ts("dve", gs[:, 0:1], gm[:, 35:36], 96 ** -0.5, ALU.mult, ["gm"], ["gs"])
        ts("dve", gs[:, 1:2], gm[:, 37:38], 64 ** -0.5, ALU.mult, ["gm"], ["gs"])
        ts("dve", gs[:, 2:3], gm[:, 40:41], 128 ** -0.5, ALU.mult, ["gm"], ["gs"])
        ones128 = cbf[:, 0:128]
        B64 = cbf[:, 128:256]
        O96 = cbf[:, 256:384]
        P_part = cbf[:, 384:512]
        P_mla = cbf[:, 512:640]
        ident = cbf[:, 640:768]
        sel = cf[:, 0:64]
        invf_part = cf[:, 64:65]
        invf_mla = cf[:, 65:66]
        CONST = ["gm", "cf", "cbf", "gs", "epsc"]

        PB = [ps(top, [128, 512], F32, "pb%d" % i) for i in range(7)]
        PT = ps(top, [128, 1024], BF16, "pt")
        PBN = ["pb%d" % i for i in range(7)]

        def load_w(es, dst, src, rows, cols, c0, dcol0, name):
            kc = rows // 128
            stg = sb(es, [128, kc, cols], F32, "wst")
            r = nm("wst")
            Sc.dma(stg[:], src[:, c0:c0 + cols].rearrange("(k p) c -> p k c", p=128), writes=[r])
            cp("pool", dst[:, :, dcol0:dcol0 + cols], stg[:], [r], [name])

        def rstd_from_ps(pss, psname, rows, n, dim, out, outname, tmp, tmpname):
            act(tmp[0:rows, 0:n], pss[0:rows, 0:n], AF.Ln, [psname, "epsc"], [tmpname], scale=1.0 / dim, bias=epsc[0:rows, :])
            act(out[0:rows, 0:n], tmp[0:rows, 0:n], AF.Exp, [tmpname], [outname], scale=-0.5)

        def norm_chunk(xs, xsname, n, gcol0, h, hname, sq, sqname, bank, tmp, tmpname, rs, rsname):
            act(sq[:, :, 0:n], xs[:, :, 0:n], AF.Square, [xsname], [sqname])
            for k in range(8):
                mm(PB[bank][:, 0:n], ones128, sq[:, k, 0:n], k == 0, k == 7, ["cbf", sqname], [PBN[bank]])
            rstd_from_ps(PB[bank], PBN[bank], 128, n, float(D), rs, rsname, tmp, tmpname)
            for k in range(8):
                stt(h[:, k, 0:n], xs[:, k, 0:n], gm[:, gcol0 + k:gcol0 + k + 1], rs[:, 0:n], ALU.mult, ALU.mult,
                    [xsname, "gm", rsname], [hname])

        def rope_tmp(es, n):
            return {"pi": sb(es, [128, n], I32, "rp_pi"), "pf": sb(es, [128, n], F32, "rp_pf"), "a": sb(es, [128, n], F32, "rp_a"),
                    "u": sb(es, [128, n], F32, "rp_u"), "ki": sb(es, [128, n], I32, "rp_ki"), "r": sb(es, [128, n], F32, "rp_r"),
                    "t": sb(es, [128, n], F32, "rp_t"), "k": nm("rp")}

        def rope_tables(RT, pos_d, t0, n, invf, rows, Ct, Ctn, St, Stn, cview=None, sview=None):
            pi, pf, a, u, ki, r, t, k = RT["pi"], RT["pf"], RT["a"], RT["u"], RT["ki"], RT["r"], RT["t"], RT["k"]
            R = slice(0, rows)
            Sc.dma(pi[:], pos_d[0:1, t0:t0 + n].to_broadcast([128, n]), writes=[k + "pi"])
            cp("dve", pf[R, :], pi[R, :], [k + "pi"], [k + "pf"])
            ts("dve", pf[R, :], pf[R, :], invf[R, :], ALU.mult, [k + "pf", "cf"], [k + "pf"])
            for shift, dst, dstn, view in ((0.0, St, Stn, sview), (math.pi / 2, Ct, Ctn, cview)):
                ts("dve", a[R, :], pf[R, :], shift, ALU.add, [k + "pf"], [k + "a"])
                ts("dve", u[R, :], a[R, :], 1.0 / (2 * math.pi), ALU.mult, [k + "a"], [k + "u"])
                cp("dve", ki[R, :], u[R, :], [k + "u"], [k + "ki"])
                cp("dve", u[R, :], ki[R, :], [k + "ki"], [k + "u"])
                stt(r[R, :], u[R, :], -2 * math.pi, a[R, :], ALU.mult, ALU.add, [k + "u", k + "a"], [k + "r"])
                ts("dve", t[R, :], r[R, :], math.pi, ALU.is_gt, [k + "r"], [k + "t"], s2=-2 * math.pi, op1=ALU.mult)
                tt("dve", r[R, :], r[R, :], t[R, :], ALU.add, [k + "r", k + "t"], [k + "r"])
                ts("dve", t[R, :], r[R, :], -math.pi, ALU.is_lt, [k + "r"], [k + "t"], s2=2 * math.pi, op1=ALU.mult)
                tt("dve", r[R, :], r[R, :], t[R, :], ALU.add, [k + "r", k + "t"], [k + "r"])
                ts("dve", r[R, :], r[R, :], math.pi, ALU.min, [k + "r"], [k + "r"], s2=-math.pi, op1=ALU.max)
                act(dst[R, :] if view is None else view, r[R, :], AF.Sin, [k + "r"], [dstn])

        def head_norm_rope(src, srcname, rows, n, blk, dim, gcol, Ct, Ctn, St, Stn, Pm, out, outname, bank, W, wn):
            R = slice(0, rows)
            if blk is not None:
                act(W["sq"][R, 0:n], src, AF.Square, [srcname], [wn + "sq"])
                mm(PB[bank][R, 0:n], blk[R, R], W["sq"][R, 0:n], True, True, ["cbf", wn + "sq"], [PBN[bank]])
                rstd_from_ps(PB[bank], PBN[bank], rows, n, float(dim), W["rs"], wn + "rs", W["tmp"], wn + "tmp")
                dst = W["xn"][R, 0:n] if Ct is not None else out
                dn = wn + "xn" if Ct is not None else outname
                stt(dst, src, gcol, W["rs"][R, 0:n], ALU.mult, ALU.mult, [srcname, "gm", "gs", wn + "rs"], [dn])
            else:
                cp("act", W["xn"][R, 0:n], src, [srcname], [wn + "xn"])
            if Ct is not None:
                mm(PB[bank][R, 0:n], Pm[R, R], W["xn"][R, 0:n], True, True, ["cbf", wn + "xn"], [PBN[bank]])
                tt("pool", W["t1"][R, 0:n], W["xn"][R, 0:n], Ct, ALU.mult, [wn + "xn", Ctn], [wn + "t1"])
                tt("dve", W["t2"][R, 0:n], PB[bank][R, 0:n], St, ALU.mult, [PBN[bank], Stn], [wn + "t2"])
                i0, i1 = W["t1"][R, 0:n], W["t2"][R, 0:n]
                if isinstance(out, tuple):
                    tt("dve", out[0], W["t1"][0:64, 0:n], W["t2"][0:64, 0:n], ALU.add, [wn + "t1", wn + "t2"], [outname])
                    tt("dve", out[1], W["t1"][64:128, 0:n], W["t2"][64:128, 0:n], ALU.add, [wn + "t1", wn + "t2"], [outname])
                    return
                if len(out.shape) == 3:
                    i0 = i0.rearrange("p (b q) -> p b q", q=128)
                    i1 = i1.rearrange("p (b q) -> p b q", q=128)
                tt("dve", out, i0, i1, ALU.add, [wn + "t1", wn + "t2"], [outname])

        def chunk_pipeline(es, nch, src_d, pos_d, tokoff, invf, rows, gcol0, pref, stage2, want_rope=True):
            xs = sb(es, [128, 8, N], F32, pref + "xs")
            hh_ = [sb(es, [128, 8, N], BF16, pref + "h%d" % i) for i in range(2)]
            sq = sb(es, [128, 8, N], BF16, pref + "sq")
            rs1 = sb(es, [128, N], F32, pref + "rs1")
            tmp1 = sb(es, [128, N], F32, pref + "tmp1")
            if want_rope:
                Cts = [sb(es, [128, N], F32, pref + "C%d" % i) for i in range(2)]
                Sts = [sb(es, [128, N], F32, pref + "S%d" % i) for i in range(2)]
                RT = rope_tmp(es, N)

            def s1a(c):
                i = c % 2
                t0 = tokoff + c * N
                Sc.dma(xs[:], src_d[:, t0:t0 + N].rearrange("(k p) n -> p k n", p=128), writes=[pref + "xs"])
                if want_rope:
                    rope_tables(RT, pos_d, t0, N, invf, rows, Cts[i], pref + "C%d" % i, Sts[i], pref + "S%d" % i)
                act(sq[:, :, 0:N], xs[:, :, 0:N], AF.Square, [pref + "xs"], [pref + "sq"])
                for k in range(8):
                    mm(PB[0][:, 0:N], ones128, sq[:, k, 0:N], k == 0, k == 7, ["cbf", pref + "sq"], [PBN[0]])
                rstd_from_ps(PB[0], PBN[0], 128, N, float(D), rs1, pref + "rs1", tmp1, pref + "tmp1")

            def s1b(c):
                i = c % 2
                for k in range(8):
                    stt(hh_[i][:, k, 0:N], xs[:, k, 0:N], gm[:, gcol0 + k:gcol0 + k + 1], rs1[:, 0:N], ALU.mult, ALU.mult,
                        [pref + "xs", "gm", pref + "rs1"], [pref + "h%d" % i])
            s1a(0)
            s1b(0)
            for c in range(nch):
                if c + 1 < nch:
                    s1a(c + 1)
                i = c % 2
                if want_rope:
                    stage2(c, hh_[i], pref + "h%d" % i, Cts[i], pref + "C%d" % i, Sts[i], pref + "S%d" % i)
                else:
                    stage2(c, hh_[i], pref + "h%d" % i, None, None, None, None)
                if c + 1 < nch:
                    s1b(c + 1)

        def work_tiles(es, n, pref):
            W = {"sq": sb(es, [128, n], BF16, pref + "sq"), "rs": sb(es, [128, n], F32, pref + "rs"),
                 "tmp": sb(es, [128, n], F32, pref + "tmp"), "xn": sb(es, [128, n], BF16, pref + "xn"),
                 "t1": sb(es, [128, n], F32, pref + "t1"), "t2": sb(es, [128, n], F32, pref + "t2")}
            return W

        def attn_epilogue(psO, psOn, bankB, n, W, wn, writer):
            cp("act", W["osb"][0:65, 0:n], psO[0:65, 0:n], [psOn], [wn + "osb"])
            mm(PB[bankB][0:64, 0:n], sel[0:65, 0:64], W["osb"][0:65, 0:n], True, True, ["cf", wn + "osb"], [PBN[bankB]])
            op("dve", lambda e: e.reciprocal(out=W["rinv"][0:64, 0:n], in_=PB[bankB][0:64, 0:n]), [PBN[bankB]], [wn + "rinv"])
            writer(W["osb"], W["rinv"])

        KmT = sb(top, [128, 4, NMEM], BF16, "KmT")
        Vm = sb(top, [128, 2, 512], BF16, "Vm")
        with scope() as es:
            wk = sb(es, [128, 8, 512], BF16, "wk")
            wv = sb(es, [128, 8, 512], BF16, "wv")
            with scope() as e2:
                load_w(e2, wk, m_wk, D, 512, 0, 0, "wk")
                load_w(e2, wv, m_wv, D, 512, 0, 0, "wv")
            ms = sb(es, [128, 8, NMEM], F32, "ms")
            mh = sb(es, [128, 8, NMEM], BF16, "mh")
            msq = sb(es, [128, 8, NMEM], BF16, "msq")
            W = work_tiles(es, NMEM, "mw")
            Sc.dma(ms[:], memT.rearrange("(k p) n -> p k n", p=128), writes=["ms"])
            norm_chunk(ms, "ms", NMEM, 16, mh, "mh", msq, "msq", 0, W["tmp"], "mwtmp", W["rs"], "mwrs")
            for hh in range(4):
                for k in range(8):
                    mm(PB[1][:, 0:NMEM], wk[:, k, hh * 128:(hh + 1) * 128], mh[:, k, :], k == 0, k == 7, ["wk", "mh"], ["pb1"])
                head_norm_rope(PB[1][:, 0:NMEM], "pb1", 128, NMEM, ones128, 128, gm[:, 41:42], None, None, None, None, None,
                               KmT[:, hh, :], "KmT", 2, W, "mw")
            for blk in range(2):
                for k in range(8):
                    mm(PB[3][:, 0:512], mh[:, k, blk * 128:(blk + 1) * 128], wv[:, k, :], k == 0, k == 7, ["mh", "wv"], ["pb3"])
                cp("act", Vm[:, blk, :], PB[3][:, 0:512], ["pb3"], ["Vm"])

        for hf in range(NH):
            tok0 = hf * T_H
            with scope() as pd:
                NBH = T_H // 128
                qdT = sb(pd, [128, NBH, 4, 128], BF16, "qdT")
                qiT = sb(pd, [128, NBH, 4, 128], BF16, "qiT")
                wabs = sb(pd, [128, NBH, 8], F32, "wabs")
                wsgn = sb(pd, [128, NBH, 8], F32, "wsgn")
                with scope() as es:
                    Wq = sb(es, [128, 8, 1032], BF16, "Wq")
                    for c4 in range(4):
                        with scope() as e2:
                            load_w(e2, Wq, w_in, D, 64, O_QD + c4 * 64, c4 * 128, "Wq")
                            load_w(e2, Wq, w_in, D, 64, O_QD + (4 + c4) * 64, c4 * 128 + 64, "Wq")
                    for q4 in range(2):
                        with scope() as e2:
                            load_w(e2, Wq, w_in, D, 256, O_QI + q4 * 256, 512 + q4 * 256, "Wq")
                    with scope() as e2:
                        load_w(e2, Wq, w_in, D, 8, O_WI, 1024, "Wq")
                    W = work_tiles(es, N, "bw")
                    wtok = sb(es, [128, NB, 8], F32, "wtok")

                    def q_stage2(c, h, hn, Ct, Ctn, St, Stn):
                        b0 = c * NB
                        for c4 in range(4):
                            for k in range(8):
                                mm(PB[1][:, 0:N], Wq[:, k, c4 * 128:(c4 + 1) * 128], h[:, k, :], k == 0, k == 7, ["Wq", hn], ["pb1"])
                            head_norm_rope(PB[1][:, 0:N], "pb1", 128, N, B64, 64, gs[:, 1:2], Ct[:, :], Ctn, St[:, :], Stn, P_part,
                                           qdT[:, b0:b0 + NB, c4, :], "qdT", 2, W, "bw")
                        for c4 in range(4):
                            for k in range(8):
                                mm(PB[3][:, 0:N], Wq[:, k, 512 + c4 * 128:512 + (c4 + 1) * 128], h[:, k, :], k == 0, k == 7, ["Wq", hn], ["pb3"])
                            head_norm_rope(PB[3][:, 0:N], "pb3", 128, N, None, 64, None, Ct[:, :], Ctn, St[:, :], Stn, P_part,
                                           qiT[:, b0:b0 + NB, c4, :], "qiT", 4, W, "bw")
                        for b in range(NB):
                            for k in range(8):
                                mm(PB[5][:, 0:8], h[:, k, b * 128:(b + 1) * 128], Wq[:, k, 1024:1032], k == 0, k == 7, [hn, "Wq"], ["pb5"])
                            cp("act", wtok[:, b, :], PB[5][:, 0:8], ["pb5"], ["wtok"])
                        act(wabs[:, b0:b0 + NB, :], wtok[:], AF.Abs, ["wtok"], ["wabs"], scale=(8 ** -0.5) * (64 ** -0.5))
                        ts("dve", wsgn[:, b0:b0 + NB, :], wtok[:], 0.0, ALU.is_ge, ["wtok"], ["wsgn"], s2=2.0, op1=ALU.mult)
                        ts("dve", wsgn[:, b0:b0 + NB, :], wsgn[:, b0:b0 + NB, :], -1.0, ALU.add, ["wsgn"], ["wsgn"])
                    chunk_pipeline(es, NCH_H, xT_own, pos_own, tok0, invf_part, 128, 0, "q", q_stage2)
                kdT = sb(pd, [128, S], BF16, "kdT")
                kiT_lo = sb(pd, [128, S], BF16, "kiTlo")
                kiT_hi = sb(pd, [128, S], BF16, "kiThi")
                op("pool", lambda e: e.memset(kiT_lo[64:128, :], 0.0), (), ["kiT"])
                op("pool", lambda e: e.memset(kiT_hi[0:64, :], 0.0), (), ["kiT"])
                vd = sb(pd, [128, NKB, 2, 65], BF16, "vd")
                op("pool", lambda e: e.memset(vd[:, :, :, 64:65], 1.0), (), ["vd"])
                with scope() as es:
                    Wk = sb(es, [128, 8, 384], BF16, "Wk1")
                    with scope() as e2:
                        load_w(e2, Wk, w_in, D, 128, O_KD, 0, "Wk1")
                        load_w(e2, Wk, w_in, D, 128, O_VD, 128, "Wk1")
                        load_w(e2, Wk, w_in, D, 64, O_KI, 256, "Wk1")
                        load_w(e2, Wk, w_in, D, 64, O_KI, 320, "Wk1")
                    W = work_tiles(es, N, "aw")

                    def a1_stage2(c, h, hn, Ct, Ctn, St, Stn):
                        t0 = c * N
                        for k in range(8):
                            mm(PB[1][:, 0:N], Wk[:, k, 0:128], h[:, k, :], k == 0, k == 7, ["Wk1", hn], ["pb1"])
                        head_norm_rope(PB[1][:, 0:N], "pb1", 128, N, B64, 64, gm[:, 38:39], Ct[:, :], Ctn, St[:, :], Stn, P_part,
                                       kdT[:, t0:t0 + N], "kdT", 2, W, "aw")
                        for k in range(8):
                            mm(PB[3][:, 0:N], Wk[:, k, 256:384], h[:, k, :], k == 0, k == 7, ["Wk1", hn], ["pb3"])
                        head_norm_rope(PB[3][:, 0:N], "pb3", 128, N, B64, 64, gm[:, 39:40], Ct[:, :], Ctn, St[:, :], Stn, P_part,
                                       (kiT_lo[0:64, t0:t0 + N], kiT_hi[64:128, t0:t0 + N]), "kiT", 4, W, "aw")
                        for b in range(N // 128):
                            kb = (t0 // 128) + b
                            for k in range(8):
                                mm(PB[5][:, 0:128], h[:, k, b * 128:(b + 1) * 128], Wk[:, k, 128:256], k == 0, k == 7, [hn, "Wk1"], ["pb5"])
                            cp("act", vd[:, kb, :, 0:64], PB[5][:, 0:128].rearrange("p (g d) -> p g d", g=2), ["pb5"], ["vd"])
                    chunk_pipeline(es, NCH_ALL, xT_all, pos_all, 0, invf_part, 128, 0, "a", a1_stage2)
                with scope() as es:
                    W = {}
                    W["osb"] = sb(es, [128, 512], F32, "bwosb")
                    W["rinv"] = sb(es, [128, 512], F32, "bwrinv")
                    Dg = sb(es, [128, 8, 128], BF16, "Dg")
                    Isb = sb(es, [128, S], F32, "Isb")
                    junk = sb(es, [128, S], mybir.dt.uint8, "junk")
                    Mall = sb(es, [128, S], BF16, "Mall")
                    Rh = [sb(es, [128, 512], BF16, "Rh%d" % i) for i in range(3)]
                    MT = [sb(es, [128, 4, 128], BF16, "MT%d" % i) for i in range(2)]
                    Eb = [sb(es, [128, 512], BF16, "Eb%d" % i) for i in range(3)]
                    bs = sb(es, [128, 8], F32, "bs")
                    qb0 = tok0 // 128

                    def stage_idx(b):
                        ext = 256 * (qb0 + b + 1)
                        for hh in range(8):
                            ts("dve", Dg[:, hh, :], ident, wsgn[:, b, hh:hh + 1], ALU.mult, ["cbf", "wsgn"], ["Dg"])
                        k0 = 0
                        while k0 < ext:
                            kw = min(512, ext - k0)

                            def ymm(hh, k0=k0, kw=kw):
                                kt = kiT_lo if hh % 2 == 0 else kiT_hi
                                mm(PB[hh % 2][:, 0:kw], qiT[:, b, hh // 2, :], kt[:, k0:k0 + kw],
                                   True, True, ["qiT", "kiT"], [PBN[hh % 2]])
                            ymm(0)
                            for hh in range(8):
                                if hh + 1 < 8:
                                    ymm(hh + 1)
                                r = Rh[hh % 3]
                                rn = "Rh%d" % (hh % 3)
                                if hh % 2 == 0:
                                    act(r[:, 0:kw], PB[hh % 2][:, 0:kw], AF.Relu, [PBN[hh % 2], "wabs"], [rn], scale=wabs[:, b, hh:hh + 1])
                                else:
                                    ts("dve", r[:, 0:kw], PB[hh % 2][:, 0:kw], 0.0, ALU.max, [PBN[hh % 2], "wabs"], [rn],
                                       s2=wabs[:, b, hh:hh + 1], op1=ALU.mult)
                                mm(PB[2][:, 0:kw], Dg[:, hh, :], r[:, 0:kw], hh == 0, hh == 7, ["Dg", rn], ["pb2"])
                            cp("act", Isb[:, k0:k0 + kw], PB[2][:, 0:kw], ["pb2"], ["Isb"])
                            k0 += kw

                    def stage_thr(b):
                        ext = 256 * (qb0 + b + 1)
                        op("dve", lambda e: e.tensor_reduce(out=bs[:, 5:6], in_=Isb[:, 0:ext], axis=AX.X, op=ALU.max, apply_absolute_value=True),
                           ["Isb"], ["bs"])
                        tt("dve", Isb[:, ext - 256:ext], Isb[:, ext - 256:ext], mI[:, :], ALU.add, ["Isb", "mI"], ["Isb"])
                        ts("dve", bs[:, 0:1], bs[:, 5:6], -1.0, ALU.mult, ["bs"], ["bs"], s2=-1.0, op1=ALU.add)
                        ts("dve", bs[:, 1:2], bs[:, 5:6], 2.0, ALU.mult, ["bs"], ["bs"], s2=2.0, op1=ALU.add)
                        for it in range(BISECT_ITERS):
                            cst = 2.0 ** -(it + 1)
                            stt(bs[:, 2:3], bs[:, 1:2], cst, bs[:, 0:1], ALU.mult, ALU.add, ["bs"], ["bs"])
                            ts("dve", junk[:, 0:ext], Isb[:, 0:ext], bs[:, 2:3], ALU.is_ge, ["Isb", "bs"], ["junk", "bs"],
                               op1=ALU.add, accum=bs[:, 3:4])
                            ts("dve", bs[:, 4:5], bs[:, 3:4], TOPK - 0.5, ALU.is_ge, ["bs"], ["bs"], s2=cst, op1=ALU.mult)
                            stt(bs[:, 0:1], bs[:, 4:5], bs[:, 1:2], bs[:, 0:1], ALU.mult, ALU.add, ["bs"], ["bs"])

                    def stage_mall(b):
                        ext = 256 * (qb0 + b + 1)
                        ts("dve", Mall[:, 0:ext], Isb[:, 0:ext], bs[:, 0:1], ALU.is_lt, ["Isb", "bs"], ["Mall"], s2=-30000.0, op1=ALU.mult)

                    def stage_attn(b):
                        ext = 256 * (qb0 + b + 1)
                        nkb = ext // 128
                        ngrp = (nkb + 3) // 4

                        def prep(gi):
                            kc = gi * 4
                            nb4 = min(4, nkb - kc)
                            mt = MT[gi % 2]
                            mtn = "MT%d" % (gi % 2)
                            for j in range(nb4):
                                op("pe", lambda e: e.transpose(out=PT[:, j * 128:(j + 1) * 128], in_=Mall[:, (kc + j) * 128:(kc + j + 1) * 128], identity=ident),
                                   ["Mall", "cbf"], ["pt"])
                            cp("act", mt[:, 0:nb4, :], PT[:, 0:nb4 * 128].rearrange("p (a q) -> p a q", a=nb4), ["pt"], [mtn])

                        units = [(kb, g) for kb in range(nkb) for g in range(2)]

                        def emit_S(u):
                            kb, g = units[u]
                            G = slice(g * 64, (g + 1) * 64)
                            bk = 3 + (u % 2)
                            mt = MT[(kb // 4) % 2]
                            mtn = "MT%d" % ((kb // 4) % 2)
                            mm(PB[bk][:, 0:512], kdT[G, kb * 128:(kb + 1) * 128], qdT[G, b, :, :].rearrange("p a q -> p (a q)"),
                               True, False, ["kdT", "qdT"], [PBN[bk]])
                            mm(PB[bk][:, 0:512].rearrange("p (a q) -> p a q", a=4), ident, mt[:, (kb % 4):(kb % 4) + 1, :].to_broadcast([128, 4, 128]),
                               False, True, ["cbf", mtn], [PBN[bk]])

                        def emit_rest(u):
                            kb, g = units[u]
                            bk = 3 + (u % 2)
                            u2 = u % 3
                            act(Eb[u2][:, :], PB[bk][:, 0:512], AF.Exp, [PBN[bk]], ["Eb%d" % u2])
                            mm(PB[5 + g][0:65, 0:512], vd[:, kb, g, :], Eb[u2][:, :], kb == 0, kb == nkb - 1, ["vd", "Eb%d" % u2], [PBN[5 + g]])

                        prep(0)
                        if ngrp > 1:
                            prep(1)
                        emit_S(0)
                        for u in range(len(units)):
                            if u + 1 < len(units):
                                emit_S(u + 1)
                            emit_rest(u)
                            kb, g = units[u]
                            if g == 1 and kb % 4 == 3 and (kb // 4) + 2 < ngrp:
                                prep(kb // 4 + 2)
                        for g in range(2):
                            def writer(osb, rinv, g=g, b=b):
                                for a in range(4):
                                    hd = g * 4 + a
                                    col = b * 128
                                    tt("dve", mixD[(hd % 2) * 64:(hd % 2) * 64 + 64, hd // 2, col:col + 128],
                                       osb[0:64, a * 128:(a + 1) * 128], rinv[0:64, a * 128:(a + 1) * 128], ALU.mult,
                                       ["bwosb", "bwrinv"], ["mixD"])
                            attn_epilogue(PB[5 + g], PBN[5 + g], 3, 512, W, "bw", writer)

                    stage_idx(0)
                    stage_thr(0)
                    stage_mall(0)
                    for b in range(NBH):
                        if b + 1 < NBH:
                            stage_idx(b + 1)
                            stage_thr(b + 1)
                        stage_attn(b)
                        if b + 1 < NBH:
                            stage_mall(b + 1)

            with scope() as pmd:
                mixM = sb(pmd, [128, 4, T_H], BF16, "mixM")
                mT = sb(pmd, [128, 2 * NB, N], BF16, "mT")
                with scope() as es:
                    st2 = sb(es, [128, 2 * NB * N], F32, "cst2")
                    Sc.dma(st2[:], mT_d[:, :], writes=["cst2"])
                    cp("dve", mT[:].rearrange("p a n -> p (a n)"), st2[:], ["cst2"], ["mT"])
                with scope() as pm:
                    ckvnT = sb(pm, [128, S], BF16, "ckvnT")
                    kpeX = sb(pm, [128, S], BF16, "kpeX")
                    cqnT = sb(pm, [128, 2, T_H], BF16, "cqnT")
                    tabC = sb(pm, [128, T_H], BF16, "tabC")
                    tabS = sb(pm, [128, T_H], BF16, "tabS")
                    with scope() as es:
                        Wk = sb(es, [128, 8, 256], BF16, "Wk2")
                        with scope() as e2:
                            load_w(e2, Wk, w_in, D, 128, O_CKV, 0, "Wk2")
                            load_w(e2, Wk, w_in, D, 32, O_KPE, 128, "Wk2")
                            load_w(e2, Wk, w_in, D, 32, O_KPE, 192, "Wk2")
                        W = work_tiles(es, N, "cw")
                        op("pool", lambda e: e.memset(Wk[:, :, 160:192], 0.0), (), ["Wk2"])
                        op("pool", lambda e: e.memset(Wk[:, :, 224:256], 0.0), (), ["Wk2"])

                        def a2_stage2(c, h, hn, Ct, Ctn, St, Stn):
                            t0 = c * N
                            for k in range(8):
                                mm(PB[1][:, 0:N], Wk[:, k, 0:128], h[:, k, :], k == 0, k == 7, ["Wk2", hn], ["pb1"])
                            head_norm_rope(PB[1][:, 0:N], "pb1", 128, N, ones128, 128, gm[:, 34:35], None, None, None, None, None,
                                           ckvnT[:, t0:t0 + N], "ckvnT", 2, W, "cw")
                            for k in range(8):
                                mm(PB[3][:, 0:N], Wk[:, k, 128:256], h[:, k, :], k == 0, k == 7, ["Wk2", hn], ["pb3"])
                            act(kpeX[0:32, t0:t0 + N], PB[3][0:32, 0:N], AF.Square, ["pb3"], ["kpeX"])
                            R = slice(64, 96)
                            ts("dve", W["xn"][R, 0:N], PB[3][R, 0:N], gm[R, 36:37], ALU.mult, ["pb3", "gm"], ["cwxn"])
                            op("pool", lambda e: e.memset(W["xn"][0:64, 0:N], 0.0), (), ["cwxn"])
                            mm(PB[4][0:96, 0:N], P_mla[0:96, 0:96], W["xn"][0:96, 0:N], True, True, ["cbf", "cwxn"], ["pb4"])
                            tt("pool", W["t1"][R, 0:N], W["xn"][R, 0:N], Ct[R, :], ALU.mult, ["cwxn", Ctn], ["cwt1"])
                            tt("dve", W["t2"][R, 0:N], PB[4][R, 0:N], St[R, :], ALU.mult, ["pb4", Stn], ["cwt2"])
                            tt("dve", kpeX[R, t0:t0 + N], W["t1"][R, 0:N], W["t2"][R, 0:N], ALU.add, ["cwt1", "cwt2"], ["kpeX"])
                        chunk_pipeline(es, NCH_ALL, xT_all, pos_all, 0, invf_mla, 96, 0, "c", a2_stage2)
                    with scope() as es:
                        Wc = sb(es, [128, 8, 256], BF16, "Wc")
                        with scope() as e2:
                            load_w(e2, Wc, w_in, D, 256, O_CQ, 0, "Wc")
                        W = work_tiles(es, N, "dw")
                        cqr = sb(es, [128, 2, N], F32, "cqr")
                        cqs = sb(es, [128, 2, N], BF16, "cqs")

                        def l_stage2(c, h, hn, Cf, Cfn, Sf, Sfn):
                            cp("pool", tabC[0:96, c * N:(c + 1) * N], Cf[0:96, :], [Cfn], ["tabC"])
                            cp("pool", tabS[0:96, c * N:(c + 1) * N], Sf[0:96, :], [Sfn], ["tabS"])
                            for r2 in range(2):
                                for k in range(8):
                                    mm(PB[1][:, 0:N], Wc[:, k, r2 * 128:(r2 + 1) * 128], h[:, k, :], k == 0, k == 7, ["Wc", hn], ["pb1"])
                                cp("act", cqr[:, r2, :], PB[1][:, 0:N], ["pb1"], ["cqr"])
                            act(cqs[:, :, :], cqr[:, :, :], AF.Square, ["cqr"], ["cqs"])
                            for r2 in range(2):
                                mm(PB[2][:, 0:N], ones128, cqs[:, r2, :], r2 == 0, r2 == 1, ["cbf", "cqs"], ["pb2"])
                            rstd_from_ps(PB[2], "pb2", 128, N, 256.0, W["rs"], "dwrs", W["tmp"], "dwtmp")
                            for r2 in range(2):
                                stt(cqnT[:, r2, c * N:(c + 1) * N], cqr[:, r2, :], gm[:, 32 + r2:33 + r2], W["rs"][:, 0:N], ALU.mult, ALU.mult,
                                    ["cqr", "gm", "dwrs"], ["cqnT"])
                        chunk_pipeline(es, NCH_H, xT_own, pos_own, tok0, invf_mla, 96, 0, "l", l_stage2)
                    with scope() as es:
                        wuq = sb(es, [128, 2, 768], BF16, "wuq")
                        wukv = sb(es, [128, 1, 1024], BF16, "wukv")
                        with scope() as e2:
                            load_w(e2, wuq, w_uq, 256, 768, 0, 0, "wuq")
                            load_w(e2, wukv, w_ukv, 128, 1024, 0, 0, "wukv")
                        KhT = sb(es, [128, S], BF16, "KhT")
                        Vh = sb(es, [128, NKB, 65], BF16, "Vh")
                        QhT = sb(es, [128, T_H], BF16, "QhT")
                        W = work_tiles(es, N, "ew")
                        W["osb"] = sb(es, [128, 512], F32, "ewosb")
                        W["rinv"] = sb(es, [128, 512], F32, "ewrinv")
                        Eb = [sb(es, [128, N], BF16, "mEb%d" % i) for i in range(3)]
                        op("pool", lambda e: e.memset(Vh[:, :, 64:65], 1.0), (), ["Vh"])
                        for hd in range(8):
                            for c in range(NCH_ALL):
                                t0 = c * N
                                mm(PB[0][0:64, 0:N], wukv[:, 0, hd * 128:hd * 128 + 64], ckvnT[:, t0:t0 + N], True, True, ["wukv", "ckvnT"], ["pb0"])
                                act(W["sq"][0:64, 0:N], PB[0][0:64, 0:N], AF.Square, ["pb0"], ["ewsq"])
                                mm(PB[1][0:96, 0:N], O96[0:64, 0:96], W["sq"][0:64, 0:N], True, False, ["cbf", "ewsq"], ["pb1"])
                                mm(PB[1][0:96, 0:N], O96[0:32, 0:96], kpeX[0:32, t0:t0 + N], False, True, ["cbf", "kpeX"], ["pb1"])
                                rstd_from_ps(PB[1], "pb1", 96, N, 96.0, W["rs"], "ewrs", W["tmp"], "ewtmp")
                                stt(KhT[0:64, t0:t0 + N], PB[0][0:64, 0:N], gm[0:64, 36:37], W["rs"][0:64, 0:N], ALU.mult, ALU.mult,
                                    ["pb0", "gm", "ewrs"], ["KhT"])
                                tt("pool", KhT[64:96, t0:t0 + N], kpeX[64:96, t0:t0 + N], W["rs"][64:96, 0:N], ALU.mult, ["kpeX", "ewrs"], ["KhT"])
                            for kb0 in range(0, NKB, 8):
                                for j in range(8):
                                    kb = kb0 + j
                                    mm(PB[2][:, j * 64:(j + 1) * 64], ckvnT[:, kb * 128:(kb + 1) * 128], wukv[:, 0, hd * 128 + 64:hd * 128 + 128],
                                       True, True, ["ckvnT", "wukv"], ["pb2"])
                                cp("act", Vh[:, kb0:kb0 + 8, 0:64], PB[2][:, 0:512].rearrange("p (a d) -> p a d", a=8), ["pb2"], ["Vh"])
                            for c in range(NCH_H):
                                for r2 in range(2):
                                    mm(PB[0][0:96, 0:N], wuq[:, r2, hd * 96:(hd + 1) * 96], cqnT[:, r2, c * N:(c + 1) * N], r2 == 0, r2 == 1,
                                       ["wuq", "cqnT"], ["pb0"])
                                head_norm_rope(PB[0][0:96, 0:N], "pb0", 96, N, O96, 96, gs[0:96, 0:1], tabC[0:96, c * N:(c + 1) * N], "tabC",
                                               tabS[0:96, c * N:(c + 1) * N], "tabS", P_mla, QhT[0:96, c * N:(c + 1) * N], "QhT", 1, W, "ew")
                            for c in range(NCH_H):
                                j = (tok0 // N) + c
                                ext = 2 * NB * (j + 1)
                                bo = 3 + (c % 2)
                                def emit_S(kb, c=c, j=j):
                                    bs_ = 5 + (kb % 2)
                                    dg = kb >= 2 * NB * j
                                    mm(PB[bs_][:, 0:N], KhT[0:96, kb * 128:(kb + 1) * 128], QhT[0:96, c * N:(c + 1) * N], True, not dg, ["KhT", "QhT"], [PBN[bs_]])
                                    if dg:
                                        mm(PB[bs_][:, 0:N], ident, mT[:, kb - 2 * NB * j, :], False, True, ["cbf", "mT"], [PBN[bs_]])

                                def emit_rest(kb, ext=ext, bo=bo):
                                    bs_ = 5 + (kb % 2)
                                    e_ = Eb[kb % 3]
                                    en = "mEb%d" % (kb % 3)
                                    act(e_[:, :], PB[bs_][:, 0:N], AF.Exp, [PBN[bs_]], [en])
                                    mm(PB[bo][0:65, 0:N], Vh[:, kb, :], e_[:, :], kb == 0, kb == ext - 1, ["Vh", en], [PBN[bo]])
                                emit_S(0)
                                for kb in range(ext):
                                    if kb + 1 < ext:
                                        emit_S(kb + 1)
                                    emit_rest(kb)

                                def writer(osb, rinv, hd=hd, c=c):
                                    tt("dve", mixM[(hd % 2) * 64:(hd % 2) * 64 + 64, hd // 2, c * N:(c + 1) * N], osb[0:64, 0:N], rinv[0:64, 0:N],
                                       ALU.mult, ["ewosb", "ewrinv"], ["mixM"])
                                attn_epilogue(PB[bo], PBN[bo], 2, N, W, "ew", writer)

                if dbg:
                    with scope() as es:
                        mf = sb(es, [128, 8, T_H], F32, "mf")
                        cp("dve", mf[:, 0:4, :], mixM[:], ["mixM"], ["mf"])
                        cp("dve", mf[:, 4:8, :], mixD[:], ["mixD"], ["mf"])
                        Sc.dma(dbg_mixed[:, tok0:tok0 + T_H].rearrange("(k p) n -> p k n", p=128), mf[:], reads=["mf"], is_out=True)

                with scope() as es:
                    wo_ = sb(es, [128, 8, D], BF16, "wo_")
                    wq_ = sb(es, [128, 8, 512], BF16, "wq_")
                    wmo = sb(es, [128, 4, D], BF16, "wmo")
                    with scope() as e2:
                        for q4 in range(4):
                            with scope() as e3:
                                load_w(e3, wo_, w_out, D, 256, q4 * 256, q4 * 256, "wo_")
                        load_w(e2, wq_, m_wq, D, 512, 0, 0, "wq_")
                    with scope() as e2:
                        load_w(e2, wmo, m_wo, 512, D, 0, 0, "wmo")
                    x1 = sb(es, [128, 8, N], F32, "x1")
                    h = sb(es, [128, 8, N], BF16, "h4")
                    W = work_tiles(es, N, "fw")
                    om = sb(es, [128, 4, N], BF16, "om")
                    Eb = [sb(es, [128, N], BF16, "cEb%d" % i) for i in range(2)]
                    actT = sb(es, [128, DFF // 128, N], BF16, "actT")
                    stg = [sb(es, [128, 4096], F32, "fstg%d" % i) for i in range(2)]
                    wsl = [sb(es, [128, 4096], BF16, "fws%d" % i) for i in range(3)]
                    sg = sb(es, [128, N], F32, "sg")
                    nld = [0]

                    def load_slab(src_ap, a_, b_):
                        i = nld[0]
                        nld[0] += 1
                        sn = "fstg%d" % (i % 2)
                        wn_ = "fws%d" % (i % 3)
                        s_ = stg[i % 2][:, 0:a_ * b_].rearrange("p (a b) -> p a b", a=a_)
                        w_ = wsl[i % 3][:, 0:a_ * b_].rearrange("p (a b) -> p a b", a=a_)
                        Sc.dma(s_, src_ap, writes=[sn])
                        cp("pool" if i % 2 == 0 else "dve", w_, s_, [sn], [wn_])
                        return w_, wn_

                    sq = actT[:, 0:8, :]
                    for c in range(NCH_H):
                        lt0 = tok0 + c * N
                        Sc.dma(x1[:], xT_own[:, lt0:lt0 + N].rearrange("(k p) n -> p k n", p=128), writes=["x1"])
                        for m in range(8):
                            bk = m % 2
                            for k in range(8):
                                mm(PB[bk][:, 0:N], wo_[:, k, m * 128:(m + 1) * 128], (mixM if k < 4 else mixD)[:, k % 4, c * N:(c + 1) * N], k == 0, k == 7, ["wo_", "mixM", "mixD"], [PBN[bk]])
                            tt("dve", x1[:, m, :], x1[:, m, :], PB[bk][:, 0:N], ALU.add, ["x1", PBN[bk]], ["x1"])
                        norm_chunk(x1, "x1", N, 8, h, "h4", sq, "actT", 2, W["tmp"], "fwtmp", W["rs"], "fwrs")
                        for hh in range(4):
                            for k in range(8):
                                mm(PB[3][:, 0:N], wq_[:, k, hh * 128:(hh + 1) * 128], h[:, k, :], k == 0, k == 7, ["wq_", "h4"], ["pb3"])
                            head_norm_rope(PB[3][:, 0:N], "pb3", 128, N, ones128, 128, gs[:, 2:3], None, None, None, None, None,
                                           W["xn"][:, 0:N], "fwxn2", 4, W, "fw")
                            for blk in range(2):
                                mm(PB[5][:, 0:N], KmT[:, hh, blk * 128:(blk + 1) * 128], W["xn"][:, 0:N], True, True, ["KmT", "fwxn2"], ["pb5"])
                                act(Eb[blk][:, :], PB[5][:, 0:N], AF.Exp, ["pb5"], ["cEb%d" % blk])
                            for blk in range(2):
                                mm(PB[6][:, 0:N], Vm[:, blk, hh * 128:(hh + 1) * 128], Eb[blk][:, :], blk == 0, blk == 1, ["Vm", "cEb%d" % blk], ["pb6"])
                            for blk in range(2):
                                mm(PB[4][:, 0:N], ones128, Eb[blk][:, :], blk == 0, blk == 1, ["cbf", "cEb%d" % blk], ["pb4"])
                            op("dve", lambda e: e.reciprocal(out=W["t1"][:, 0:N], in_=PB[4][:, 0:N]), ["pb4"], ["fwt1"])
                            tt("dve", om[:, hh, :], PB[6][:, 0:N], W["t1"][:, 0:N], ALU.mult, ["pb6", "fwt1"], ["om"])
                        for m in range(8):
                            bk = m % 2
                            for hh in range(4):
                                mm(PB[bk][:, 0:N], wmo[:, hh, m * 128:(m + 1) * 128], om[:, hh, :], hh == 0, hh == 3, ["wmo", "om"], [PBN[bk]])
                            tt("dve", x1[:, m, :], x1[:, m, :], PB[bk][:, 0:N], ALU.add, ["x1", PBN[bk]], ["x1"])
                        norm_chunk(x1, "x1", N, 24, h, "h4", sq, "actT", 2, W["tmp"], "fwtmp", W["rs"], "fwrs")
                        slabs = [(c0, min(512, DFF - c0)) for c0 in range(0, DFF, 512)]
                        for (c0, cw) in slabs:
                            wg, wgn = load_slab(f_wg[:, c0:c0 + cw].rearrange("(k p) c -> p k c", p=128), 8, cw)
                            wu, wun = load_slab(f_wu[:, c0:c0 + cw].rearrange("(k p) c -> p k c", p=128), 8, cw)
                            for f in range(cw // 128):
                                ff = c0 // 128 + f
                                bg = 3 + (ff % 2)
                                bu = 5 + (ff % 2)
                                for k in range(8):
                                    mm(PB[bg][:, 0:N], wg[:, k, f * 128:(f + 1) * 128], h[:, k, :], k == 0, k == 7, [wgn, "h4"], [PBN[bg]])
                                for k in range(8):
                                    mm(PB[bu][:, 0:N], wu[:, k, f * 128:(f + 1) * 128], h[:, k, :], k == 0, k == 7, [wun, "h4"], [PBN[bu]])
                                act(sg[:, :], PB[bg][:, 0:N], AF.Silu, [PBN[bg]], ["sg"])
                                tt("dve", actT[:, ff, :], sg[:, :], PB[bu][:, 0:N], ALU.mult, ["sg", PBN[bu]], ["actT"])
                        for grp in ((0, 1, 2, 3), (4, 5, 6, 7)):
                            rslabs = [(r0, min(512, DFF - r0)) for r0 in range(0, DFF, 512)]
                            for (r0, rw) in rslabs:
                                nf = rw // 128
                                wd, wdn = load_slab(f_wd[r0:r0 + rw, grp[0] * 128:grp[0] * 128 + 512].rearrange("(f p) c -> p f c", p=128), nf, 512)
                                for f in range(nf):
                                    ff = r0 // 128 + f
                                    for mi, m in enumerate(grp):
                                        mm(PB[mi][:, 0:N], wd[:, f, mi * 128:(mi + 1) * 128], actT[:, ff, :], ff == 0, ff == DFF // 128 - 1, [wdn, "actT"], [PBN[mi]])
                            for mi, m in enumerate(grp):
                                tt("dve", x1[:, m, :], x1[:, m, :], PB[mi][:, 0:N], ALU.add, ["x1", PBN[mi]], ["x1"])
                        Sc.dma(outT[:, lt0:lt0 + N].rearrange("(k p) n -> p k n", p=128), x1[:], reads=["x1"], is_out=True)
        Sc.finish()
    return nc, Sc


def _consts(S, NH, half):
    T_OWN = S // 2
    T_H = T_OWN // NH
    N = min(512, T_H)
    NB = N // 128
    cb = np.zeros((128, 6 * 128), np.float32)
    cb[:, 0:128] = 1.0
    cb[0:64, 128:192] = 1.0
    cb[64:128, 192:256] = 1.0
    cb[0:96, 256:352] = 1.0
    Pp = np.zeros((128, 128), np.float32)
    for base in (0, 64):
        for i in range(8):
            Pp[base + 8 + i, base + i] = -1.0
            Pp[base + i, base + 8 + i] = 1.0
    cb[:, 384:512] = Pp
    Pm = np.zeros((128, 128), np.float32)
    for i in range(16):
        Pm[80 + i, 64 + i] = -1.0
        Pm[64 + i, 80 + i] = 1.0
    cb[:, 512:640] = Pm
    cb[:, 640:768] = np.eye(128, dtype=np.float32)
    cf = np.zeros((128, 66), np.float32)
    cf[64, 0:64] = 1.0
    f8 = (THETA ** (-np.arange(0, 16, 2, dtype=np.float32) / np.float32(16))).astype(np.float32)
    f16 = (THETA ** (-np.arange(0, 32, 2, dtype=np.float32) / np.float32(32))).astype(np.float32)
    for base in (0, 64):
        cf[base:base + 8, 64] = f8
        cf[base + 8:base + 16, 64] = f8
    cf[64:80, 65] = f16
    cf[80:96, 65] = f16
    mI = np.zeros((128, 256), np.float32)
    q = np.arange(128)[:, None]
    k = np.arange(256)[None, :]
    qpos = half * 128 + q
    mI[:] = np.where(k <= qpos, 0.0, NEG)
    mT = np.zeros((128, 2 * NB, N), np.float32)
    p = np.arange(128)[:, None]
    for kbp in range(2 * NB):
        for m in range(NB):
            r = np.arange(128)[None, :]
            gb = 2 * m + half
            if kbp < gb:
                v = np.zeros((128, 128), np.float32)
            elif kbp == gb:
                v = np.where(p <= r, 0.0, -30000.0).astype(np.float32)
            else:
                v = np.full((128, 128), -30000.0, np.float32)
            mT[:, kbp, m * 128:(m + 1) * 128] = v
    return cb, cf, mI, mT.reshape(128, 2 * NB * N)


def _gains(inp):
    gm = np.zeros((128, 42), np.float32)

    def col8(v, c0):
        gm[:, c0:c0 + 8] = np.asarray(v, np.float32).reshape(8, 128).T
    col8(inp["norm_mix"][0], 0)
    col8(inp["norm_mem_x"][0], 8)
    col8(inp["norm_mem_kv"][0], 16)
    col8(inp["norm_ffn"][0], 24)
    gm[:, 32:34] = np.asarray(inp["mla_q_a_norm"][0], np.float32).reshape(2, 128).T
    gm[:, 34] = inp["mla_kv_a_norm"][0]
    gm[0:96, 35] = inp["mla_q_norm"][0]
    gm[0:96, 36] = inp["mla_k_norm"][0]
    gm[:, 37] = np.tile(np.asarray(inp["dsa_q_norm"][0], np.float32), 2)
    gm[:, 38] = np.tile(np.asarray(inp["dsa_k_norm"][0], np.float32), 2)
    gm[:, 39] = np.tile(np.asarray(inp["idx_k_norm"][0], np.float32), 2)
    gm[:, 40] = inp["mem_q_norm"][0]
    gm[:, 41] = inp["mem_k_norm"][0]
    return gm


_CACHE = {}


def run(inputs, S, NH, dbg=False):
    inp = {k: np.asarray(v) for k, v in inputs.items()}
    B = inp["x"].shape[0]
    ncores = 2 * B
    key = (S, NH, dbg)
    if key not in _CACHE:
        _CACHE[key] = build(S, NH, dbg)
    nc, Sc = _CACHE[key]
    gm = _gains(inp)
    nblk = S // 128
    in_maps = []
    for c in range(ncores):
        b, half = c // 2, c % 2
        own_blocks = np.arange(half, nblk, 2)
        own_idx = (own_blocks[:, None] * 128 + np.arange(128)[None, :]).reshape(-1)
        xb = np.asarray(inp["x"][b], np.float32)
        cb, cf, mI, mT = _consts(S, NH, half)
        pos = np.asarray(inp["positions"][b], np.int32)
        in_maps.append({
            "xT_all": np.ascontiguousarray(xb.T),
            "xT_own": np.ascontiguousarray(xb[own_idx].T),
            "pos_all": np.ascontiguousarray(pos.reshape(1, S)),
            "pos_own": np.ascontiguousarray(pos[own_idx].reshape(1, S // 2)),
            "memT": np.ascontiguousarray(np.asarray(inp["mem"][b], np.float32).T),
            "w_in": np.ascontiguousarray(inp["w_in"][0], dtype=np.float32),
            "w_uq": np.ascontiguousarray(inp["mla_w_uq"][0], dtype=np.float32),
            "w_ukv": np.ascontiguousarray(inp["mla_w_ukv"][0], dtype=np.float32),
            "w_out": np.ascontiguousarray(inp["w_out"][0], dtype=np.float32),
            "m_wq": np.ascontiguousarray(inp["mem_w_q"][0], dtype=np.float32),
            "m_wk": np.ascontiguousarray(inp["mem_w_k"][0], dtype=np.float32),
            "m_wv": np.ascontiguousarray(inp["mem_w_v"][0], dtype=np.float32),
            "m_wo": np.ascontiguousarray(inp["mem_w_o"][0], dtype=np.float32),
            "f_wg": np.ascontiguousarray(inp["ffn_w_gate"][0], dtype=np.float32),
            "f_wu": np.ascontiguousarray(inp["ffn_w_up"][0], dtype=np.float32),
            "f_wd": np.ascontiguousarray(inp["ffn_w_down"][0], dtype=np.float32),
            "gm": gm, "cb": cb, "cf": cf, "mI": mI, "mT": mT,
        })
    res = run_bass_kernel_spmd(nc, in_maps, core_ids=list(range(ncores)))
    out = np.zeros((B, S, D), np.float32)
    extra = {}
    for c in range(ncores):
        b, half = c // 2, c % 2
        own_blocks = np.arange(half, nblk, 2)
        own_idx = (own_blocks[:, None] * 128 + np.arange(128)[None, :]).reshape(-1)
        out[b, own_idx, :] = np.asarray(res.results[c]["outT"]).T
        if dbg:
            extra[c] = (own_idx, np.asarray(res.results[c]["dbg_mixed"]).T)
    return out, extra


def kernel(**inputs):
    out, _ = run(inputs, 8192, 2)
    return out
```

```python
import math
from contextlib import ExitStack

import numpy as np
import concourse.bass as bass
import concourse.mybir as mybir
from concourse.bass_utils import run_bass_kernel_spmd

F32 = mybir.dt.float32
BF16 = mybir.dt.bfloat16
I32 = mybir.dt.int32
AF = mybir.ActivationFunctionType
ALU = mybir.AluOpType
AX = mybir.AxisListType

D = 1024
NMEM = 256
DFF = 2816
EPS = 1e-6
TOPK = 256
BISECT_ITERS = 20
NEG = -1.0e30
O_CQ, O_CKV, O_KPE, O_QD, O_KD, O_VD, O_QI, O_KI, O_WI = 0, 256, 384, 416, 928, 1056, 1184, 1696, 1760
THETA = 500000.0


class Sched:
    def __init__(self, nc):
        self.nc = nc
        self.engs = {"pe": nc.tensor, "act": nc.scalar, "dve": nc.vector, "pool": nc.gpsimd, "sp": nc.sync}
        self.sems = {}
        self.cnt = {}
        self.seen = {k: {} for k in self.engs}
        self.lastw = {}
        self.readers = {}
        self._stack = []
        self.ninst = 0
        self.nwait = 0
        for k in ["pe", "act", "dve", "pool"]:
            self.sems[k] = self.sem("s_" + k)
            self.cnt[k] = 0
        self.dsems = [[self.sem("d%d" % i), 0] for i in range(24)]
        self.dnext = 0
        self.out_events = []

    def sem(self, name):
        cm = self.nc.semaphore(name)
        s = cm.__enter__()
        self._stack.append(cm)
        return s

    def _wait(self, e, ev):
        if ev is None:
            return
        s, v = ev
        if e == "pe" and s is self.sems["pe"]:
            return
        key = id(s)
        if self.seen[e].get(key, 0) >= v:
            return
        self.engs[e].wait_ge(s, v)
        self.seen[e][key] = v
        self.nwait += 1

    def deps(self, e, reads, writes):
        for r in reads:
            self._wait(e, self.lastw.get(r))
        for w in writes:
            self._wait(e, self.lastw.get(w))
            for ev in self.readers.get(w, {}).values():
                self._wait(e, ev)

    def record(self, ev, reads, writes):
        for r in reads:
            d = self.readers.setdefault(r, {})
            d[id(ev[0])] = ev
        for w in writes:
            self.lastw[w] = ev
            self.readers[w] = {}

    def op(self, e, fn, reads=(), writes=()):
        self.deps(e, reads, writes)
        inst = fn(self.engs[e])
        self.cnt[e] += 1
        inst.then_inc(self.sems[e], 1)
        ev = (self.sems[e], self.cnt[e])
        self.record(ev, reads, writes)
        self.ninst += 1
        return ev

    def dma(self, out, in_, reads=(), writes=(), is_out=False):
        q = "sp"
        ds = self.dsems[self.dnext % len(self.dsems)]
        self.dnext += 1
        if ds[1] > 0:
            self._wait(q, (ds[0], ds[1]))
        self.deps(q, reads, writes)
        inst = self.engs[q].dma_start(out=out, in_=in_)
        ds[1] += 16
        inst.then_inc(ds[0], 16)
        ev = (ds[0], ds[1])
        self.record(ev, reads, writes)
        self.ninst += 1
        if is_out:
            self.out_events.append(ev)
        return ev

    def barrier(self):
        evs = [(self.sems[k], self.cnt[k]) for k in ["pe", "act", "dve", "pool"] if self.cnt[k] > 0]
        evs += [(d[0], d[1]) for d in self.dsems if d[1] > 0]
        for e in ["pe", "act", "dve", "pool", "sp"]:
            for ev in evs:
                self._wait(e, ev)

    def finish(self):
        for ev in self.out_events:
            self._wait("sp", ev)
        for d in self.dsems:
            if d[1] > 0:
                self._wait("sp", (d[0], d[1]))
        for k in ["pe", "act", "dve", "pool"]:
            if self.cnt[k] > 0:
                self._wait("sp", (self.sems[k], self.cnt[k]))
        for cm in reversed(self._stack):
            cm.__exit__(None, None, None)


def build(S, NH, dbg=False):
    T_OWN = S // 2
    T_H = T_OWN // NH
    N = min(512, T_H)
    NB = N // 128
    NCH_ALL = S // N
    NCH_H = T_H // N
    NKB = S // 128
    nc = bass.Bass("TRN2", target_bir_lowering=False)

    def dram(name, shape, dt=F32, kind="ExternalInput"):
        return nc.dram_tensor(name, shape, dt, kind=kind).ap()

    xT_all = dram("xT_all", [D, S])
    xT_own = dram("xT_own", [D, T_OWN])
    pos_all = dram("pos_all", [1, S], I32)
    pos_own = dram("pos_own", [1, T_OWN], I32)
    memT = dram("memT", [D, NMEM])
    w_in = dram("w_in", [D, 1768])
    w_uq = dram("w_uq", [256, 768])
    w_ukv = dram("w_ukv", [128, 1024])
    w_out = dram("w_out", [D, D])
    m_wq = dram("m_wq", [D, 512])
    m_wk = dram("m_wk", [D, 512])
    m_wv = dram("m_wv", [D, 512])
    m_wo = dram("m_wo", [512, D])
    f_wg = dram("f_wg", [D, DFF])
    f_wu = dram("f_wu", [D, DFF])
    f_wd = dram("f_wd", [DFF, D])
    gm_d = dram("gm", [128, 42])
    cb_d = dram("cb", [128, 6 * 128])
    cf_d = dram("cf", [128, 66])
    mI_d = dram("mI", [128, 256])
    mT_d = dram("mT", [128, 2 * NB * N])
    outT = dram("outT", [D, T_OWN], kind="ExternalOutput")
    if dbg:
        dbg_mixed = dram("dbg_mixed", [D, T_OWN], kind="ExternalOutput")

    Sc = Sched(nc)
    uid = [0]

    def nm(p):
        uid[0] += 1
        return "%s_%d" % (p, uid[0])

    def sb(es, shape, dt, name="t"):
        return es.enter_context(nc.sbuf_tensor(nm(name), shape, dt))

    def ps(es, shape, dt, name="p"):
        return es.enter_context(nc.psum_tensor(nm(name), shape, dt))

    op = Sc.op

    from contextlib import contextmanager

    @contextmanager
    def scope():
        with ExitStack() as es_:
            yield es_
        Sc.barrier()

    def mm(out, lhsT, rhs, start, stop, reads, writes):
        return op("pe", lambda e: e.matmul(out, lhsT=lhsT, rhs=rhs, start=start, stop=stop), reads, writes)

    def act(out, in_, func, reads, writes, scale=None, bias=None):
        kw = {}
        if scale is not None:
            kw["scale"] = scale
        if bias is not None:
            kw["bias"] = bias
        return op("act", lambda e: e.activation(out=out, in_=in_, func=func, **kw), reads, writes)

    def ts(eng, out, in0, s1, op0, reads, writes, s2=None, op1=None, accum=None):
        kw = {}
        if op1 is not None:
            kw["op1"] = op1
        if accum is not None:
            kw["accum_out"] = accum
        return op(eng, lambda e: e.tensor_scalar(out=out, in0=in0, scalar1=s1, scalar2=s2, op0=op0, **kw), reads, writes)

    def tt(eng, out, in0, in1, o, reads, writes):
        return op(eng, lambda e: e.tensor_tensor(out=out, in0=in0, in1=in1, op=o), reads, writes)

    def stt(out, in0, scalar, in1, op0, op1, reads, writes):
        return op("dve", lambda e: e.scalar_tensor_tensor(out=out, in0=in0, scalar=scalar, in1=in1, op0=op0, op1=op1), reads, writes)

    def cp(eng, out, in_, reads, writes):
        if eng == "act":
            return act(out, in_, AF.Copy, reads, writes)
        return op(eng, lambda e: e.tensor_copy(out=out, in_=in_), reads, writes)

    with ExitStack() as top:
        gm = sb(top, [128, 42], F32, "gm")
        cf = sb(top, [128, 66], F32, "cf")
        cbf = sb(top, [128, 6 * 128], BF16, "cbf")
        gs = sb(top, [128, 4], F32, "gs")
        epsc = sb(top, [128, 1], F32, "epsc")
        shc = sb(top, [128, 2], F32, "shc")
        mI = sb(top, [128, 256], F32, "mI")
        mixD = sb(top, [128, 4, T_H], BF16, "mixD")
        with scope() as es:
            st = sb(es, [128, 6 * 128], F32, "cst")
            Sc.dma(st[:], cb_d[:, :], writes=["cst"])
            cp("dve", cbf[:], st[:], ["cst"], ["cbf"])
        Sc.dma(gm[:], gm_d[:, :], writes=["gm"])
        Sc.dma(cf[:], cf_d[:, :], writes=["cf"])
        Sc.dma(mI[:], mI_d[:, :], writes=["mI"])
        op("dve", lambda e: e.memset(epsc[:], EPS), (), ["epsc"])
        op("dve", lambda e: e.memset(shc[:, 0:1], 0.0), (), ["shc"])
        op("dve", lambda e: e.memset(shc[:, 1:2], math.pi / 2), (), ["shc"])
        ts("dve", gs[:, 0:1], gm[:, 35:36], 96 ** -0.5, ALU.mult, ["gm"], ["gs"])
        ts("dve", gs[:, 1:2], gm[:, 37:38], 64 ** -0.5, ALU.mult, ["gm"], ["gs"])
        ts("dve", gs[:, 2:3], gm[:, 40:41], 128 ** -0.5, ALU.mult, ["gm"], ["gs"])
        ones128 = cbf[:, 0:128]
        B64 = cbf[:, 128:256]
        O96 = cbf[:, 256:384]
        P_part = cbf[:, 384:512]
        P_mla = cbf[:, 512:640]
        ident = cbf[:, 640:768]
        sel = cf[:, 0:64]
        invf_part = cf[:, 64:65]
        invf_mla = cf[:, 65:66]
        CONST = ["gm", "cf", "cbf", "gs", "epsc"]

        PB = [ps(top, [128, 512], F32, "pb%d" % i) for i in range(7)]
        PT = ps(top, [128, 1024], BF16, "pt")
        PBN = ["pb%d" % i for i in range(7)]

        def load_w(es, dst, src, rows, cols, c0, dcol0, name):
            kc = rows // 128
            stg = sb(es, [128, kc, cols], F32, "wst")
            r = nm("wst")
            Sc.dma(stg[:], src[:, c0:c0 + cols].rearrange("(k p) c -> p k c", p=128), writes=[r])
            cp("pool", dst[:, :, dcol0:dcol0 + cols], stg[:], [r], [name])

        def rstd_from_ps(pss, psname, rows, n, dim, out, outname, tmp, tmpname):
            act(tmp[0:rows, 0:n], pss[0:rows, 0:n], AF.Ln, [psname, "epsc"], [tmpname], scale=1.0 / dim, bias=epsc[0:rows, :])
            act(out[0:rows, 0:n], tmp[0:rows, 0:n], AF.Exp, [tmpname], [outname], scale=-0.5)

        def norm_chunk(xs, xsname, n, gcol0, h, hname, sq, sqname, bank, tmp, tmpname, rs, rsname):
            act(sq[:, :, 0:n], xs[:, :, 0:n], AF.Square, [xsname], [sqname])
            for k in range(8):
                mm(PB[bank][:, 0:n], ones128, sq[:, k, 0:n], k == 0, k == 7, ["cbf", sqname], [PBN[bank]])
            rstd_from_ps(PB[bank], PBN[bank], 128, n, float(D), rs, rsname, tmp, tmpname)
            for k in range(8):
                stt(h[:, k, 0:n], xs[:, k, 0:n], gm[:, gcol0 + k:gcol0 + k + 1], rs[:, 0:n], ALU.mult, ALU.mult,
                    [xsname, "gm", rsname], [hname])

        def rope_tmp(es, n):
            return {"pi": sb(es, [128, n], I32, "rp_pi"), "pf": sb(es, [128, n], F32, "rp_pf"), "a": sb(es, [128, n], F32, "rp_a"),
                    "u": sb(es, [128, n], F32, "rp_u"), "ki": sb(es, [128, n], I32, "rp_ki"), "r": sb(es, [128, n], F32, "rp_r"),
                    "t": sb(es, [128, n], F32, "rp_t"), "k": nm("rp")}

        def rope_tables(RT, pos_d, t0, n, invf, rows, Ct, Ctn, St, Stn, cview=None, sview=None):
            pi, pf, a, u, ki, r, t, k = RT["pi"], RT["pf"], RT["a"], RT["u"], RT["ki"], RT["r"], RT["t"], RT["k"]
            R = slice(0, rows)
            Sc.dma(pi[:], pos_d[0:1, t0:t0 + n].to_broadcast([128, n]), writes=[k + "pi"])
            cp("dve", pf[R, :], pi[R, :], [k + "pi"], [k + "pf"])
            ts("dve", pf[R, :], pf[R, :], invf[R, :], ALU.mult, [k + "pf", "cf"], [k + "pf"])
            for shift, dst, dstn, view in ((0.0, St, Stn, sview), (math.pi / 2, Ct, Ctn, cview)):
                ts("dve", a[R, :], pf[R, :], shift, ALU.add, [k + "pf"], [k + "a"])
                ts("dve", u[R, :], a[R, :], 1.0 / (2 * math.pi), ALU.mult, [k + "a"], [k + "u"])
                cp("dve", ki[R, :], u[R, :], [k + "u"], [k + "ki"])
                cp("dve", u[R, :], ki[R, :], [k + "ki"], [k + "u"])
                stt(r[R, :], u[R, :], -2 * math.pi, a[R, :], ALU.mult, ALU.add, [k + "u", k + "a"], [k + "r"])
                ts("dve", t[R, :], r[R, :], math.pi, ALU.is_gt, [k + "r"], [k + "t"], s2=-2 * math.pi, op1=ALU.mult)
                tt("dve", r[R, :], r[R, :], t[R, :], ALU.add, [k + "r", k + "t"], [k + "r"])
                ts("dve", t[R, :], r[R, :], -math.pi, ALU.is_lt, [k + "r"], [k + "t"], s2=2 * math.pi, op1=ALU.mult)
                tt("dve", r[R, :], r[R, :], t[R, :], ALU.add, [k + "r", k + "t"], [k + "r"])
                ts("dve", r[R, :], r[R, :], math.pi, ALU.min, [k + "r"], [k + "r"], s2=-math.pi, op1=ALU.max)
                act(dst[R, :] if view is None else view, r[R, :], AF.Sin, [k + "r"], [dstn])

        def head_norm_rope(src, srcname, rows, n, blk, dim, gcol, Ct, Ctn, St, Stn, Pm, out, outname, bank, W, wn):
            R = slice(0, rows)
            if blk is not None:
                act(W["sq"][R, 0:n], src, AF.Square, [srcname], [wn + "sq"])
                mm(PB[bank][R, 0:n], blk[R, R], W["sq"][R, 0:n], True, True, ["cbf", wn + "sq"], [PBN[bank]])
                rstd_from_ps(PB[bank], PBN[bank], rows, n, float(dim), W["rs"], wn + "rs", W["tmp"], wn + "tmp")
                dst = W["xn"][R, 0:n] if Ct is not None else out
                dn = wn + "xn" if Ct is not None else outname
                stt(dst, src, gcol, W["rs"][R, 0:n], ALU.mult, ALU.mult, [srcname, "gm", "gs", wn + "rs"], [dn])
            else:
                cp("act", W["xn"][R, 0:n], src, [srcname], [wn + "xn"])
            if Ct is not None:
                mm(PB[bank][R, 0:n], Pm[R, R], W["xn"][R, 0:n], True, True, ["cbf", wn + "xn"], [PBN[bank]])
                tt("pool", W["t1"][R, 0:n], W["xn"][R, 0:n], Ct, ALU.mult, [wn + "xn", Ctn], [wn + "t1"])
                tt("dve", W["t2"][R, 0:n], PB[bank][R, 0:n], St, ALU.mult, [PBN[bank], Stn], [wn + "t2"])
                i0, i1 = W["t1"][R, 0:n], W["t2"][R, 0:n]
                if isinstance(out, tuple):
                    tt("dve", out[0], W["t1"][0:64, 0:n], W["t2"][0:64, 0:n], ALU.add, [wn + "t1", wn + "t2"], [outname])
                    tt("dve", out[1], W["t1"][64:128, 0:n], W["t2"][64:128, 0:n], ALU.add, [wn + "t1", wn + "t2"], [outname])
                    return
                if len(out.shape) == 3:
                    i0 = i0.rearrange("p (b q) -> p b q", q=128)
                    i1 = i1.rearrange("p (b q) -> p b q", q=128)
                tt("dve", out, i0, i1, ALU.add, [wn + "t1", wn + "t2"], [outname])

        def chunk_pipeline(es, nch, src_d, pos_d, tokoff, invf, rows, gcol0, pref, stage2, want_rope=True):
            xs = sb(es, [128, 8, N], F32, pref + "xs")
            hh_ = [sb(es, [128, 8, N], BF16, pref + "h%d" % i) for i in range(2)]
            sq = sb(es, [128, 8, N], BF16, pref + "sq")
            rs1 = sb(es, [128, N], F32, pref + "rs1")
            tmp1 = sb(es, [128, N], F32, pref + "tmp1")
            if want_rope:
                Cts = [sb(es, [128, N], F32, pref + "C%d" % i) for i in range(2)]
                Sts = [sb(es, [128, N], F32, pref + "S%d" % i) for i in range(2)]
                RT = rope_tmp(es, N)

            def s1a(c):
                i = c % 2
                t0 = tokoff + c * N
                Sc.dma(xs[:], src_d[:, t0:t0 + N].rearrange("(k p) n -> p k n", p=128), writes=[pref + "xs"])
                if want_rope:
                    rope_tables(RT, pos_d, t0, N, invf, rows, Cts[i], pref + "C%d" % i, Sts[i], pref + "S%d" % i)
                act(sq[:, :, 0:N], xs[:, :, 0:N], AF.Square, [pref + "xs"], [pref + "sq"])
                for k in range(8):
                    mm(PB[0][:, 0:N], ones128, sq[:, k, 0:N], k == 0, k == 7, ["cbf", pref + "sq"], [PBN[0]])
                rstd_from_ps(PB[0], PBN[0], 128, N, float(D), rs1, pref + "rs1", tmp1, pref + "tmp1")

            def s1b(c):
                i = c % 2
                for k in range(8):
                    stt(hh_[i][:, k, 0:N], xs[:, k, 0:N], gm[:, gcol0 + k:gcol0 + k + 1], rs1[:, 0:N], ALU.mult, ALU.mult,
                        [pref + "xs", "gm", pref + "rs1"], [pref + "h%d" % i])
            s1a(0)
            s1b(0)
            for c in range(nch):
                if c + 1 < nch:
                    s1a(c + 1)
                    s1b(c + 1)
                i = c % 2
                if want_rope:
                    stage2(c, hh_[i], pref + "h%d" % i, Cts[i], pref + "C%d" % i, Sts[i], pref + "S%d" % i)
                else:
                    stage2(c, hh_[i], pref + "h%d" % i, None, None, None, None)

        def work_tiles(es, n, pref):
            W = {"sq": sb(es, [128, n], BF16, pref + "sq"), "rs": sb(es, [128, n], F32, pref + "rs"),
                 "tmp": sb(es, [128, n], F32, pref + "tmp"), "xn": sb(es, [128, n], BF16, pref + "xn"),
                 "t1": sb(es, [128, n], F32, pref + "t1"), "t2": sb(es, [128, n], F32, pref + "t2")}
            return W

        def attn_epilogue(psO, psOn, bankB, n, W, wn, writer):
            cp("act", W["osb"][0:65, 0:n], psO[0:65, 0:n], [psOn], [wn + "osb"])
            mm(PB[bankB][0:64, 0:n], sel[0:65, 0:64], W["osb"][0:65, 0:n], True, True, ["cf", wn + "osb"], [PBN[bankB]])
            op("dve", lambda e: e.reciprocal(out=W["rinv"][0:64, 0:n], in_=PB[bankB][0:64, 0:n]), [PBN[bankB]], [wn + "rinv"])
            writer(W["osb"], W["rinv"])

        KmT = sb(top, [128, 4, NMEM], BF16, "KmT")
        Vm = sb(top, [128, 2, 512], BF16, "Vm")
        with scope() as es:
            wk = sb(es, [128, 8, 512], BF16, "wk")
            wv = sb(es, [128, 8, 512], BF16, "wv")
            with scope() as e2:
                load_w(e2, wk, m_wk, D, 512, 0, 0, "wk")
                load_w(e2, wv, m_wv, D, 512, 0, 0, "wv")
            ms = sb(es, [128, 8, NMEM], F32, "ms")
            mh = sb(es, [128, 8, NMEM], BF16, "mh")
            msq = sb(es, [128, 8, NMEM], BF16, "msq")
            W = work_tiles(es, NMEM, "mw")
            Sc.dma(ms[:], memT.rearrange("(k p) n -> p k n", p=128), writes=["ms"])
            norm_chunk(ms, "ms", NMEM, 16, mh, "mh", msq, "msq", 0, W["tmp"], "mwtmp", W["rs"], "mwrs")
            for hh in range(4):
                for k in range(8):
                    mm(PB[1][:, 0:NMEM], wk[:, k, hh * 128:(hh + 1) * 128], mh[:, k, :], k == 0, k == 7, ["wk", "mh"], ["pb1"])
                head_norm_rope(PB[1][:, 0:NMEM], "pb1", 128, NMEM, ones128, 128, gm[:, 41:42], None, None, None, None, None,
                               KmT[:, hh, :], "KmT", 2, W, "mw")
            for blk in range(2):
                for k in range(8):
                    mm(PB[3][:, 0:512], mh[:, k, blk * 128:(blk + 1) * 128], wv[:, k, :], k == 0, k == 7, ["mh", "wv"], ["pb3"])
                cp("act", Vm[:, blk, :], PB[3][:, 0:512], ["pb3"], ["Vm"])

        for hf in range(NH):
            tok0 = hf * T_H
            with scope() as pd:
                NBH = T_H // 128
                qdT = sb(pd, [128, NBH, 4, 128], BF16, "qdT")
                qiT = sb(pd, [128, NBH, 4, 128], BF16, "qiT")
                wabs = sb(pd, [128, NBH, 8], F32, "wabs")
                wsgn = sb(pd, [128, NBH, 8], F32, "wsgn")
                with scope() as es:
                    Wq = sb(es, [128, 8, 1032], BF16, "Wq")
                    for c4 in range(4):
                        with scope() as e2:
                            load_w(e2, Wq, w_in, D, 64, O_QD + c4 * 64, c4 * 128, "Wq")
                            load_w(e2, Wq, w_in, D, 64, O_QD + (4 + c4) * 64, c4 * 128 + 64, "Wq")
                    for q4 in range(2):
                        with scope() as e2:
                            load_w(e2, Wq, w_in, D, 256, O_QI + q4 * 256, 512 + q4 * 256, "Wq")
                    with scope() as e2:
                        load_w(e2, Wq, w_in, D, 8, O_WI, 1024, "Wq")
                    W = work_tiles(es, N, "bw")
                    wtok = sb(es, [128, NB, 8], F32, "wtok")

                    def q_stage2(c, h, hn, Ct, Ctn, St, Stn):
                        b0 = c * NB
                        for c4 in range(4):
                            for k in range(8):
                                mm(PB[1][:, 0:N], Wq[:, k, c4 * 128:(c4 + 1) * 128], h[:, k, :], k == 0, k == 7, ["Wq", hn], ["pb1"])
                            head_norm_rope(PB[1][:, 0:N], "pb1", 128, N, B64, 64, gs[:, 1:2], Ct[:, :], Ctn, St[:, :], Stn, P_part,
                                           qdT[:, b0:b0 + NB, c4, :], "qdT", 2, W, "bw")
                        for c4 in range(4):
                            for k in range(8):
                                mm(PB[3][:, 0:N], Wq[:, k, 512 + c4 * 128:512 + (c4 + 1) * 128], h[:, k, :], k == 0, k == 7, ["Wq", hn], ["pb3"])
                            head_norm_rope(PB[3][:, 0:N], "pb3", 128, N, None, 64, None, Ct[:, :], Ctn, St[:, :], Stn, P_part,
                                           qiT[:, b0:b0 + NB, c4, :], "qiT", 4, W, "bw")
                        for b in range(NB):
                            for k in range(8):
                                mm(PB[5][:, 0:8], h[:, k, b * 128:(b + 1) * 128], Wq[:, k, 1024:1032], k == 0, k == 7, [hn, "Wq"], ["pb5"])
                            cp("act", wtok[:, b, :], PB[5][:, 0:8], ["pb5"], ["wtok"])
                        act(wabs[:, b0:b0 + NB, :], wtok[:], AF.Abs, ["wtok"], ["wabs"], scale=(8 ** -0.5) * (64 ** -0.5))
                        ts("dve", wsgn[:, b0:b0 + NB, :], wtok[:], 0.0, ALU.is_ge, ["wtok"], ["wsgn"], s2=2.0, op1=ALU.mult)
                        ts("dve", wsgn[:, b0:b0 + NB, :], wsgn[:, b0:b0 + NB, :], -1.0, ALU.add, ["wsgn"], ["wsgn"])
                    chunk_pipeline(es, NCH_H, xT_own, pos_own, tok0, invf_part, 128, 0, "q", q_stage2)
                kdT = sb(pd, [128, S], BF16, "kdT")
                kiT_lo = sb(pd, [128, S], BF16, "kiTlo")
                kiT_hi = sb(pd, [128, S], BF16, "kiThi")
                op("pool", lambda e: e.memset(kiT_lo[64:128, :], 0.0), (), ["kiT"])
                op("pool", lambda e: e.memset(kiT_hi[0:64, :], 0.0), (), ["kiT"])
                vd = sb(pd, [128, NKB, 2, 65], BF16, "vd")
                op("pool", lambda e: e.memset(vd[:, :, :, 64:65], 1.0), (), ["vd"])
                with scope() as es:
                    Wk = sb(es, [128, 8, 384], BF16, "Wk1")
                    with scope() as e2:
                        load_w(e2, Wk, w_in, D, 128, O_KD, 0, "Wk1")
                        load_w(e2, Wk, w_in, D, 128, O_VD, 128, "Wk1")
                        load_w(e2, Wk, w_in, D, 64, O_KI, 256, "Wk1")
                        load_w(e2, Wk, w_in, D, 64, O_KI, 320, "Wk1")
                    W = work_tiles(es, N, "aw")

                    def a1_stage2(c, h, hn, Ct, Ctn, St, Stn):
                        t0 = c * N
                        for k in range(8):
                            mm(PB[1][:, 0:N], Wk[:, k, 0:128], h[:, k, :], k == 0, k == 7, ["Wk1", hn], ["pb1"])
                        head_norm_rope(PB[1][:, 0:N], "pb1", 128, N, B64, 64, gm[:, 38:39], Ct[:, :], Ctn, St[:, :], Stn, P_part,
                                       kdT[:, t0:t0 + N], "kdT", 2, W, "aw")
                        for k in range(8):
                            mm(PB[3][:, 0:N], Wk[:, k, 256:384], h[:, k, :], k == 0, k == 7, ["Wk1", hn], ["pb3"])
                        head_norm_rope(PB[3][:, 0:N], "pb3", 128, N, B64, 64, gm[:, 39:40], Ct[:, :], Ctn, St[:, :], Stn, P_part,
                                       (kiT_lo[0:64, t0:t0 + N], kiT_hi[64:128, t0:t0 + N]), "kiT", 4, W, "aw")
                        for b in range(N // 128):
                            kb = (t0 // 128) + b
                            for k in range(8):
                                mm(PB[5][:, 0:128], h[:, k, b * 128:(b + 1) * 128], Wk[:, k, 128:256], k == 0, k == 7, [hn, "Wk1"], ["pb5"])
                            cp("act", vd[:, kb, :, 0:64], PB[5][:, 0:128].rearrange("p (g d) -> p g d", g=2), ["pb5"], ["vd"])
                    chunk_pipeline(es, NCH_ALL, xT_all, pos_all, 0, invf_part, 128, 0, "a", a1_stage2)
                with scope() as es:
                    W = {}
                    W["osb"] = sb(es, [128, 512], F32, "bwosb")
                    W["rinv"] = sb(es, [128, 512], F32, "bwrinv")
                    Dg = sb(es, [128, 8, 128], BF16, "Dg")
                    Isb = sb(es, [128, S], F32, "Isb")
                    junk = sb(es, [128, S], mybir.dt.uint8, "junk")
                    Mall = sb(es, [128, S], BF16, "Mall")
                    Rh = [sb(es, [128, 512], BF16, "Rh%d" % i) for i in range(3)]
                    MT = [sb(es, [128, 4, 128], BF16, "MT%d" % i) for i in range(2)]
                    Eb = [sb(es, [128, 512], BF16, "Eb%d" % i) for i in range(3)]
                    bs = sb(es, [128, 8], F32, "bs")
                    qb0 = tok0 // 128

                    def stage_idx(b):
                        ext = 256 * (qb0 + b + 1)
                        for hh in range(8):
                            ts("dve", Dg[:, hh, :], ident, wsgn[:, b, hh:hh + 1], ALU.mult, ["cbf", "wsgn"], ["Dg"])
                        k0 = 0
                        while k0 < ext:
                            kw = min(512, ext - k0)

                            def ymm(hh, k0=k0, kw=kw):
                                kt = kiT_lo if hh % 2 == 0 else kiT_hi
                                mm(PB[hh % 2][:, 0:kw], qiT[:, b, hh // 2, :], kt[:, k0:k0 + kw],
                                   True, True, ["qiT", "kiT"], [PBN[hh % 2]])
                            ymm(0)
                            for hh in range(8):
                                if hh + 1 < 8:
                                    ymm(hh + 1)
                                r = Rh[hh % 3]
                                rn = "Rh%d" % (hh % 3)
                                if hh % 2 == 0:
                                    act(r[:, 0:kw], PB[hh % 2][:, 0:kw], AF.Relu, [PBN[hh % 2], "wabs"], [rn], scale=wabs[:, b, hh:hh + 1])
                                else:
                                    ts("dve", r[:, 0:kw], PB[hh % 2][:, 0:kw], 0.0, ALU.max, [PBN[hh % 2], "wabs"], [rn],
                                       s2=wabs[:, b, hh:hh + 1], op1=ALU.mult)
                                mm(PB[2][:, 0:kw], Dg[:, hh, :], r[:, 0:kw], hh == 0, hh == 7, ["Dg", rn], ["pb2"])
                            cp("act", Isb[:, k0:k0 + kw], PB[2][:, 0:kw], ["pb2"], ["Isb"])
                            k0 += kw

                    def stage_thr(b):
                        ext = 256 * (qb0 + b + 1)
                        op("dve", lambda e: e.tensor_reduce(out=bs[:, 5:6], in_=Isb[:, 0:ext], axis=AX.X, op=ALU.max, apply_absolute_value=True),
                           ["Isb"], ["bs"])
                        tt("dve", Isb[:, ext - 256:ext], Isb[:, ext - 256:ext], mI[:, :], ALU.add, ["Isb", "mI"], ["Isb"])
                        ts("dve", bs[:, 0:1], bs[:, 5:6], -1.0, ALU.mult, ["bs"], ["bs"], s2=-1.0, op1=ALU.add)
                        ts("dve", bs[:, 1:2], bs[:, 5:6], 2.0, ALU.mult, ["bs"], ["bs"], s2=2.0, op1=ALU.add)
                        for it in range(BISECT_ITERS):
                            cst = 2.0 ** -(it + 1)
                            stt(bs[:, 2:3], bs[:, 1:2], cst, bs[:, 0:1], ALU.mult, ALU.add, ["bs"], ["bs"])
                            ts("dve", junk[:, 0:ext], Isb[:, 0:ext], bs[:, 2:3], ALU.is_ge, ["Isb", "bs"], ["junk", "bs"],
                               op1=ALU.add, accum=bs[:, 3:4])
                            ts("dve", bs[:, 4:5], bs[:, 3:4], TOPK - 0.5, ALU.is_ge, ["bs"], ["bs"], s2=cst, op1=ALU.mult)
                            stt(bs[:, 0:1], bs[:, 4:5], bs[:, 1:2], bs[:, 0:1], ALU.mult, ALU.add, ["bs"], ["bs"])

                    def stage_mall(b):
                        ext = 256 * (qb0 + b + 1)
                        ts("dve", Mall[:, 0:ext], Isb[:, 0:ext], bs[:, 0:1], ALU.is_lt, ["Isb", "bs"], ["Mall"], s2=-30000.0, op1=ALU.mult)

                    def stage_attn(b):
                        ext = 256 * (qb0 + b + 1)
                        nkb = ext // 128
                        ngrp = (nkb + 3) // 4

                        def prep(gi):
                            kc = gi * 4
                            nb4 = min(4, nkb - kc)
                            mt = MT[gi % 2]
                            mtn = "MT%d" % (gi % 2)
                            for j in range(nb4):
                                op("pe", lambda e: e.transpose(out=PT[:, j * 128:(j + 1) * 128], in_=Mall[:, (kc + j) * 128:(kc + j + 1) * 128], identity=ident),
                                   ["Mall", "cbf"], ["pt"])
                            cp("act", mt[:, 0:nb4, :], PT[:, 0:nb4 * 128].rearrange("p (a q) -> p a q", a=nb4), ["pt"], [mtn])

                        units = [(kb, g) for kb in range(nkb) for g in range(2)]

                        def emit_S(u):
                            kb, g = units[u]
                            G = slice(g * 64, (g + 1) * 64)
                            bk = 3 + (u % 2)
                            mt = MT[(kb // 4) % 2]
                            mtn = "MT%d" % ((kb // 4) % 2)
                            mm(PB[bk][:, 0:512], kdT[G, kb * 128:(kb + 1) * 128], qdT[G, b, :, :].rearrange("p a q -> p (a q)"),
                               True, False, ["kdT", "qdT"], [PBN[bk]])
                            mm(PB[bk][:, 0:512].rearrange("p (a q) -> p a q", a=4), ident, mt[:, (kb % 4):(kb % 4) + 1, :].to_broadcast([128, 4, 128]),
                               False, True, ["cbf", mtn], [PBN[bk]])

                        def emit_rest(u):
                            kb, g = units[u]
                            bk = 3 + (u % 2)
                            u2 = u % 3
                            act(Eb[u2][:, :], PB[bk][:, 0:512], AF.Exp, [PBN[bk]], ["Eb%d" % u2])
                            mm(PB[5 + g][0:65, 0:512], vd[:, kb, g, :], Eb[u2][:, :], kb == 0, kb == nkb - 1, ["vd", "Eb%d" % u2], [PBN[5 + g]])

                        prep(0)
                        if ngrp > 1:
                            prep(1)
                        emit_S(0)
                        for u in range(len(units)):
                            if u + 1 < len(units):
                                emit_S(u + 1)
                            emit_rest(u)
                            kb, g = units[u]
                            if g == 1 and kb % 4 == 3 and (kb // 4) + 2 < ngrp:
                                prep(kb // 4 + 2)
                        for g in range(2):
                            def writer(osb, rinv, g=g, b=b):
                                for a in range(4):
                                    hd = g * 4 + a
                                    col = b * 128
                                    tt("dve", mixD[(hd % 2) * 64:(hd % 2) * 64 + 64, hd // 2, col:col + 128],
                                       osb[0:64, a * 128:(a + 1) * 128], rinv[0:64, a * 128:(a + 1) * 128], ALU.mult,
                                       ["bwosb", "bwrinv"], ["mixD"])
                            attn_epilogue(PB[5 + g], PBN[5 + g], 3, 512, W, "bw", writer)

                    stage_idx(0)
                    stage_thr(0)
                    stage_mall(0)
                    for b in range(NBH):
                        if b + 1 < NBH:
                            stage_idx(b + 1)
                            stage_thr(b + 1)
                        stage_attn(b)
                        if b + 1 < NBH:
                            stage_mall(b + 1)

            with scope() as pmd:
                mixM = sb(pmd, [128, 4, T_H], BF16, "mixM")
                mT = sb(pmd, [128, 2 * NB, N], BF16, "mT")
                with scope() as es:
                    st2 = sb(es, [128, 2 * NB * N], F32, "cst2")
                    Sc.dma(st2[:], mT_d[:, :], writes=["cst2"])
                    cp("dve", mT[:].rearrange("p a n -> p (a n)"), st2[:], ["cst2"], ["mT"])
                with scope() as pm:
                    ckvnT = sb(pm, [128, S], BF16, "ckvnT")
                    kpeX = sb(pm, [128, S], BF16, "kpeX")
                    cqnT = sb(pm, [128, 2, T_H], BF16, "cqnT")
                    tabC = sb(pm, [128, T_H], BF16, "tabC")
                    tabS = sb(pm, [128, T_H], BF16, "tabS")
                    with scope() as es:
                        Wk = sb(es, [128, 8, 256], BF16, "Wk2")
                        with scope() as e2:
                            load_w(e2, Wk, w_in, D, 128, O_CKV, 0, "Wk2")
                            load_w(e2, Wk, w_in, D, 32, O_KPE, 128, "Wk2")
                            load_w(e2, Wk, w_in, D, 32, O_KPE, 192, "Wk2")
                        W = work_tiles(es, N, "cw")
                        op("pool", lambda e: e.memset(Wk[:, :, 160:192], 0.0), (), ["Wk2"])
                        op("pool", lambda e: e.memset(Wk[:, :, 224:256], 0.0), (), ["Wk2"])

                        def a2_stage2(c, h, hn, Ct, Ctn, St, Stn):
                            t0 = c * N
                            for k in range(8):
                                mm(PB[1][:, 0:N], Wk[:, k, 0:128], h[:, k, :], k == 0, k == 7, ["Wk2", hn], ["pb1"])
                            head_norm_rope(PB[1][:, 0:N], "pb1", 128, N, ones128, 128, gm[:, 34:35], None, None, None, None, None,
                                           ckvnT[:, t0:t0 + N], "ckvnT", 2, W, "cw")
                            for k in range(8):
                                mm(PB[3][:, 0:N], Wk[:, k, 128:256], h[:, k, :], k == 0, k == 7, ["Wk2", hn], ["pb3"])
                            act(kpeX[0:32, t0:t0 + N], PB[3][0:32, 0:N], AF.Square, ["pb3"], ["kpeX"])
                            R = slice(64, 96)
                            ts("dve", W["xn"][R, 0:N], PB[3][R, 0:N], gm[R, 36:37], ALU.mult, ["pb3", "gm"], ["cwxn"])
                            op("pool", lambda e: e.memset(W["xn"][0:64, 0:N], 0.0), (), ["cwxn"])
                            mm(PB[4][0:96, 0:N], P_mla[0:96, 0:96], W["xn"][0:96, 0:N], True, True, ["cbf", "cwxn"], ["pb4"])
                            tt("pool", W["t1"][R, 0:N], W["xn"][R, 0:N], Ct[R, :], ALU.mult, ["cwxn", Ctn], ["cwt1"])
                            tt("dve", W["t2"][R, 0:N], PB[4][R, 0:N], St[R, :], ALU.mult, ["pb4", Stn], ["cwt2"])
                            tt("dve", kpeX[R, t0:t0 + N], W["t1"][R, 0:N], W["t2"][R, 0:N], ALU.add, ["cwt1", "cwt2"], ["kpeX"])
                        chunk_pipeline(es, NCH_ALL, xT_all, pos_all, 0, invf_mla, 96, 0, "c", a2_stage2)
                    with scope() as es:
                        Wc = sb(es, [128, 8, 256], BF16, "Wc")
                        with scope() as e2:
                            load_w(e2, Wc, w_in, D, 256, O_CQ, 0, "Wc")
                        W = work_tiles(es, N, "dw")
                        cqr = sb(es, [128, 2, N], F32, "cqr")
                        cqs = sb(es, [128, 2, N], BF16, "cqs")

                        def l_stage2(c, h, hn, Cf, Cfn, Sf, Sfn):
                            cp("pool", tabC[0:96, c * N:(c + 1) * N], Cf[0:96, :], [Cfn], ["tabC"])
                            cp("pool", tabS[0:96, c * N:(c + 1) * N], Sf[0:96, :], [Sfn], ["tabS"])
                            for r2 in range(2):
                                for k in range(8):
                                    mm(PB[1][:, 0:N], Wc[:, k, r2 * 128:(r2 + 1) * 128], h[:, k, :], k == 0, k == 7, ["Wc", hn], ["pb1"])
                                cp("act", cqr[:, r2, :], PB[1][:, 0:N], ["pb1"], ["cqr"])
                            act(cqs[:, :, :], cqr[:, :, :], AF.Square, ["cqr"], ["cqs"])
                            for r2 in range(2):
                                mm(PB[2][:, 0:N], ones128, cqs[:, r2, :], r2 == 0, r2 == 1, ["cbf", "cqs"], ["pb2"])
                            rstd_from_ps(PB[2], "pb2", 128, N, 256.0, W["rs"], "dwrs", W["tmp"], "dwtmp")
                            for r2 in range(2):
                                stt(cqnT[:, r2, c * N:(c + 1) * N], cqr[:, r2, :], gm[:, 32 + r2:33 + r2], W["rs"][:, 0:N], ALU.mult, ALU.mult,
                                    ["cqr", "gm", "dwrs"], ["cqnT"])
                        chunk_pipeline(es, NCH_H, xT_own, pos_own, tok0, invf_mla, 96, 0, "l", l_stage2)
                    with scope() as es:
                        wuq = sb(es, [128, 2, 768], BF16, "wuq")
                        wukv = sb(es, [128, 1, 1024], BF16, "wukv")
                        with scope() as e2:
                            load_w(e2, wuq, w_uq, 256, 768, 0, 0, "wuq")
                            load_w(e2, wukv, w_ukv, 128, 1024, 0, 0, "wukv")
                        KhT = sb(es, [128, S], BF16, "KhT")
                        Vh = sb(es, [128, NKB, 65], BF16, "Vh")
                        QhT = sb(es, [128, T_H], BF16, "QhT")
                        W = work_tiles(es, N, "ew")
                        W["osb"] = sb(es, [128, 512], F32, "ewosb")
                        W["rinv"] = sb(es, [128, 512], F32, "ewrinv")
                        Eb = [sb(es, [128, N], BF16, "mEb%d" % i) for i in range(3)]
                        op("pool", lambda e: e.memset(Vh[:, :, 64:65], 1.0), (), ["Vh"])
                        for hd in range(8):
                            for c in range(NCH_ALL):
                                t0 = c * N
                                mm(PB[0][0:64, 0:N], wukv[:, 0, hd * 128:hd * 128 + 64], ckvnT[:, t0:t0 + N], True, True, ["wukv", "ckvnT"], ["pb0"])
                                act(W["sq"][0:64, 0:N], PB[0][0:64, 0:N], AF.Square, ["pb0"], ["ewsq"])
                                mm(PB[1][0:96, 0:N], O96[0:64, 0:96], W["sq"][0:64, 0:N], True, False, ["cbf", "ewsq"], ["pb1"])
                                mm(PB[1][0:96, 0:N], O96[0:32, 0:96], kpeX[0:32, t0:t0 + N], False, True, ["cbf", "kpeX"], ["pb1"])
                                rstd_from_ps(PB[1], "pb1", 96, N, 96.0, W["rs"], "ewrs", W["tmp"], "ewtmp")
                                stt(KhT[0:64, t0:t0 + N], PB[0][0:64, 0:N], gm[0:64, 36:37], W["rs"][0:64, 0:N], ALU.mult, ALU.mult,
                                    ["pb0", "gm", "ewrs"], ["KhT"])
                                tt("pool", KhT[64:96, t0:t0 + N], kpeX[64:96, t0:t0 + N], W["rs"][64:96, 0:N], ALU.mult, ["kpeX", "ewrs"], ["KhT"])
                            for kb0 in range(0, NKB, 8):
                                for j in range(8):
                                    kb = kb0 + j
                                    mm(PB[2][:, j * 64:(j + 1) * 64], ckvnT[:, kb * 128:(kb + 1) * 128], wukv[:, 0, hd * 128 + 64:hd * 128 + 128],
                                       True, True, ["ckvnT", "wukv"], ["pb2"])
                                cp("act", Vh[:, kb0:kb0 + 8, 0:64], PB[2][:, 0:512].rearrange("p (a d) -> p a d", a=8), ["pb2"], ["Vh"])
                            for c in range(NCH_H):
                                for r2 in range(2):
                                    mm(PB[0][0:96, 0:N], wuq[:, r2, hd * 96:(hd + 1) * 96], cqnT[:, r2, c * N:(c + 1) * N], r2 == 0, r2 == 1,
                                       ["wuq", "cqnT"], ["pb0"])
                                head_norm_rope(PB[0][0:96, 0:N], "pb0", 96, N, O96, 96, gs[0:96, 0:1], tabC[0:96, c * N:(c + 1) * N], "tabC",
                                               tabS[0:96, c * N:(c + 1) * N], "tabS", P_mla, QhT[0:96, c * N:(c + 1) * N], "QhT", 1, W, "ew")
                            for c in range(NCH_H):
                                j = (tok0 // N) + c
                                ext = 2 * NB * (j + 1)
                                bo = 3 + (c % 2)
                                def emit_S(kb, c=c, j=j):
                                    bs_ = 5 + (kb % 2)
                                    dg = kb >= 2 * NB * j
                                    mm(PB[bs_][:, 0:N], KhT[0:96, kb * 128:(kb + 1) * 128], QhT[0:96, c * N:(c + 1) * N], True, not dg, ["KhT", "QhT"], [PBN[bs_]])
                                    if dg:
                                        mm(PB[bs_][:, 0:N], ident, mT[:, kb - 2 * NB * j, :], False, True, ["cbf", "mT"], [PBN[bs_]])

                                def emit_rest(kb, ext=ext, bo=bo):
                                    bs_ = 5 + (kb % 2)
                                    e_ = Eb[kb % 3]
                                    en = "mEb%d" % (kb % 3)
                                    act(e_[:, :], PB[bs_][:, 0:N], AF.Exp, [PBN[bs_]], [en])
                                    mm(PB[bo][0:65, 0:N], Vh[:, kb, :], e_[:, :], kb == 0, kb == ext - 1, ["Vh", en], [PBN[bo]])
                                emit_S(0)
                                for kb in range(ext):
                                    if kb + 1 < ext:
                                        emit_S(kb + 1)
                                    emit_rest(kb)

                                def writer(osb, rinv, hd=hd, c=c):
                                    tt("dve", mixM[(hd % 2) * 64:(hd % 2) * 64 + 64, hd // 2, c * N:(c + 1) * N], osb[0:64, 0:N], rinv[0:64, 0:N],
                                       ALU.mult, ["ewosb", "ewrinv"], ["mixM"])
                                attn_epilogue(PB[bo], PBN[bo], 2, N, W, "ew", writer)

                if dbg:
                    with scope() as es:
                        mf = sb(es, [128, 8, T_H], F32, "mf")
                        cp("dve", mf[:, 0:4, :], mixM[:], ["mixM"], ["mf"])
                        cp("dve", mf[:, 4:8, :], mixD[:], ["mixD"], ["mf"])
                        Sc.dma(dbg_mixed[:, tok0:tok0 + T_H].rearrange("(k p) n -> p k n", p=128), mf[:], reads=["mf"], is_out=True)

                with scope() as es:
                    wo_ = sb(es, [128, 8, D], BF16, "wo_")
                    wq_ = sb(es, [128, 8, 512], BF16, "wq_")
                    wmo = sb(es, [128, 4, D], BF16, "wmo")
                    with scope() as e2:
                        for q4 in range(4):
                            with scope() as e3:
                                load_w(e3, wo_, w_out, D, 256, q4 * 256, q4 * 256, "wo_")
                        load_w(e2, wq_, m_wq, D, 512, 0, 0, "wq_")
                    with scope() as e2:
                        load_w(e2, wmo, m_wo, 512, D, 0, 0, "wmo")
                    x1 = sb(es, [128, 8, N], F32, "x1")
                    h = sb(es, [128, 8, N], BF16, "h4")
                    W = work_tiles(es, N, "fw")
                    om = sb(es, [128, 4, N], BF16, "om")
                    Eb = [sb(es, [128, N], BF16, "cEb%d" % i) for i in range(2)]
                    actT = sb(es, [128, DFF // 128, N], BF16, "actT")
                    stg = [sb(es, [128, 2048], F32, "fstg%d" % i) for i in range(4)]
                    wsl = [sb(es, [128, 2048], BF16, "fws%d" % i) for i in range(4)]
                    sg = sb(es, [128, N], F32, "sg")
                    nld = [0]

                    def load_slab(src_ap, a_, b_):
                        i = nld[0]
                        nld[0] += 1
                        sn = "fstg%d" % (i % 4)
                        wn_ = "fws%d" % (i % 4)
                        s_ = stg[i % 4][:, 0:a_ * b_].rearrange("p (a b) -> p a b", a=a_)
                        w_ = wsl[i % 4][:, 0:a_ * b_].rearrange("p (a b) -> p a b", a=a_)
                        Sc.dma(s_, src_ap, writes=[sn])
                        cp(("pool", "dve", "act")[i % 3], w_, s_, [sn], [wn_])
                        return w_, wn_

                    sq = actT[:, 0:8, :]
                    for c in range(NCH_H):
                        lt0 = tok0 + c * N
                        Sc.dma(x1[:], xT_own[:, lt0:lt0 + N].rearrange("(k p) n -> p k n", p=128), writes=["x1"])
                        for m in range(8):
                            bk = m % 2
                            for k in range(8):
                                mm(PB[bk][:, 0:N], wo_[:, k, m * 128:(m + 1) * 128], (mixM if k < 4 else mixD)[:, k % 4, c * N:(c + 1) * N], k == 0, k == 7, ["wo_", "mixM", "mixD"], [PBN[bk]])
                            tt("dve", x1[:, m, :], x1[:, m, :], PB[bk][:, 0:N], ALU.add, ["x1", PBN[bk]], ["x1"])
                        norm_chunk(x1, "x1", N, 8, h, "h4", sq, "actT", 2, W["tmp"], "fwtmp", W["rs"], "fwrs")
                        for hh in range(4):
                            for k in range(8):
                                mm(PB[3][:, 0:N], wq_[:, k, hh * 128:(hh + 1) * 128], h[:, k, :], k == 0, k == 7, ["wq_", "h4"], ["pb3"])
                            head_norm_rope(PB[3][:, 0:N], "pb3", 128, N, ones128, 128, gs[:, 2:3], None, None, None, None, None,
                                           W["xn"][:, 0:N], "fwxn2", 4, W, "fw")
                            for blk in range(2):
                                mm(PB[5][:, 0:N], KmT[:, hh, blk * 128:(blk + 1) * 128], W["xn"][:, 0:N], True, True, ["KmT", "fwxn2"], ["pb5"])
                                act(Eb[blk][:, :], PB[5][:, 0:N], AF.Exp, ["pb5"], ["cEb%d" % blk])
                            for blk in range(2):
                                mm(PB[6][:, 0:N], Vm[:, blk, hh * 128:(hh + 1) * 128], Eb[blk][:, :], blk == 0, blk == 1, ["Vm", "cEb%d" % blk], ["pb6"])
                            for blk in range(2):
                                mm(PB[4][:, 0:N], ones128, Eb[blk][:, :], blk == 0, blk == 1, ["cbf", "cEb%d" % blk], ["pb4"])
                            op("dve", lambda e: e.reciprocal(out=W["t1"][:, 0:N], in_=PB[4][:, 0:N]), ["pb4"], ["fwt1"])
                            tt("dve", om[:, hh, :], PB[6][:, 0:N], W["t1"][:, 0:N], ALU.mult, ["pb6", "fwt1"], ["om"])
                        for m in range(8):
                            bk = m % 2
                            for hh in range(4):
                                mm(PB[bk][:, 0:N], wmo[:, hh, m * 128:(m + 1) * 128], om[:, hh, :], hh == 0, hh == 3, ["wmo", "om"], [PBN[bk]])
                            tt("dve", x1[:, m, :], x1[:, m, :], PB[bk][:, 0:N], ALU.add, ["x1", PBN[bk]], ["x1"])
                        norm_chunk(x1, "x1", N, 24, h, "h4", sq, "actT", 2, W["tmp"], "fwtmp", W["rs"], "fwrs")
                        slabs = [(c0, 256) for c0 in range(0, DFF, 256)]
                        for (c0, cw) in slabs:
                            wg, wgn = load_slab(f_wg[:, c0:c0 + cw].rearrange("(k p) c -> p k c", p=128), 8, cw)
                            wu, wun = load_slab(f_wu[:, c0:c0 + cw].rearrange("(k p) c -> p k c", p=128), 8, cw)
                            for f in range(cw // 128):
                                ff = c0 // 128 + f
                                bg = 3 + (ff % 2)
                                bu = 5 + (ff % 2)
                                for k in range(8):
                                    mm(PB[bg][:, 0:N], wg[:, k, f * 128:(f + 1) * 128], h[:, k, :], k == 0, k == 7, [wgn, "h4"], [PBN[bg]])
                                for k in range(8):
                                    mm(PB[bu][:, 0:N], wu[:, k, f * 128:(f + 1) * 128], h[:, k, :], k == 0, k == 7, [wun, "h4"], [PBN[bu]])
                                act(sg[:, :], PB[bg][:, 0:N], AF.Silu, [PBN[bg]], ["sg"])
                                tt("dve", actT[:, ff, :], sg[:, :], PB[bu][:, 0:N], ALU.mult, ["sg", PBN[bu]], ["actT"])
                        for grp in ((0, 1, 2, 3), (4, 5, 6, 7)):
                            rslabs = [(r0, 256) for r0 in range(0, DFF, 256)]
                            for (r0, rw) in rslabs:
                                nf = rw // 128
                                wd, wdn = load_slab(f_wd[r0:r0 + rw, grp[0] * 128:grp[0] * 128 + 512].rearrange("(f p) c -> p f c", p=128), nf, 512)
                                for f in range(nf):
                                    ff = r0 // 128 + f
                                    for mi, m in enumerate(grp):
                                        mm(PB[mi][:, 0:N], wd[:, f, mi * 128:(mi + 1) * 128], actT[:, ff, :], ff == 0, ff == DFF // 128 - 1, [wdn, "actT"], [PBN[mi]])
                            for mi, m in enumerate(grp):
                                tt("dve", x1[:, m, :], x1[:, m, :], PB[mi][:, 0:N], ALU.add, ["x1", PBN[mi]], ["x1"])
                        Sc.dma(outT[:, lt0:lt0 + N].rearrange("(k p) n -> p k n", p=128), x1[:], reads=["x1"], is_out=True)
        Sc.finish()
    return nc, Sc


def _consts(S, NH, half):
    T_OWN = S // 2
    T_H = T_OWN // NH
    N = min(512, T_H)
    NB = N // 128
    cb = np.zeros((128, 6 * 128), np.float32)
    cb[:, 0:128] = 1.0
    cb[0:64, 128:192] = 1.0
    cb[64:128, 192:256] = 1.0
    cb[0:96, 256:352] = 1.0
    Pp = np.zeros((128, 128), np.float32)
    for base in (0, 64):
        for i in range(8):
            Pp[base + 8 + i, base + i] = -1.0
            Pp[base + i, base + 8 + i] = 1.0
    cb[:, 384:512] = Pp
    Pm = np.zeros((128, 128), np.float32)
    for i in range(16):
        Pm[80 + i, 64 + i] = -1.0
        Pm[64 + i, 80 + i] = 1.0
    cb[:, 512:640] = Pm
    cb[:, 640:768] = np.eye(128, dtype=np.float32)
    cf = np.zeros((128, 66), np.float32)
    cf[64, 0:64] = 1.0
    f8 = (THETA ** (-np.arange(0, 16, 2, dtype=np.float32) / np.float32(16))).astype(np.float32)
    f16 = (THETA ** (-np.arange(0, 32, 2, dtype=np.float32) / np.float32(32))).astype(np.float32)
    for base in (0, 64):
        cf[base:base + 8, 64] = f8
        cf[base + 8:base + 16, 64] = f8
    cf[64:80, 65] = f16
    cf[80:96, 65] = f16
    mI = np.zeros((128, 256), np.float32)
    q = np.arange(128)[:, None]
    k = np.arange(256)[None, :]
    qpos = half * 128 + q
    mI[:] = np.where(k <= qpos, 0.0, NEG)
    mT = np.zeros((128, 2 * NB, N), np.float32)
    p = np.arange(128)[:, None]
    for kbp in range(2 * NB):
        for m in range(NB):
            r = np.arange(128)[None, :]
            gb = 2 * m + half
            if kbp < gb:
                v = np.zeros((128, 128), np.float32)
            elif kbp == gb:
                v = np.where(p <= r, 0.0, -30000.0).astype(np.float32)
            else:
                v = np.full((128, 128), -30000.0, np.float32)
            mT[:, kbp, m * 128:(m + 1) * 128] = v
    return cb, cf, mI, mT.reshape(128, 2 * NB * N)


def _gains(inp):
    gm = np.zeros((128, 42), np.float32)

    def col8(v, c0):
        gm[:, c0:c0 + 8] = np.asarray(v, np.float32).reshape(8, 128).T
    col8(inp["norm_mix"][0], 0)
    col8(inp["norm_mem_x"][0], 8)
    col8(inp["norm_mem_kv"][0], 16)
    col8(inp["norm_ffn"][0], 24)
    gm[:, 32:34] = np.asarray(inp["mla_q_a_norm"][0], np.float32).reshape(2, 128).T
    gm[:, 34] = inp["mla_kv_a_norm"][0]
    gm[0:96, 35] = inp["mla_q_norm"][0]
    gm[0:96, 36] = inp["mla_k_norm"][0]
    gm[:, 37] = np.tile(np.asarray(inp["dsa_q_norm"][0], np.float32), 2)
    gm[:, 38] = np.tile(np.asarray(inp["dsa_k_norm"][0], np.float32), 2)
    gm[:, 39] = np.tile(np.asarray(inp["idx_k_norm"][0], np.float32), 2)
    gm[:, 40] = inp["mem_q_norm"][0]
    gm[:, 41] = inp["mem_k_norm"][0]
    return gm


_CACHE = {}


def run(inputs, S, NH, dbg=False):
    inp = {k: np.asarray(v) for k, v in inputs.items()}
    B = inp["x"].shape[0]
    ncores = 2 * B
    key = (S, NH, dbg)
    if key not in _CACHE:
        _CACHE[key] = build(S, NH, dbg)
    nc, Sc = _CACHE[key]
    gm = _gains(inp)
    nblk = S // 128
    in_maps = []
    for c in range(ncores):
        b, half = c // 2, c % 2
        own_blocks = np.arange(half, nblk, 2)
        own_idx = (own_blocks[:, None] * 128 + np.arange(128)[None, :]).reshape(-1)
        xb = np.asarray(inp["x"][b], np.float32)
        cb, cf, mI, mT = _consts(S, NH, half)
        pos = np.asarray(inp["positions"][b], np.int32)
        in_maps.append({
            "xT_all": np.ascontiguousarray(xb.T),
            "xT_own": np.ascontiguousarray(xb[own_idx].T),
            "pos_all": np.ascontiguousarray(pos.reshape(1, S)),
            "pos_own": np.ascontiguousarray(pos[own_idx].reshape(1, S // 2)),
            "memT": np.ascontiguousarray(np.asarray(inp["mem"][b], np.float32).T),
            "w_in": np.ascontiguousarray(inp["w_in"][0], dtype=np.float32),
            "w_uq": np.ascontiguousarray(inp["mla_w_uq"][0], dtype=np.float32),
            "w_ukv": np.ascontiguousarray(inp["mla_w_ukv"][0], dtype=np.float32),
            "w_out": np.ascontiguousarray(inp["w_out"][0], dtype=np.float32),
            "m_wq": np.ascontiguousarray(inp["mem_w_q"][0], dtype=np.float32),
            "m_wk": np.ascontiguousarray(inp["mem_w_k"][0], dtype=np.float32),
            "m_wv": np.ascontiguousarray(inp["mem_w_v"][0], dtype=np.float32),
            "m_wo": np.ascontiguousarray(inp["mem_w_o"][0], dtype=np.float32),
            "f_wg": np.ascontiguousarray(inp["ffn_w_gate"][0], dtype=np.float32),
            "f_wu": np.ascontiguousarray(inp["ffn_w_up"][0], dtype=np.float32),
            "f_wd": np.ascontiguousarray(inp["ffn_w_down"][0], dtype=np.float32),
            "gm": gm, "cb": cb, "cf": cf, "mI": mI, "mT": mT,
        })
    res = run_bass_kernel_spmd(nc, in_maps, core_ids=list(range(ncores)))
    out = np.zeros((B, S, D), np.float32)
    extra = {}
    for c in range(ncores):
        b, half = c // 2, c % 2
        own_blocks = np.arange(half, nblk, 2)
        own_idx = (own_blocks[:, None] * 128 + np.arange(128)[None, :]).reshape(-1)
        out[b, own_idx, :] = np.asarray(res.results[c]["outT"]).T
        if dbg:
            extra[c] = (own_idx, np.asarray(res.results[c]["dbg_mixed"]).T)
    return out, extra


def kernel(**inputs):
    out, _ = run(inputs, 8192, 2)
    return out
```

```python
import math
from contextlib import ExitStack

import numpy as np
import concourse.bass as bass
import concourse.mybir as mybir
from concourse.bass_utils import run_bass_kernel_spmd

F32 = mybir.dt.float32
BF16 = mybir.dt.bfloat16
I32 = mybir.dt.int32
AF = mybir.ActivationFunctionType
ALU = mybir.AluOpType
AX = mybir.AxisListType

D = 1024
NMEM = 256
DFF = 2816
EPS = 1e-6
TOPK = 256
BISECT_ITERS = 20
NEG = -1.0e30
O_CQ, O_CKV, O_KPE, O_QD, O_KD, O_VD, O_QI, O_KI, O_WI = 0, 256, 384, 416, 928, 1056, 1184, 1696, 1760
THETA = 500000.0


class Sched:
    def __init__(self, nc):
        self.nc = nc
        self.engs = {"pe": nc.tensor, "act": nc.scalar, "dve": nc.vector, "pool": nc.gpsimd, "sp": nc.sync}
        self.sems = {}
        self.cnt = {}
        self.seen = {k: {} for k in self.engs}
        self.lastw = {}
        self.readers = {}
        self._stack = []
        self.ninst = 0
        self.nwait = 0
        for k in ["pe", "act", "dve", "pool"]:
            self.sems[k] = self.sem("s_" + k)
            self.cnt[k] = 0
        self.dsems = [[self.sem("d%d" % i), 0] for i in range(24)]
        self.dnext = 0
        self.out_events = []

    def sem(self, name):
        cm = self.nc.semaphore(name)
        s = cm.__enter__()
        self._stack.append(cm)
        return s

    def _wait(self, e, ev):
        if ev is None:
            return
        s, v = ev
        if e == "pe" and s is self.sems["pe"]:
            return
        key = id(s)
        if self.seen[e].get(key, 0) >= v:
            return
        self.engs[e].wait_ge(s, v)
        self.seen[e][key] = v
        self.nwait += 1

    def deps(self, e, reads, writes):
        for r in reads:
            self._wait(e, self.lastw.get(r))
        for w in writes:
            self._wait(e, self.lastw.get(w))
            for ev in self.readers.get(w, {}).values():
                self._wait(e, ev)

    def record(self, ev, reads, writes):
        for r in reads:
            d = self.readers.setdefault(r, {})
            d[id(ev[0])] = ev
        for w in writes:
            self.lastw[w] = ev
            self.readers[w] = {}

    def op(self, e, fn, reads=(), writes=()):
        self.deps(e, reads, writes)
        inst = fn(self.engs[e])
        self.cnt[e] += 1
        inst.then_inc(self.sems[e], 1)
        ev = (self.sems[e], self.cnt[e])
        self.record(ev, reads, writes)
        self.ninst += 1
        return ev

    def dma(self, out, in_, reads=(), writes=(), is_out=False):
        q = "sp"
        ds = self.dsems[self.dnext % len(self.dsems)]
        self.dnext += 1
        if ds[1] > 0:
            self._wait(q, (ds[0], ds[1]))
        self.deps(q, reads, writes)
        inst = self.engs[q].dma_start(out=out, in_=in_)
        ds[1] += 16
        inst.then_inc(ds[0], 16)
        ev = (ds[0], ds[1])
        self.record(ev, reads, writes)
        self.ninst += 1
        if is_out:
            self.out_events.append(ev)
        return ev

    def barrier(self):
        evs = [(self.sems[k], self.cnt[k]) for k in ["pe", "act", "dve", "pool"] if self.cnt[k] > 0]
        evs += [(d[0], d[1]) for d in self.dsems if d[1] > 0]
        for e in ["pe", "act", "dve", "pool", "sp"]:
            for ev in evs:
                self._wait(e, ev)

    def finish(self):
        for ev in self.out_events:
            self._wait("sp", ev)
        for d in self.dsems:
            if d[1] > 0:
                self._wait("sp", (d[0], d[1]))
        for k in ["pe", "act", "dve", "pool"]:
            if self.cnt[k] > 0:
                self._wait("sp", (self.sems[k], self.cnt[k]))
        for cm in reversed(self._stack):
            cm.__exit__(None, None, None)


def build(S, NH, dbg=False):
    T_OWN = S // 2
    T_H = T_OWN // NH
    N = min(512, T_H)
    NB = N // 128
    NCH_ALL = S // N
    NCH_H = T_H // N
    NKB = S // 128
    nc = bass.Bass("TRN2", target_bir_lowering=False)

    def dram(name, shape, dt=F32, kind="ExternalInput"):
        return nc.dram_tensor(name, shape, dt, kind=kind).ap()

    xT_all = dram("xT_all", [D, S])
    xT_own = dram("xT_own", [D, T_OWN])
    pos_all = dram("pos_all", [1, S], I32)
    pos_own = dram("pos_own", [1, T_OWN], I32)
    memT = dram("memT", [D, NMEM])
    w_in = dram("w_in", [D, 1768])
    w_uq = dram("w_uq", [256, 768])
    w_ukv = dram("w_ukv", [128, 1024])
    w_out = dram("w_out", [D, D])
    m_wq = dram("m_wq", [D, 512])
    m_wk = dram("m_wk", [D, 512])
    m_wv = dram("m_wv", [D, 512])
    m_wo = dram("m_wo", [512, D])
    f_wg = dram("f_wg", [D, DFF])
    f_wu = dram("f_wu", [D, DFF])
    f_wd = dram("f_wd", [DFF, D])
    gm_d = dram("gm", [128, 42])
    cb_d = dram("cb", [128, 6 * 128])
    cf_d = dram("cf", [128, 66])
    mI_d = dram("mI", [128, 256])
    mT_d = dram("mT", [128, 2 * NB * N])
    outT = dram("outT", [D, T_OWN], kind="ExternalOutput")
    if dbg:
        dbg_mixed = dram("dbg_mixed", [D, T_OWN], kind="ExternalOutput")

    Sc = Sched(nc)
    uid = [0]

    def nm(p):
        uid[0] += 1
        return "%s_%d" % (p, uid[0])

    def sb(es, shape, dt, name="t"):
        return es.enter_context(nc.sbuf_tensor(nm(name), shape, dt))

    def ps(es, shape, dt, name="p"):
        return es.enter_context(nc.psum_tensor(nm(name), shape, dt))

    op = Sc.op

    from contextlib import contextmanager

    @contextmanager
    def scope():
        with ExitStack() as es_:
            yield es_
        Sc.barrier()

    def mm(out, lhsT, rhs, start, stop, reads, writes):
        return op("pe", lambda e: e.matmul(out, lhsT=lhsT, rhs=rhs, start=start, stop=stop), reads, writes)

    def act(out, in_, func, reads, writes, scale=None, bias=None):
        kw = {}
        if scale is not None:
            kw["scale"] = scale
        if bias is not None:
            kw["bias"] = bias
        return op("act", lambda e: e.activation(out=out, in_=in_, func=func, **kw), reads, writes)

    def ts(eng, out, in0, s1, op0, reads, writes, s2=None, op1=None, accum=None):
        kw = {}
        if op1 is not None:
            kw["op1"] = op1
        if accum is not None:
            kw["accum_out"] = accum
        return op(eng, lambda e: e.tensor_scalar(out=out, in0=in0, scalar1=s1, scalar2=s2, op0=op0, **kw), reads, writes)

    def tt(eng, out, in0, in1, o, reads, writes):
        return op(eng, lambda e: e.tensor_tensor(out=out, in0=in0, in1=in1, op=o), reads, writes)

    def stt(out, in0, scalar, in1, op0, op1, reads, writes):
        return op("dve", lambda e: e.scalar_tensor_tensor(out=out, in0=in0, scalar=scalar, in1=in1, op0=op0, op1=op1), reads, writes)

    def cp(eng, out, in_, reads, writes):
        if eng == "act":
            return act(out, in_, AF.Copy, reads, writes)
        return op(eng, lambda e: e.tensor_copy(out=out, in_=in_), reads, writes)

    with ExitStack() as top:
        gm = sb(top, [128, 42], F32, "gm")
        cf = sb(top, [128, 66], F32, "cf")
        cbf = sb(top, [128, 6 * 128], BF16, "cbf")
        gs = sb(top, [128, 4], F32, "gs")
        epsc = sb(top, [128, 1], F32, "epsc")
        shc = sb(top, [128, 2], F32, "shc")
        mI = sb(top, [128, 256], F32, "mI")
        mixD = sb(top, [128, 4, T_H], BF16, "mixD")
        with scope() as es:
            st = sb(es, [128, 6 * 128], F32, "cst")
            Sc.dma(st[:], cb_d[:, :], writes=["cst"])
            cp("dve", cbf[:], st[:], ["cst"], ["cbf"])
        Sc.dma(gm[:], gm_d[:, :], writes=["gm"])
        Sc.dma(cf[:], cf_d[:, :], writes=["cf"])
        Sc.dma(mI[:], mI_d[:, :], writes=["mI"])
        op("dve", lambda e: e.memset(epsc[:], EPS), (), ["epsc"])
        op("dve", lambda e: e.memset(shc[:, 0:1], 0.0), (), ["shc"])
        op("dve", lambda e: e.memset(shc[:, 1:2], math.pi / 2), (), ["shc"])
        ts("dve", gs[:, 0:1], gm[:, 35:36], 96 ** -0.5, ALU.mult, ["gm"], ["gs"])
        ts("dve", gs[:, 1:2], gm[:, 37:38], 64 ** -0.5, ALU.mult, ["gm"], ["gs"])
        ts("dve", gs[:, 2:3], gm[:, 40:41], 128 ** -0.5, ALU.mult, ["gm"], ["gs"])
        ones128 = cbf[:, 0:128]
        B64 = cbf[:, 128:256]
        O96 = cbf[:, 256:384]
        P_part = cbf[:, 384:512]
        P_mla = cbf[:, 512:640]
        ident = cbf[:, 640:768]
        sel = cf[:, 0:64]
        invf_part = cf[:, 64:65]
        invf_mla = cf[:, 65:66]
        CONST = ["gm", "cf", "cbf", "gs", "epsc"]

        PB = [ps(top, [128, 512], F32, "pb%d" % i) for i in range(7)]
        PT = ps(top, [128, 1024], BF16, "pt")
        PBN = ["pb%d" % i for i in range(7)]

        def load_w(es, dst, src, rows, cols, c0, dcol0, name):
            kc = rows // 128
            stg = sb(es, [128, kc, cols], F32, "wst")
            r = nm("wst")
            Sc.dma(stg[:], src[:, c0:c0 + cols].rearrange("(k p) c -> p k c", p=128), writes=[r])
            cp("pool", dst[:, :, dcol0:dcol0 + cols], stg[:], [r], [name])

        def rstd_from_ps(pss, psname, rows, n, dim, out, outname, tmp, tmpname):
            act(tmp[0:rows, 0:n], pss[0:rows, 0:n], AF.Ln, [psname, "epsc"], [tmpname], scale=1.0 / dim, bias=epsc[0:rows, :])
            act(out[0:rows, 0:n], tmp[0:rows, 0:n], AF.Exp, [tmpname], [outname], scale=-0.5)

        def norm_chunk(xs, xsname, n, gcol0, h, hname, sq, sqname, bank, tmp, tmpname, rs, rsname):
            act(sq[:, :, 0:n], xs[:, :, 0:n], AF.Square, [xsname], [sqname])
            for k in range(8):
                mm(PB[bank][:, 0:n], ones128, sq[:, k, 0:n], k == 0, k == 7, ["cbf", sqname], [PBN[bank]])
            rstd_from_ps(PB[bank], PBN[bank], 128, n, float(D), rs, rsname, tmp, tmpname)
            for k in range(8):
                stt(h[:, k, 0:n], xs[:, k, 0:n], gm[:, gcol0 + k:gcol0 + k + 1], rs[:, 0:n], ALU.mult, ALU.mult,
                    [xsname, "gm", rsname], [hname])

        def rope_tmp(es, n):
            return {"pi": sb(es, [128, n], I32, "rp_pi"), "pf": sb(es, [128, n], F32, "rp_pf"), "a": sb(es, [128, n], F32, "rp_a"),
                    "u": sb(es, [128, n], F32, "rp_u"), "ki": sb(es, [128, n], I32, "rp_ki"), "r": sb(es, [128, n], F32, "rp_r"),
                    "t": sb(es, [128, n], F32, "rp_t"), "k": nm("rp")}

        def rope_tables(RT, pos_d, t0, n, invf, rows, Ct, Ctn, St, Stn, cview=None, sview=None):
            pi, pf, a, u, ki, r, t, k = RT["pi"], RT["pf"], RT["a"], RT["u"], RT["ki"], RT["r"], RT["t"], RT["k"]
            R = slice(0, rows)
            Sc.dma(pi[:], pos_d[0:1, t0:t0 + n].to_broadcast([128, n]), writes=[k + "pi"])
            cp("dve", pf[R, :], pi[R, :], [k + "pi"], [k + "pf"])
            ts("dve", pf[R, :], pf[R, :], invf[R, :], ALU.mult, [k + "pf", "cf"], [k + "pf"])
            for shift, dst, dstn, view in ((0.0, St, Stn, sview), (math.pi / 2, Ct, Ctn, cview)):
                ts("dve", a[R, :], pf[R, :], shift, ALU.add, [k + "pf"], [k + "a"])
                ts("dve", u[R, :], a[R, :], 1.0 / (2 * math.pi), ALU.mult, [k + "a"], [k + "u"])
                cp("dve", ki[R, :], u[R, :], [k + "u"], [k + "ki"])
                cp("dve", u[R, :], ki[R, :], [k + "ki"], [k + "u"])
                stt(r[R, :], u[R, :], -2 * math.pi, a[R, :], ALU.mult, ALU.add, [k + "u", k + "a"], [k + "r"])
                ts("dve", t[R, :], r[R, :], math.pi, ALU.is_gt, [k + "r"], [k + "t"], s2=-2 * math.pi, op1=ALU.mult)
                tt("dve", r[R, :], r[R, :], t[R, :], ALU.add, [k + "r", k + "t"], [k + "r"])
                ts("dve", t[R, :], r[R, :], -math.pi, ALU.is_lt, [k + "r"], [k + "t"], s2=2 * math.pi, op1=ALU.mult)
                tt("dve", r[R, :], r[R, :], t[R, :], ALU.add, [k + "r", k + "t"], [k + "r"])
                ts("dve", r[R, :], r[R, :], math.pi, ALU.min, [k + "r"], [k + "r"], s2=-math.pi, op1=ALU.max)
                act(dst[R, :] if view is None else view, r[R, :], AF.Sin, [k + "r"], [dstn])

        def head_norm_rope(src, srcname, rows, n, blk, dim, gcol, Ct, Ctn, St, Stn, Pm, out, outname, bank, W, wn):
            R = slice(0, rows)
            if blk is not None:
                act(W["sq"][R, 0:n], src, AF.Square, [srcname], [wn + "sq"])
                mm(PB[bank][R, 0:n], blk[R, R], W["sq"][R, 0:n], True, True, ["cbf", wn + "sq"], [PBN[bank]])
                rstd_from_ps(PB[bank], PBN[bank], rows, n, float(dim), W["rs"], wn + "rs", W["tmp"], wn + "tmp")
                dst = W["xn"][R, 0:n] if Ct is not None else out
                dn = wn + "xn" if Ct is not None else outname
                stt(dst, src, gcol, W["rs"][R, 0:n], ALU.mult, ALU.mult, [srcname, "gm", "gs", wn + "rs"], [dn])
            else:
                cp("act", W["xn"][R, 0:n], src, [srcname], [wn + "xn"])
            if Ct is not None:
                mm(PB[bank][R, 0:n], Pm[R, R], W["xn"][R, 0:n], True, True, ["cbf", wn + "xn"], [PBN[bank]])
                tt("pool", W["t1"][R, 0:n], W["xn"][R, 0:n], Ct, ALU.mult, [wn + "xn", Ctn], [wn + "t1"])
                tt("dve", W["t2"][R, 0:n], PB[bank][R, 0:n], St, ALU.mult, [PBN[bank], Stn], [wn + "t2"])
                i0, i1 = W["t1"][R, 0:n], W["t2"][R, 0:n]
                if isinstance(out, tuple):
                    tt("dve", out[0], W["t1"][0:64, 0:n], W["t2"][0:64, 0:n], ALU.add, [wn + "t1", wn + "t2"], [outname])
                    tt("dve", out[1], W["t1"][64:128, 0:n], W["t2"][64:128, 0:n], ALU.add, [wn + "t1", wn + "t2"], [outname])
                    return
                if len(out.shape) == 3:
                    i0 = i0.rearrange("p (b q) -> p b q", q=128)
                    i1 = i1.rearrange("p (b q) -> p b q", q=128)
                tt("dve", out, i0, i1, ALU.add, [wn + "t1", wn + "t2"], [outname])

        def chunk_pipeline(es, nch, src_d, pos_d, tokoff, invf, rows, gcol0, pref, stage2, want_rope=True):
            xs = sb(es, [128, 8, N], F32, pref + "xs")
            hh_ = [sb(es, [128, 8, N], BF16, pref + "h%d" % i) for i in range(2)]
            sq = sb(es, [128, 8, N], BF16, pref + "sq")
            rs1 = sb(es, [128, N], F32, pref + "rs1")
            tmp1 = sb(es, [128, N], F32, pref + "tmp1")
            if want_rope:
                Cts = [sb(es, [128, N], F32, pref + "C%d" % i) for i in range(2)]
                Sts = [sb(es, [128, N], F32, pref + "S%d" % i) for i in range(2)]
                RT = rope_tmp(es, N)

            def s1a(c):
                i = c % 2
                t0 = tokoff + c * N
                Sc.dma(xs[:], src_d[:, t0:t0 + N].rearrange("(k p) n -> p k n", p=128), writes=[pref + "xs"])
                if want_rope:
                    rope_tables(RT, pos_d, t0, N, invf, rows, Cts[i], pref + "C%d" % i, Sts[i], pref + "S%d" % i)
                act(sq[:, :, 0:N], xs[:, :, 0:N], AF.Square, [pref + "xs"], [pref + "sq"])
                for k in range(8):
                    mm(PB[0][:, 0:N], ones128, sq[:, k, 0:N], k == 0, k == 7, ["cbf", pref + "sq"], [PBN[0]])
                rstd_from_ps(PB[0], PBN[0], 128, N, float(D), rs1, pref + "rs1", tmp1, pref + "tmp1")

            def s1b(c):
                i = c % 2
                for k in range(8):
                    stt(hh_[i][:, k, 0:N], xs[:, k, 0:N], gm[:, gcol0 + k:gcol0 + k + 1], rs1[:, 0:N], ALU.mult, ALU.mult,
                        [pref + "xs", "gm", pref + "rs1"], [pref + "h%d" % i])
            s1a(0)
            s1b(0)
            for c in range(nch):
                if c + 1 < nch:
                    s1a(c + 1)
                    s1b(c + 1)
                i = c % 2
                if want_rope:
                    stage2(c, hh_[i], pref + "h%d" % i, Cts[i], pref + "C%d" % i, Sts[i], pref + "S%d" % i)
                else:
                    stage2(c, hh_[i], pref + "h%d" % i, None, None, None, None)

        def work_tiles(es, n, pref):
            W = {"sq": sb(es, [128, n], BF16, pref + "sq"), "rs": sb(es, [128, n], F32, pref + "rs"),
                 "tmp": sb(es, [128, n], F32, pref + "tmp"), "xn": sb(es, [128, n], BF16, pref + "xn"),
                 "t1": sb(es, [128, n], F32, pref + "t1"), "t2": sb(es, [128, n], F32, pref + "t2")}
            return W

        def attn_epilogue(psO, psOn, bankB, n, W, wn, writer):
            cp("act", W["osb"][0:65, 0:n], psO[0:65, 0:n], [psOn], [wn + "osb"])
            mm(PB[bankB][0:64, 0:n], sel[0:65, 0:64], W["osb"][0:65, 0:n], True, True, ["cf", wn + "osb"], [PBN[bankB]])
            op("dve", lambda e: e.reciprocal(out=W["rinv"][0:64, 0:n], in_=PB[bankB][0:64, 0:n]), [PBN[bankB]], [wn + "rinv"])
            writer(W["osb"], W["rinv"])

        KmT = sb(top, [128, 4, NMEM], BF16, "KmT")
        Vm = sb(top, [128, 2, 512], BF16, "Vm")
        with scope() as es:
            wk = sb(es, [128, 8, 512], BF16, "wk")
            wv = sb(es, [128, 8, 512], BF16, "wv")
            with scope() as e2:
                load_w(e2, wk, m_wk, D, 512, 0, 0, "wk")
                load_w(e2, wv, m_wv, D, 512, 0, 0, "wv")
            ms = sb(es, [128, 8, NMEM], F32, "ms")
            mh = sb(es, [128, 8, NMEM], BF16, "mh")
            msq = sb(es, [128, 8, NMEM], BF16, "msq")
            W = work_tiles(es, NMEM, "mw")
            Sc.dma(ms[:], memT.rearrange("(k p) n -> p k n", p=128), writes=["ms"])
            norm_chunk(ms, "ms", NMEM, 16, mh, "mh", msq, "msq", 0, W["tmp"], "mwtmp", W["rs"], "mwrs")
            for hh in range(4):
                for k in range(8):
                    mm(PB[1][:, 0:NMEM], wk[:, k, hh * 128:(hh + 1) * 128], mh[:, k, :], k == 0, k == 7, ["wk", "mh"], ["pb1"])
                head_norm_rope(PB[1][:, 0:NMEM], "pb1", 128, NMEM, ones128, 128, gm[:, 41:42], None, None, None, None, None,
                               KmT[:, hh, :], "KmT", 2, W, "mw")
            for blk in range(2):
                for k in range(8):
                    mm(PB[3][:, 0:512], mh[:, k, blk * 128:(blk + 1) * 128], wv[:, k, :], k == 0, k == 7, ["mh", "wv"], ["pb3"])
                cp("act", Vm[:, blk, :], PB[3][:, 0:512], ["pb3"], ["Vm"])

        for hf in range(NH):
            tok0 = hf * T_H
            with scope() as pd:
                NBH = T_H // 128
                qdT = sb(pd, [128, NBH, 4, 128], BF16, "qdT")
                qiT = sb(pd, [128, NBH, 4, 128], BF16, "qiT")
                wabs = sb(pd, [128, NBH, 8], F32, "wabs")
                wsgn = sb(pd, [128, NBH, 8], F32, "wsgn")
                with scope() as es:
                    Wq = sb(es, [128, 8, 1032], BF16, "Wq")
                    for c4 in range(4):
                        with scope() as e2:
                            load_w(e2, Wq, w_in, D, 64, O_QD + c4 * 64, c4 * 128, "Wq")
                            load_w(e2, Wq, w_in, D, 64, O_QD + (4 + c4) * 64, c4 * 128 + 64, "Wq")
                    for q4 in range(2):
                        with scope() as e2:
                            load_w(e2, Wq, w_in, D, 256, O_QI + q4 * 256, 512 + q4 * 256, "Wq")
                    with scope() as e2:
                        load_w(e2, Wq, w_in, D, 8, O_WI, 1024, "Wq")
                    W = work_tiles(es, N, "bw")
                    wtok = sb(es, [128, NB, 8], F32, "wtok")

                    def q_stage2(c, h, hn, Ct, Ctn, St, Stn):
                        b0 = c * NB
                        for c4 in range(4):
                            for k in range(8):
                                mm(PB[1][:, 0:N], Wq[:, k, c4 * 128:(c4 + 1) * 128], h[:, k, :], k == 0, k == 7, ["Wq", hn], ["pb1"])
                            head_norm_rope(PB[1][:, 0:N], "pb1", 128, N, B64, 64, gs[:, 1:2], Ct[:, :], Ctn, St[:, :], Stn, P_part,
                                           qdT[:, b0:b0 + NB, c4, :], "qdT", 2, W, "bw")
                        for c4 in range(4):
                            for k in range(8):
                                mm(PB[3][:, 0:N], Wq[:, k, 512 + c4 * 128:512 + (c4 + 1) * 128], h[:, k, :], k == 0, k == 7, ["Wq", hn], ["pb3"])
                            head_norm_rope(PB[3][:, 0:N], "pb3", 128, N, None, 64, None, Ct[:, :], Ctn, St[:, :], Stn, P_part,
                                           qiT[:, b0:b0 + NB, c4, :], "qiT", 4, W, "bw")
                        for b in range(NB):
                            for k in range(8):
                                mm(PB[5][:, 0:8], h[:, k, b * 128:(b + 1) * 128], Wq[:, k, 1024:1032], k == 0, k == 7, [hn, "Wq"], ["pb5"])
                            cp("act", wtok[:, b, :], PB[5][:, 0:8], ["pb5"], ["wtok"])
                        act(wabs[:, b0:b0 + NB, :], wtok[:], AF.Abs, ["wtok"], ["wabs"], scale=(8 ** -0.5) * (64 ** -0.5))
                        ts("dve", wsgn[:, b0:b0 + NB, :], wtok[:], 0.0, ALU.is_ge, ["wtok"], ["wsgn"], s2=2.0, op1=ALU.mult)
                        ts("dve", wsgn[:, b0:b0 + NB, :], wsgn[:, b0:b0 + NB, :], -1.0, ALU.add, ["wsgn"], ["wsgn"])
                    chunk_pipeline(es, NCH_H, xT_own, pos_own, tok0, invf_part, 128, 0, "q", q_stage2)
                kdT = sb(pd, [128, S], BF16, "kdT")
                kiT_lo = sb(pd, [128, S], BF16, "kiTlo")
                kiT_hi = sb(pd, [128, S], BF16, "kiThi")
                op("pool", lambda e: e.memset(kiT_lo[64:128, :], 0.0), (), ["kiT"])
                op("pool", lambda e: e.memset(kiT_hi[0:64, :], 0.0), (), ["kiT"])
                vd = sb(pd, [128, NKB, 2, 65], BF16, "vd")
                op("pool", lambda e: e.memset(vd[:, :, :, 64:65], 1.0), (), ["vd"])
                with scope() as es:
                    Wk = sb(es, [128, 8, 384], BF16, "Wk1")
                    with scope() as e2:
                        load_w(e2, Wk, w_in, D, 128, O_KD, 0, "Wk1")
                        load_w(e2, Wk, w_in, D, 128, O_VD, 128, "Wk1")
                        load_w(e2, Wk, w_in, D, 64, O_KI, 256, "Wk1")
                        load_w(e2, Wk, w_in, D, 64, O_KI, 320, "Wk1")
                    W = work_tiles(es, N, "aw")

                    def a1_stage2(c, h, hn, Ct, Ctn, St, Stn):
                        t0 = c * N
                        for k in range(8):
                            mm(PB[1][:, 0:N], Wk[:, k, 0:128], h[:, k, :], k == 0, k == 7, ["Wk1", hn], ["pb1"])
                        head_norm_rope(PB[1][:, 0:N], "pb1", 128, N, B64, 64, gm[:, 38:39], Ct[:, :], Ctn, St[:, :], Stn, P_part,
                                       kdT[:, t0:t0 + N], "kdT", 2, W, "aw")
                        for k in range(8):
                            mm(PB[3][:, 0:N], Wk[:, k, 256:384], h[:, k, :], k == 0, k == 7, ["Wk1", hn], ["pb3"])
                        head_norm_rope(PB[3][:, 0:N], "pb3", 128, N, B64, 64, gm[:, 39:40], Ct[:, :], Ctn, St[:, :], Stn, P_part,
                                       (kiT_lo[0:64, t0:t0 + N], kiT_hi[64:128, t0:t0 + N]), "kiT", 4, W, "aw")
                        for b in range(N // 128):
                            kb = (t0 // 128) + b
                            for k in range(8):
                                mm(PB[5][:, 0:128], h[:, k, b * 128:(b + 1) * 128], Wk[:, k, 128:256], k == 0, k == 7, [hn, "Wk1"], ["pb5"])
                            cp("act", vd[:, kb, :, 0:64], PB[5][:, 0:128].rearrange("p (g d) -> p g d", g=2), ["pb5"], ["vd"])
                    chunk_pipeline(es, NCH_ALL, xT_all, pos_all, 0, invf_part, 128, 0, "a", a1_stage2)
                with scope() as es:
                    W = {}
                    W["osb"] = sb(es, [128, 512], F32, "bwosb")
                    W["rinv"] = sb(es, [128, 512], F32, "bwrinv")
                    Dg = sb(es, [128, 8, 128], BF16, "Dg")
                    Isb = sb(es, [128, S], F32, "Isb")
                    junk = sb(es, [128, S], mybir.dt.uint8, "junk")
                    Mall = sb(es, [128, S], BF16, "Mall")
                    Rh = [sb(es, [128, 512], BF16, "Rh%d" % i) for i in range(4)]
                    MT = [sb(es, [128, 4, 128], BF16, "MT%d" % i) for i in range(2)]
                    Eb = [sb(es, [128, 512], BF16, "Eb%d" % i) for i in range(3)]
                    bs = sb(es, [128, 8], F32, "bs")
                    qb0 = tok0 // 128

                    def stage_idx(b):
                        ext = 256 * (qb0 + b + 1)
                        for hh in range(8):
                            ts("dve", Dg[:, hh, :], ident, wsgn[:, b, hh:hh + 1], ALU.mult, ["cbf", "wsgn"], ["Dg"])
                        YB = [0, 1, 3, 4]
                        items = []
                        k0 = 0
                        while k0 < ext:
                            kw = min(512, ext - k0)
                            for hh in range(8):
                                items.append((k0, kw, hh))
                            k0 += kw

                        def ymm(i):
                            k0, kw, hh = items[i]
                            kt = kiT_lo if hh % 2 == 0 else kiT_hi
                            bk = YB[i % 4]
                            mm(PB[bk][:, 0:kw], qiT[:, b, hh // 2, :], kt[:, k0:k0 + kw], True, True, ["qiT", "kiT"], [PBN[bk]])
                        LA = 3
                        for i in range(min(LA, len(items))):
                            ymm(i)
                        for i in range(len(items)):
                            if i + LA < len(items):
                                ymm(i + LA)
                            k0, kw, hh = items[i]
                            bk = YB[i % 4]
                            r = Rh[i % 4]
                            rn = "Rh%d" % (i % 4)
                            if hh % 2 == 0:
                                act(r[:, 0:kw], PB[bk][:, 0:kw], AF.Relu, [PBN[bk], "wabs"], [rn], scale=wabs[:, b, hh:hh + 1])
                            else:
                                ts("dve", r[:, 0:kw], PB[bk][:, 0:kw], 0.0, ALU.max, [PBN[bk], "wabs"], [rn],
                                   s2=wabs[:, b, hh:hh + 1], op1=ALU.mult)
                            mm(PB[2][:, 0:kw], Dg[:, hh, :], r[:, 0:kw], hh == 0, hh == 7, ["Dg", rn], ["pb2"])
                            if hh == 7:
                                cp("act", Isb[:, k0:k0 + kw], PB[2][:, 0:kw], ["pb2"], ["Isb"])

                    def stage_thr(b):
                        ext = 256 * (qb0 + b + 1)
                        op("dve", lambda e: e.tensor_reduce(out=bs[:, 5:6], in_=Isb[:, 0:ext], axis=AX.X, op=ALU.max, apply_absolute_value=True),
                           ["Isb"], ["bs"])
                        tt("dve", Isb[:, ext - 256:ext], Isb[:, ext - 256:ext], mI[:, :], ALU.add, ["Isb", "mI"], ["Isb"])
                        ts("dve", bs[:, 0:1], bs[:, 5:6], -1.0, ALU.mult, ["bs"], ["bs"], s2=-1.0, op1=ALU.add)
                        ts("dve", bs[:, 1:2], bs[:, 5:6], 2.0, ALU.mult, ["bs"], ["bs"], s2=2.0, op1=ALU.add)
                        for it in range(BISECT_ITERS):
                            cst = 2.0 ** -(it + 1)
                            stt(bs[:, 2:3], bs[:, 1:2], cst, bs[:, 0:1], ALU.mult, ALU.add, ["bs"], ["bs"])
                            ts("dve", junk[:, 0:ext], Isb[:, 0:ext], bs[:, 2:3], ALU.is_ge, ["Isb", "bs"], ["junk", "bs"],
                               op1=ALU.add, accum=bs[:, 3:4])
                            ts("dve", bs[:, 4:5], bs[:, 3:4], TOPK - 0.5, ALU.is_ge, ["bs"], ["bs"], s2=cst, op1=ALU.mult)
                            stt(bs[:, 0:1], bs[:, 4:5], bs[:, 1:2], bs[:, 0:1], ALU.mult, ALU.add, ["bs"], ["bs"])

                    def stage_mall(b):
                        ext = 256 * (qb0 + b + 1)
                        ts("dve", Mall[:, 0:ext], Isb[:, 0:ext], bs[:, 0:1], ALU.is_lt, ["Isb", "bs"], ["Mall"], s2=-30000.0, op1=ALU.mult)

                    def stage_attn(b):
                        ext = 256 * (qb0 + b + 1)
                        nkb = ext // 128
                        ngrp = (nkb + 3) // 4

                        def prep(gi):
                            kc = gi * 4
                            nb4 = min(4, nkb - kc)
                            mt = MT[gi % 2]
                            mtn = "MT%d" % (gi % 2)
                            for j in range(nb4):
                                op("pe", lambda e: e.transpose(out=PT[:, j * 128:(j + 1) * 128], in_=Mall[:, (kc + j) * 128:(kc + j + 1) * 128], identity=ident),
                                   ["Mall", "cbf"], ["pt"])
                            cp("act", mt[:, 0:nb4, :], PT[:, 0:nb4 * 128].rearrange("p (a q) -> p a q", a=nb4), ["pt"], [mtn])

                        units = [(kb, g) for kb in range(nkb) for g in range(2)]

                        def emit_S(u):
                            kb, g = units[u]
                            G = slice(g * 64, (g + 1) * 64)
                            bk = 3 + (u % 2)
                            mt = MT[(kb // 4) % 2]
                            mtn = "MT%d" % ((kb // 4) % 2)
                            mm(PB[bk][:, 0:512], kdT[G, kb * 128:(kb + 1) * 128], qdT[G, b, :, :].rearrange("p a q -> p (a q)"),
                               True, False, ["kdT", "qdT"], [PBN[bk]])
                            mm(PB[bk][:, 0:512].rearrange("p (a q) -> p a q", a=4), ident, mt[:, (kb % 4):(kb % 4) + 1, :].to_broadcast([128, 4, 128]),
                               False, True, ["cbf", mtn], [PBN[bk]])

                        def emit_rest(u):
                            kb, g = units[u]
                            bk = 3 + (u % 2)
                            u2 = u % 3
                            act(Eb[u2][:, :], PB[bk][:, 0:512], AF.Exp, [PBN[bk]], ["Eb%d" % u2])
                            mm(PB[5 + g][0:65, 0:512], vd[:, kb, g, :], Eb[u2][:, :], kb == 0, kb == nkb - 1, ["vd", "Eb%d" % u2], [PBN[5 + g]])

                        prep(0)
                        if ngrp > 1:
                            prep(1)
                        emit_S(0)
                        for u in range(len(units)):
                            if u + 1 < len(units):
                                emit_S(u + 1)
                            emit_rest(u)
                            kb, g = units[u]
                            if g == 1 and kb % 4 == 3 and (kb // 4) + 2 < ngrp:
                                prep(kb // 4 + 2)
                        for g in range(2):
                            def writer(osb, rinv, g=g, b=b):
                                for a in range(4):
                                    hd = g * 4 + a
                                    col = b * 128
                                    tt("dve", mixD[(hd % 2) * 64:(hd % 2) * 64 + 64, hd // 2, col:col + 128],
                                       osb[0:64, a * 128:(a + 1) * 128], rinv[0:64, a * 128:(a + 1) * 128], ALU.mult,
                                       ["bwosb", "bwrinv"], ["mixD"])
                            attn_epilogue(PB[5 + g], PBN[5 + g], 3, 512, W, "bw", writer)

                    stage_idx(0)
                    stage_thr(0)
                    stage_mall(0)
                    for b in range(NBH):
                        if b + 1 < NBH:
                            stage_idx(b + 1)
                            stage_thr(b + 1)
                        stage_attn(b)
                        if b + 1 < NBH:
                            stage_mall(b + 1)

            with scope() as pmd:
                mixM = sb(pmd, [128, 4, T_H], BF16, "mixM")
                mT = sb(pmd, [128, 2 * NB, N], BF16, "mT")
                with scope() as es:
                    st2 = sb(es, [128, 2 * NB * N], F32, "cst2")
                    Sc.dma(st2[:], mT_d[:, :], writes=["cst2"])
                    cp("dve", mT[:].rearrange("p a n -> p (a n)"), st2[:], ["cst2"], ["mT"])
                with scope() as pm:
                    ckvnT = sb(pm, [128, S], BF16, "ckvnT")
                    kpeX = sb(pm, [128, S], BF16, "kpeX")
                    cqnT = sb(pm, [128, 2, T_H], BF16, "cqnT")
                    tabC = sb(pm, [128, T_H], BF16, "tabC")
                    tabS = sb(pm, [128, T_H], BF16, "tabS")
                    with scope() as es:
                        Wk = sb(es, [128, 8, 256], BF16, "Wk2")
                        with scope() as e2:
                            load_w(e2, Wk, w_in, D, 128, O_CKV, 0, "Wk2")
                            load_w(e2, Wk, w_in, D, 32, O_KPE, 128, "Wk2")
                            load_w(e2, Wk, w_in, D, 32, O_KPE, 192, "Wk2")
                        W = work_tiles(es, N, "cw")
                        op("pool", lambda e: e.memset(Wk[:, :, 160:192], 0.0), (), ["Wk2"])
                        op("pool", lambda e: e.memset(Wk[:, :, 224:256], 0.0), (), ["Wk2"])

                        def a2_stage2(c, h, hn, Ct, Ctn, St, Stn):
                            t0 = c * N
                            for k in range(8):
                                mm(PB[1][:, 0:N], Wk[:, k, 0:128], h[:, k, :], k == 0, k == 7, ["Wk2", hn], ["pb1"])
                            head_norm_rope(PB[1][:, 0:N], "pb1", 128, N, ones128, 128, gm[:, 34:35], None, None, None, None, None,
                                           ckvnT[:, t0:t0 + N], "ckvnT", 2, W, "cw")
                            for k in range(8):
                                mm(PB[3][:, 0:N], Wk[:, k, 128:256], h[:, k, :], k == 0, k == 7, ["Wk2", hn], ["pb3"])
                            act(kpeX[0:32, t0:t0 + N], PB[3][0:32, 0:N], AF.Square, ["pb3"], ["kpeX"])
                            R = slice(64, 96)
                            ts("dve", W["xn"][R, 0:N], PB[3][R, 0:N], gm[R, 36:37], ALU.mult, ["pb3", "gm"], ["cwxn"])
                            op("pool", lambda e: e.memset(W["xn"][0:64, 0:N], 0.0), (), ["cwxn"])
                            mm(PB[4][0:96, 0:N], P_mla[0:96, 0:96], W["xn"][0:96, 0:N], True, True, ["cbf", "cwxn"], ["pb4"])
                            tt("pool", W["t1"][R, 0:N], W["xn"][R, 0:N], Ct[R, :], ALU.mult, ["cwxn", Ctn], ["cwt1"])
                            tt("dve", W["t2"][R, 0:N], PB[4][R, 0:N], St[R, :], ALU.mult, ["pb4", Stn], ["cwt2"])
                            tt("dve", kpeX[R, t0:t0 + N], W["t1"][R, 0:N], W["t2"][R, 0:N], ALU.add, ["cwt1", "cwt2"], ["kpeX"])
                        chunk_pipeline(es, NCH_ALL, xT_all, pos_all, 0, invf_mla, 96, 0, "c", a2_stage2)
                    with scope() as es:
                        Wc = sb(es, [128, 8, 256], BF16, "Wc")
                        with scope() as e2:
                            load_w(e2, Wc, w_in, D, 256, O_CQ, 0, "Wc")
                        W = work_tiles(es, N, "dw")
                        cqr = sb(es, [128, 2, N], F32, "cqr")
                        cqs = sb(es, [128, 2, N], BF16, "cqs")

                        def l_stage2(c, h, hn, Cf, Cfn, Sf, Sfn):
                            cp("pool", tabC[0:96, c * N:(c + 1) * N], Cf[0:96, :], [Cfn], ["tabC"])
                            cp("pool", tabS[0:96, c * N:(c + 1) * N], Sf[0:96, :], [Sfn], ["tabS"])
                            for r2 in range(2):
                                for k in range(8):
                                    mm(PB[1][:, 0:N], Wc[:, k, r2 * 128:(r2 + 1) * 128], h[:, k, :], k == 0, k == 7, ["Wc", hn], ["pb1"])
                                cp("act", cqr[:, r2, :], PB[1][:, 0:N], ["pb1"], ["cqr"])
                            act(cqs[:, :, :], cqr[:, :, :], AF.Square, ["cqr"], ["cqs"])
                            for r2 in range(2):
                                mm(PB[2][:, 0:N], ones128, cqs[:, r2, :], r2 == 0, r2 == 1, ["cbf", "cqs"], ["pb2"])
                            rstd_from_ps(PB[2], "pb2", 128, N, 256.0, W["rs"], "dwrs", W["tmp"], "dwtmp")
                            for r2 in range(2):
                                stt(cqnT[:, r2, c * N:(c + 1) * N], cqr[:, r2, :], gm[:, 32 + r2:33 + r2], W["rs"][:, 0:N], ALU.mult, ALU.mult,
                                    ["cqr", "gm", "dwrs"], ["cqnT"])
                        chunk_pipeline(es, NCH_H, xT_own, pos_own, tok0, invf_mla, 96, 0, "l", l_stage2)
                    with scope() as es:
                        wuq = sb(es, [128, 2, 768], BF16, "wuq")
                        wukv = sb(es, [128, 1, 1024], BF16, "wukv")
                        with scope() as e2:
                            load_w(e2, wuq, w_uq, 256, 768, 0, 0, "wuq")
                            load_w(e2, wukv, w_ukv, 128, 1024, 0, 0, "wukv")
                        KhT = sb(es, [128, S], BF16, "KhT")
                        Vh = sb(es, [128, NKB, 65], BF16, "Vh")
                        QhT = sb(es, [128, T_H], BF16, "QhT")
                        W = work_tiles(es, N, "ew")
                        W["osb"] = sb(es, [128, 512], F32, "ewosb")
                        W["rinv"] = sb(es, [128, 512], F32, "ewrinv")
                        Eb = [sb(es, [128, N], BF16, "mEb%d" % i) for i in range(3)]
                        op("pool", lambda e: e.memset(Vh[:, :, 64:65], 1.0), (), ["Vh"])
                        for hd in range(8):
                            for c in range(NCH_ALL):
                                t0 = c * N
                                mm(PB[0][0:64, 0:N], wukv[:, 0, hd * 128:hd * 128 + 64], ckvnT[:, t0:t0 + N], True, True, ["wukv", "ckvnT"], ["pb0"])
                                act(W["sq"][0:64, 0:N], PB[0][0:64, 0:N], AF.Square, ["pb0"], ["ewsq"])
                                mm(PB[1][0:96, 0:N], O96[0:64, 0:96], W["sq"][0:64, 0:N], True, False, ["cbf", "ewsq"], ["pb1"])
                                mm(PB[1][0:96, 0:N], O96[0:32, 0:96], kpeX[0:32, t0:t0 + N], False, True, ["cbf", "kpeX"], ["pb1"])
                                rstd_from_ps(PB[1], "pb1", 96, N, 96.0, W["rs"], "ewrs", W["tmp"], "ewtmp")
                                stt(KhT[0:64, t0:t0 + N], PB[0][0:64, 0:N], gm[0:64, 36:37], W["rs"][0:64, 0:N], ALU.mult, ALU.mult,
                                    ["pb0", "gm", "ewrs"], ["KhT"])
                                tt("pool", KhT[64:96, t0:t0 + N], kpeX[64:96, t0:t0 + N], W["rs"][64:96, 0:N], ALU.mult, ["kpeX", "ewrs"], ["KhT"])
                            for kb0 in range(0, NKB, 8):
                                for j in range(8):
                                    kb = kb0 + j
                                    mm(PB[2][:, j * 64:(j + 1) * 64], ckvnT[:, kb * 128:(kb + 1) * 128], wukv[:, 0, hd * 128 + 64:hd * 128 + 128],
                                       True, True, ["ckvnT", "wukv"], ["pb2"])
                                cp("act", Vh[:, kb0:kb0 + 8, 0:64], PB[2][:, 0:512].rearrange("p (a d) -> p a d", a=8), ["pb2"], ["Vh"])
                            for c in range(NCH_H):
                                for r2 in range(2):
                                    mm(PB[0][0:96, 0:N], wuq[:, r2, hd * 96:(hd + 1) * 96], cqnT[:, r2, c * N:(c + 1) * N], r2 == 0, r2 == 1,
                                       ["wuq", "cqnT"], ["pb0"])
                                head_norm_rope(PB[0][0:96, 0:N], "pb0", 96, N, O96, 96, gs[0:96, 0:1], tabC[0:96, c * N:(c + 1) * N], "tabC",
                                               tabS[0:96, c * N:(c + 1) * N], "tabS", P_mla, QhT[0:96, c * N:(c + 1) * N], "QhT", 1, W, "ew")
                            for c in range(NCH_H):
                                j = (tok0 // N) + c
                                ext = 2 * NB * (j + 1)
                                bo = 3 + (c % 2)
                                def emit_S(kb, c=c, j=j):
                                    bs_ = 5 + (kb % 2)
                                    dg = kb >= 2 * NB * j
                                    mm(PB[bs_][:, 0:N], KhT[0:96, kb * 128:(kb + 1) * 128], QhT[0:96, c * N:(c + 1) * N], True, not dg, ["KhT", "QhT"], [PBN[bs_]])
                                    if dg:
                                        mm(PB[bs_][:, 0:N], ident, mT[:, kb - 2 * NB * j, :], False, True, ["cbf", "mT"], [PBN[bs_]])

                                def emit_rest(kb, ext=ext, bo=bo):
                                    bs_ = 5 + (kb % 2)
                                    e_ = Eb[kb % 3]
                                    en = "mEb%d" % (kb % 3)
                                    act(e_[:, :], PB[bs_][:, 0:N], AF.Exp, [PBN[bs_]], [en])
                                    mm(PB[bo][0:65, 0:N], Vh[:, kb, :], e_[:, :], kb == 0, kb == ext - 1, ["Vh", en], [PBN[bo]])
                                emit_S(0)
                                for kb in range(ext):
                                    if kb + 1 < ext:
                                        emit_S(kb + 1)
                                    emit_rest(kb)

                                def writer(osb, rinv, hd=hd, c=c):
                                    tt("dve", mixM[(hd % 2) * 64:(hd % 2) * 64 + 64, hd // 2, c * N:(c + 1) * N], osb[0:64, 0:N], rinv[0:64, 0:N],
                                       ALU.mult, ["ewosb", "ewrinv"], ["mixM"])
                                attn_epilogue(PB[bo], PBN[bo], 2, N, W, "ew", writer)

                if dbg:
                    with scope() as es:
                        mf = sb(es, [128, 8, T_H], F32, "mf")
                        cp("dve", mf[:, 0:4, :], mixM[:], ["mixM"], ["mf"])
                        cp("dve", mf[:, 4:8, :], mixD[:], ["mixD"], ["mf"])
                        Sc.dma(dbg_mixed[:, tok0:tok0 + T_H].rearrange("(k p) n -> p k n", p=128), mf[:], reads=["mf"], is_out=True)

                with scope() as es:
                    wo_ = sb(es, [128, 8, D], BF16, "wo_")
                    wq_ = sb(es, [128, 8, 512], BF16, "wq_")
                    wmo = sb(es, [128, 4, D], BF16, "wmo")
                    with scope() as e2:
                        for q4 in range(4):
                            with scope() as e3:
                                load_w(e3, wo_, w_out, D, 256, q4 * 256, q4 * 256, "wo_")
                        load_w(e2, wq_, m_wq, D, 512, 0, 0, "wq_")
                    with scope() as e2:
                        load_w(e2, wmo, m_wo, 512, D, 0, 0, "wmo")
                    x1 = sb(es, [128, 8, N], F32, "x1")
                    h = sb(es, [128, 8, N], BF16, "h4")
                    W = work_tiles(es, N, "fw")
                    om = sb(es, [128, 4, N], BF16, "om")
                    Eb = [sb(es, [128, N], BF16, "cEb%d" % i) for i in range(2)]
                    actT = sb(es, [128, DFF // 128, N], BF16, "actT")
                    stg = [sb(es, [128, 2048], F32, "fstg%d" % i) for i in range(4)]
                    wsl = [sb(es, [128, 2048], BF16, "fws%d" % i) for i in range(4)]
                    sg = sb(es, [128, N], F32, "sg")
                    nld = [0]

                    def load_slab(src_ap, a_, b_):
                        i = nld[0]
                        nld[0] += 1
                        sn = "fstg%d" % (i % 4)
                        wn_ = "fws%d" % (i % 4)
                        s_ = stg[i % 4][:, 0:a_ * b_].rearrange("p (a b) -> p a b", a=a_)
                        w_ = wsl[i % 4][:, 0:a_ * b_].rearrange("p (a b) -> p a b", a=a_)
                        Sc.dma(s_, src_ap, writes=[sn])
                        cp(("pool", "dve", "act")[i % 3], w_, s_, [sn], [wn_])
                        return w_, wn_

                    sq = actT[:, 0:8, :]
                    for c in range(NCH_H):
                        lt0 = tok0 + c * N
                        Sc.dma(x1[:], xT_own[:, lt0:lt0 + N].rearrange("(k p) n -> p k n", p=128), writes=["x1"])
                        for m in range(8):
                            bk = m % 2
                            for k in range(8):
                                mm(PB[bk][:, 0:N], wo_[:, k, m * 128:(m + 1) * 128], (mixM if k < 4 else mixD)[:, k % 4, c * N:(c + 1) * N], k == 0, k == 7, ["wo_", "mixM", "mixD"], [PBN[bk]])
                            tt("dve", x1[:, m, :], x1[:, m, :], PB[bk][:, 0:N], ALU.add, ["x1", PBN[bk]], ["x1"])
                        norm_chunk(x1, "x1", N, 8, h, "h4", sq, "actT", 2, W["tmp"], "fwtmp", W["rs"], "fwrs")
                        for hh in range(4):
                            for k in range(8):
                                mm(PB[3][:, 0:N], wq_[:, k, hh * 128:(hh + 1) * 128], h[:, k, :], k == 0, k == 7, ["wq_", "h4"], ["pb3"])
                            head_norm_rope(PB[3][:, 0:N], "pb3", 128, N, ones128, 128, gs[:, 2:3], None, None, None, None, None,
                                           W["xn"][:, 0:N], "fwxn2", 4, W, "fw")
                            for blk in range(2):
                                mm(PB[5][:, 0:N], KmT[:, hh, blk * 128:(blk + 1) * 128], W["xn"][:, 0:N], True, True, ["KmT", "fwxn2"], ["pb5"])
                                act(Eb[blk][:, :], PB[5][:, 0:N], AF.Exp, ["pb5"], ["cEb%d" % blk])
                            for blk in range(2):
                                mm(PB[6][:, 0:N], Vm[:, blk, hh * 128:(hh + 1) * 128], Eb[blk][:, :], blk == 0, blk == 1, ["Vm", "cEb%d" % blk], ["pb6"])
                            for blk in range(2):
                                mm(PB[4][:, 0:N], ones128, Eb[blk][:, :], blk == 0, blk == 1, ["cbf", "cEb%d" % blk], ["pb4"])
                            op("dve", lambda e: e.reciprocal(out=W["t1"][:, 0:N], in_=PB[4][:, 0:N]), ["pb4"], ["fwt1"])
                            tt("dve", om[:, hh, :], PB[6][:, 0:N], W["t1"][:, 0:N], ALU.mult, ["pb6", "fwt1"], ["om"])
                        for m in range(8):
                            bk = m % 2
                            for hh in range(4):
                                mm(PB[bk][:, 0:N], wmo[:, hh, m * 128:(m + 1) * 128], om[:, hh, :], hh == 0, hh == 3, ["wmo", "om"], [PBN[bk]])
                            tt("dve", x1[:, m, :], x1[:, m, :], PB[bk][:, 0:N], ALU.add, ["x1", PBN[bk]], ["x1"])
                        norm_chunk(x1, "x1", N, 24, h, "h4", sq, "actT", 2, W["tmp"], "fwtmp", W["rs"], "fwrs")
                        slabs = [(c0, 256) for c0 in range(0, DFF, 256)]
                        for (c0, cw) in slabs:
                            wg, wgn = load_slab(f_wg[:, c0:c0 + cw].rearrange("(k p) c -> p k c", p=128), 8, cw)
                            wu, wun = load_slab(f_wu[:, c0:c0 + cw].rearrange("(k p) c -> p k c", p=128), 8, cw)
                            for f in range(cw // 128):
                                ff = c0 // 128 + f
                                bg = 3 + (ff % 2)
                                bu = 5 + (ff % 2)
                                for k in range(8):
                                    mm(PB[bg][:, 0:N], wg[:, k, f * 128:(f + 1) * 128], h[:, k, :], k == 0, k == 7, [wgn, "h4"], [PBN[bg]])
                                for k in range(8):
                                    mm(PB[bu][:, 0:N], wu[:, k, f * 128:(f + 1) * 128], h[:, k, :], k == 0, k == 7, [wun, "h4"], [PBN[bu]])
                                act(sg[:, :], PB[bg][:, 0:N], AF.Silu, [PBN[bg]], ["sg"])
                                tt("dve", actT[:, ff, :], sg[:, :], PB[bu][:, 0:N], ALU.mult, ["sg", PBN[bu]], ["actT"])
                        for grp in ((0, 1, 2, 3), (4, 5, 6, 7)):
                            rslabs = [(r0, 256) for r0 in range(0, DFF, 256)]
                            for (r0, rw) in rslabs:
                                nf = rw // 128
                                wd, wdn = load_slab(f_wd[r0:r0 + rw, grp[0] * 128:grp[0] * 128 + 512].rearrange("(f p) c -> p f c", p=128), nf, 512)
                                for f in range(nf):
                                    ff = r0 // 128 + f
                                    for mi, m in enumerate(grp):
                                        mm(PB[mi][:, 0:N], wd[:, f, mi * 128:(mi + 1) * 128], actT[:, ff, :], ff == 0, ff == DFF // 128 - 1, [wdn, "actT"], [PBN[mi]])
                            for mi, m in enumerate(grp):
                                tt("dve", x1[:, m, :], x1[:, m, :], PB[mi][:, 0:N], ALU.add, ["x1", PBN[mi]], ["x1"])
                        Sc.dma(outT[:, lt0:lt0 + N].rearrange("(k p) n -> p k n", p=128), x1[:], reads=["x1"], is_out=True)
        Sc.finish()
    return nc, Sc


def _consts(S, NH, half):
    T_OWN = S // 2
    T_H = T_OWN // NH
    N = min(512, T_H)
    NB = N // 128
    cb = np.zeros((128, 6 * 128), np.float32)
    cb[:, 0:128] = 1.0
    cb[0:64, 128:192] = 1.0
    cb[64:128, 192:256] = 1.0
    cb[0:96, 256:352] = 1.0
    Pp = np.zeros((128, 128), np.float32)
    for base in (0, 64):
        for i in range(8):
            Pp[base + 8 + i, base + i] = -1.0
            Pp[base + i, base + 8 + i] = 1.0
    cb[:, 384:512] = Pp
    Pm = np.zeros((128, 128), np.float32)
    for i in range(16):
        Pm[80 + i, 64 + i] = -1.0
        Pm[64 + i, 80 + i] = 1.0
    cb[:, 512:640] = Pm
    cb[:, 640:768] = np.eye(128, dtype=np.float32)
    cf = np.zeros((128, 66), np.float32)
    cf[64, 0:64] = 1.0
    f8 = (THETA ** (-np.arange(0, 16, 2, dtype=np.float32) / np.float32(16))).astype(np.float32)
    f16 = (THETA ** (-np.arange(0, 32, 2, dtype=np.float32) / np.float32(32))).astype(np.float32)
    for base in (0, 64):
        cf[base:base + 8, 64] = f8
        cf[base + 8:base + 16, 64] = f8
    cf[64:80, 65] = f16
    cf[80:96, 65] = f16
    mI = np.zeros((128, 256), np.float32)
    q = np.arange(128)[:, None]
    k = np.arange(256)[None, :]
    qpos = half * 128 + q
    mI[:] = np.where(k <= qpos, 0.0, NEG)
    mT = np.zeros((128, 2 * NB, N), np.float32)
    p = np.arange(128)[:, None]
    for kbp in range(2 * NB):
        for m in range(NB):
            r = np.arange(128)[None, :]
            gb = 2 * m + half
            if kbp < gb:
                v = np.zeros((128, 128), np.float32)
            elif kbp == gb:
                v = np.where(p <= r, 0.0, -30000.0).astype(np.float32)
            else:
                v = np.full((128, 128), -30000.0, np.float32)
            mT[:, kbp, m * 128:(m + 1) * 128] = v
    return cb, cf, mI, mT.reshape(128, 2 * NB * N)


def _gains(inp):
    gm = np.zeros((128, 42), np.float32)

    def col8(v, c0):
        gm[:, c0:c0 + 8] = np.asarray(v, np.float32).reshape(8, 128).T
    col8(inp["norm_mix"][0], 0)
    col8(inp["norm_mem_x"][0], 8)
    col8(inp["norm_mem_kv"][0], 16)
    col8(inp["norm_ffn"][0], 24)
    gm[:, 32:34] = np.asarray(inp["mla_q_a_norm"][0], np.float32).reshape(2, 128).T
    gm[:, 34] = inp["mla_kv_a_norm"][0]
    gm[0:96, 35] = inp["mla_q_norm"][0]
    gm[0:96, 36] = inp["mla_k_norm"][0]
    gm[:, 37] = np.tile(np.asarray(inp["dsa_q_norm"][0], np.float32), 2)
    gm[:, 38] = np.tile(np.asarray(inp["dsa_k_norm"][0], np.float32), 2)
    gm[:, 39] = np.tile(np.asarray(inp["idx_k_norm"][0], np.float32), 2)
    gm[:, 40] = inp["mem_q_norm"][0]
    gm[:, 41] = inp["mem_k_norm"][0]
    return gm


_CACHE = {}


def run(inputs, S, NH, dbg=False):
    inp = {k: np.asarray(v) for k, v in inputs.items()}
    B = inp["x"].shape[0]
    ncores = 2 * B
    key = (S, NH, dbg)
    if key not in _CACHE:
        _CACHE[key] = build(S, NH, dbg)
    nc, Sc = _CACHE[key]
    gm = _gains(inp)
    nblk = S // 128
    in_maps = []
    for c in range(ncores):
        b, half = c // 2, c % 2
        own_blocks = np.arange(half, nblk, 2)
        own_idx = (own_blocks[:, None] * 128 + np.arange(128)[None, :]).reshape(-1)
        xb = np.asarray(inp["x"][b], np.float32)
        cb, cf, mI, mT = _consts(S, NH, half)
        pos = np.asarray(inp["positions"][b], np.int32)
        in_maps.append({
            "xT_all": np.ascontiguousarray(xb.T),
            "xT_own": np.ascontiguousarray(xb[own_idx].T),
            "pos_all": np.ascontiguousarray(pos.reshape(1, S)),
            "pos_own": np.ascontiguousarray(pos[own_idx].reshape(1, S // 2)),
            "memT": np.ascontiguousarray(np.asarray(inp["mem"][b], np.float32).T),
            "w_in": np.ascontiguousarray(inp["w_in"][0], dtype=np.float32),
            "w_uq": np.ascontiguousarray(inp["mla_w_uq"][0], dtype=np.float32),
            "w_ukv": np.ascontiguousarray(inp["mla_w_ukv"][0], dtype=np.float32),
            "w_out": np.ascontiguousarray(inp["w_out"][0], dtype=np.float32),
            "m_wq": np.ascontiguousarray(inp["mem_w_q"][0], dtype=np.float32),
            "m_wk": np.ascontiguousarray(inp["mem_w_k"][0], dtype=np.float32),
            "m_wv": np.ascontiguousarray(inp["mem_w_v"][0], dtype=np.float32),
            "m_wo": np.ascontiguousarray(inp["mem_w_o"][0], dtype=np.float32),
            "f_wg": np.ascontiguousarray(inp["ffn_w_gate"][0], dtype=np.float32),
            "f_wu": np.ascontiguousarray(inp["ffn_w_up"][0], dtype=np.float32),
            "f_wd": np.ascontiguousarray(inp["ffn_w_down"][0], dtype=np.float32),
            "gm": gm, "cb": cb, "cf": cf, "mI": mI, "mT": mT,
        })
    res = run_bass_kernel_spmd(nc, in_maps, core_ids=list(range(ncores)))
    out = np.zeros((B, S, D), np.float32)
    extra = {}
    for c in range(ncores):
        b, half = c // 2, c % 2
        own_blocks = np.arange(half, nblk, 2)
        own_idx = (own_blocks[:, None] * 128 + np.arange(128)[None, :]).reshape(-1)
        out[b, own_idx, :] = np.asarray(res.results[c]["outT"]).T
        if dbg:
            extra[c] = (own_idx, np.asarray(res.results[c]["dbg_mixed"]).T)
    return out, extra


def kernel(**inputs):
    out, _ = run(inputs, 8192, 2)
    return out
```

```python
import math
from contextlib import ExitStack

import numpy as np
import concourse.bass as bass
import concourse.mybir as mybir
from concourse.bass_utils import run_bass_kernel_spmd

F32 = mybir.dt.float32
BF16 = mybir.dt.bfloat16
I32 = mybir.dt.int32
AF = mybir.ActivationFunctionType
ALU = mybir.AluOpType
AX = mybir.AxisListType

D = 1024
NMEM = 256
DFF = 2816
EPS = 1e-6
TOPK = 256
BISECT_ITERS = 16
NEG = -1.0e30
O_CQ, O_CKV, O_KPE, O_QD, O_KD, O_VD, O_QI, O_KI, O_WI = 0, 256, 384, 416, 928, 1056, 1184, 1696, 1760
THETA = 500000.0


class Sched:
    def __init__(self, nc):
        self.nc = nc
        self.engs = {"pe": nc.tensor, "act": nc.scalar, "dve": nc.vector, "pool": nc.gpsimd, "sp": nc.sync}
        self.sems = {}
        self.cnt = {}
        self.seen = {k: {} for k in self.engs}
        self.lastw = {}
        self.readers = {}
        self._stack = []
        self.ninst = 0
        self.nwait = 0
        for k in ["pe", "act", "dve", "pool"]:
            self.sems[k] = self.sem("s_" + k)
            self.cnt[k] = 0
        self.dsems = [[self.sem("d%d" % i), 0] for i in range(24)]
        self.dnext = 0
        self.out_events = []

    def sem(self, name):
        cm = self.nc.semaphore(name)
        s = cm.__enter__()
        self._stack.append(cm)
        return s

    def _wait(self, e, ev):
        if ev is None:
            return
        s, v = ev
        if e == "pe" and s is self.sems["pe"]:
            return
        key = id(s)
        if self.seen[e].get(key, 0) >= v:
            return
        self.engs[e].wait_ge(s, v)
        self.seen[e][key] = v
        self.nwait += 1

    def deps(self, e, reads, writes):
        for r in reads:
            self._wait(e, self.lastw.get(r))
        for w in writes:
            self._wait(e, self.lastw.get(w))
            for ev in self.readers.get(w, {}).values():
                self._wait(e, ev)

    def record(self, ev, reads, writes):
        for r in reads:
            d = self.readers.setdefault(r, {})
            d[id(ev[0])] = ev
        for w in writes:
            self.lastw[w] = ev
            self.readers[w] = {}

    def op(self, e, fn, reads=(), writes=()):
        self.deps(e, reads, writes)
        inst = fn(self.engs[e])
        self.cnt[e] += 1
        inst.then_inc(self.sems[e], 1)
        ev = (self.sems[e], self.cnt[e])
        self.record(ev, reads, writes)
        self.ninst += 1
        return ev

    def dma(self, out, in_, reads=(), writes=(), is_out=False):
        q = "sp"
        ds = self.dsems[self.dnext % len(self.dsems)]
        self.dnext += 1
        if ds[1] > 0:
            self._wait(q, (ds[0], ds[1]))
        self.deps(q, reads, writes)
        inst = self.engs[q].dma_start(out=out, in_=in_)
        ds[1] += 16
        inst.then_inc(ds[0], 16)
        ev = (ds[0], ds[1])
        self.record(ev, reads, writes)
        self.ninst += 1
        if is_out:
            self.out_events.append(ev)
        return ev

    def barrier(self):
        evs = [(self.sems[k], self.cnt[k]) for k in ["pe", "act", "dve", "pool"] if self.cnt[k] > 0]
        evs += [(d[0], d[1]) for d in self.dsems if d[1] > 0]
        for e in ["pe", "act", "dve", "pool", "sp"]:
            for ev in evs:
                self._wait(e, ev)

    def finish(self):
        for ev in self.out_events:
            self._wait("sp", ev)
        for d in self.dsems:
            if d[1] > 0:
                self._wait("sp", (d[0], d[1]))
        for k in ["pe", "act", "dve", "pool"]:
            if self.cnt[k] > 0:
                self._wait("sp", (self.sems[k], self.cnt[k]))
        for cm in reversed(self._stack):
            cm.__exit__(None, None, None)


def build(S, NH, dbg=False):
    T_OWN = S // 2
    T_H = T_OWN // NH
    N = min(512, T_H)
    NB = N // 128
    NCH_ALL = S // N
    NCH_H = T_H // N
    NKB = S // 128
    nc = bass.Bass("TRN2", target_bir_lowering=False)

    def dram(name, shape, dt=F32, kind="ExternalInput"):
        return nc.dram_tensor(name, shape, dt, kind=kind).ap()

    xT_all = dram("xT_all", [D, S])
    xT_own = dram("xT_own", [D, T_OWN])
    pos_all = dram("pos_all", [1, S], I32)
    pos_own = dram("pos_own", [1, T_OWN], I32)
    memT = dram("memT", [D, NMEM])
    w_in = dram("w_in", [D, 1768])
    w_uq = dram("w_uq", [256, 768])
    w_ukv = dram("w_ukv", [128, 1024])
    w_out = dram("w_out", [D, D])
    m_wq = dram("m_wq", [D, 512])
    m_wk = dram("m_wk", [D, 512])
    m_wv = dram("m_wv", [D, 512])
    m_wo = dram("m_wo", [512, D])
    f_wg = dram("f_wg", [D, DFF])
    f_wu = dram("f_wu", [D, DFF])
    f_wd = dram("f_wd", [DFF, D])
    gm_d = dram("gm", [128, 42])
    cb_d = dram("cb", [128, 6 * 128])
    cf_d = dram("cf", [128, 66])
    mI_d = dram("mI", [128, 256])
    mT_d = dram("mT", [128, 2 * NB * N])
    outT = dram("outT", [D, T_OWN], kind="ExternalOutput")
    if dbg:
        dbg_mixed = dram("dbg_mixed", [D, T_OWN], kind="ExternalOutput")

    Sc = Sched(nc)
    uid = [0]

    def nm(p):
        uid[0] += 1
        return "%s_%d" % (p, uid[0])

    def sb(es, shape, dt, name="t"):
        return es.enter_context(nc.sbuf_tensor(nm(name), shape, dt))

    def ps(es, shape, dt, name="p"):
        return es.enter_context(nc.psum_tensor(nm(name), shape, dt))

    op = Sc.op

    from contextlib import contextmanager

    @contextmanager
    def scope():
        with ExitStack() as es_:
            yield es_
        Sc.barrier()

    def mm(out, lhsT, rhs, start, stop, reads, writes):
        return op("pe", lambda e: e.matmul(out, lhsT=lhsT, rhs=rhs, start=start, stop=stop), reads, writes)

    def act(out, in_, func, reads, writes, scale=None, bias=None):
        kw = {}
        if scale is not None:
            kw["scale"] = scale
        if bias is not None:
            kw["bias"] = bias
        return op("act", lambda e: e.activation(out=out, in_=in_, func=func, **kw), reads, writes)

    def ts(eng, out, in0, s1, op0, reads, writes, s2=None, op1=None, accum=None):
        kw = {}
        if op1 is not None:
            kw["op1"] = op1
        if accum is not None:
            kw["accum_out"] = accum
        return op(eng, lambda e: e.tensor_scalar(out=out, in0=in0, scalar1=s1, scalar2=s2, op0=op0, **kw), reads, writes)

    def tt(eng, out, in0, in1, o, reads, writes):
        return op(eng, lambda e: e.tensor_tensor(out=out, in0=in0, in1=in1, op=o), reads, writes)

    def stt(out, in0, scalar, in1, op0, op1, reads, writes):
        return op("dve", lambda e: e.scalar_tensor_tensor(out=out, in0=in0, scalar=scalar, in1=in1, op0=op0, op1=op1), reads, writes)

    def cp(eng, out, in_, reads, writes):
        if eng == "act":
            return act(out, in_, AF.Copy, reads, writes)
        return op(eng, lambda e: e.tensor_copy(out=out, in_=in_), reads, writes)

    with ExitStack() as top:
        gm = sb(top, [128, 42], F32, "gm")
        cf = sb(top, [128, 66], F32, "cf")
        cbf = sb(top, [128, 6 * 128], BF16, "cbf")
        gs = sb(top, [128, 4], F32, "gs")
        epsc = sb(top, [128, 1], F32, "epsc")
        shc = sb(top, [128, 2], F32, "shc")
        mI = sb(top, [128, 256], F32, "mI")
        mixD = sb(top, [128, 4, T_H], BF16, "mixD")
        with scope() as es:
            st = sb(es, [128, 6 * 128], F32, "cst")
            Sc.dma(st[:], cb_d[:, :], writes=["cst"])
            cp("dve", cbf[:], st[:], ["cst"], ["cbf"])
        Sc.dma(gm[:], gm_d[:, :], writes=["gm"])
        Sc.dma(cf[:], cf_d[:, :], writes=["cf"])
        Sc.dma(mI[:], mI_d[:, :], writes=["mI"])
        op("dve", lambda e: e.memset(epsc[:], EPS), (), ["epsc"])
        op("dve", lambda e: e.memset(shc[:, 0:1], 0.0), (), ["shc"])
        op("dve", lambda e: e.memset(shc[:, 1:2], math.pi / 2), (), ["shc"])
        ts("dve", gs[:, 0:1], gm[:, 35:36], 96 ** -0.5, ALU.mult, ["gm"], ["gs"])
        ts("dve", gs[:, 1:2], gm[:, 37:38], 64 ** -0.5, ALU.mult, ["gm"], ["gs"])
        ts("dve", gs[:, 2:3], gm[:, 40:41], 128 ** -0.5, ALU.mult, ["gm"], ["gs"])
        ones128 = cbf[:, 0:128]
        B64 = cbf[:, 128:256]
        O96 = cbf[:, 256:384]
        P_part = cbf[:, 384:512]
        P_mla = cbf[:, 512:640]
        ident = cbf[:, 640:768]
        sel = cf[:, 0:64]
        invf_part = cf[:, 64:65]
        invf_mla = cf[:, 65:66]
        CONST = ["gm", "cf", "cbf", "gs", "epsc"]

        PB = [ps(top, [128, 512], F32, "pb%d" % i) for i in range(7)]
        PT = ps(top, [128, 1024], BF16, "pt")
        PBN = ["pb%d" % i for i in range(7)]

        def load_w(es, dst, src, rows, cols, c0, dcol0, name):
            kc = rows // 128
            stg = sb(es, [128, kc, cols], F32, "wst")
            r = nm("wst")
            Sc.dma(stg[:], src[:, c0:c0 + cols].rearrange("(k p) c -> p k c", p=128), writes=[r])
            cp("pool", dst[:, :, dcol0:dcol0 + cols], stg[:], [r], [name])

        def rstd_from_ps(pss, psname, rows, n, dim, out, outname, tmp, tmpname):
            act(tmp[0:rows, 0:n], pss[0:rows, 0:n], AF.Ln, [psname, "epsc"], [tmpname], scale=1.0 / dim, bias=epsc[0:rows, :])
            act(out[0:rows, 0:n], tmp[0:rows, 0:n], AF.Exp, [tmpname], [outname], scale=-0.5)

        def norm_chunk(xs, xsname, n, gcol0, h, hname, sq, sqname, bank, tmp, tmpname, rs, rsname):
            act(sq[:, :, 0:n], xs[:, :, 0:n], AF.Square, [xsname], [sqname])
            for k in range(8):
                mm(PB[bank][:, 0:n], ones128, sq[:, k, 0:n], k == 0, k == 7, ["cbf", sqname], [PBN[bank]])
            rstd_from_ps(PB[bank], PBN[bank], 128, n, float(D), rs, rsname, tmp, tmpname)
            for k in range(8):
                stt(h[:, k, 0:n], xs[:, k, 0:n], gm[:, gcol0 + k:gcol0 + k + 1], rs[:, 0:n], ALU.mult, ALU.mult,
                    [xsname, "gm", rsname], [hname])

        def rope_tmp(es, n):
            return {"pi": sb(es, [128, n], I32, "rp_pi"), "pf": sb(es, [128, n], F32, "rp_pf"), "a": sb(es, [128, n], F32, "rp_a"),
                    "u": sb(es, [128, n], F32, "rp_u"), "ki": sb(es, [128, n], I32, "rp_ki"), "r": sb(es, [128, n], F32, "rp_r"),
                    "t": sb(es, [128, n], F32, "rp_t"), "k": nm("rp")}

        def rope_tables(RT, pos_d, t0, n, invf, rows, Ct, Ctn, St, Stn, cview=None, sview=None):
            pi, pf, a, u, ki, r, t, k = RT["pi"], RT["pf"], RT["a"], RT["u"], RT["ki"], RT["r"], RT["t"], RT["k"]
            R = slice(0, rows)
            Sc.dma(pi[:], pos_d[0:1, t0:t0 + n].to_broadcast([128, n]), writes=[k + "pi"])
            cp("dve", pf[R, :], pi[R, :], [k + "pi"], [k + "pf"])
            ts("dve", pf[R, :], pf[R, :], invf[R, :], ALU.mult, [k + "pf", "cf"], [k + "pf"])
            for shift, dst, dstn, view in ((0.0, St, Stn, sview), (math.pi / 2, Ct, Ctn, cview)):
                ts("dve", a[R, :], pf[R, :], shift, ALU.add, [k + "pf"], [k + "a"])
                ts("dve", u[R, :], a[R, :], 1.0 / (2 * math.pi), ALU.mult, [k + "a"], [k + "u"])
                cp("dve", ki[R, :], u[R, :], [k + "u"], [k + "ki"])
                cp("dve", u[R, :], ki[R, :], [k + "ki"], [k + "u"])
                stt(r[R, :], u[R, :], -2 * math.pi, a[R, :], ALU.mult, ALU.add, [k + "u", k + "a"], [k + "r"])
                ts("dve", t[R, :], r[R, :], math.pi, ALU.is_gt, [k + "r"], [k + "t"], s2=-2 * math.pi, op1=ALU.mult)
                tt("dve", r[R, :], r[R, :], t[R, :], ALU.add, [k + "r", k + "t"], [k + "r"])
                ts("dve", t[R, :], r[R, :], -math.pi, ALU.is_lt, [k + "r"], [k + "t"], s2=2 * math.pi, op1=ALU.mult)
                tt("dve", r[R, :], r[R, :], t[R, :], ALU.add, [k + "r", k + "t"], [k + "r"])
                ts("dve", r[R, :], r[R, :], math.pi, ALU.min, [k + "r"], [k + "r"], s2=-math.pi, op1=ALU.max)
                act(dst[R, :] if view is None else view, r[R, :], AF.Sin, [k + "r"], [dstn])

        def head_norm_rope(src, srcname, rows, n, blk, dim, gcol, Ct, Ctn, St, Stn, Pm, out, outname, bank, W, wn):
            R = slice(0, rows)
            if blk is not None:
                act(W["sq"][R, 0:n], src, AF.Square, [srcname], [wn + "sq"])
                mm(PB[bank][R, 0:n], blk[R, R], W["sq"][R, 0:n], True, True, ["cbf", wn + "sq"], [PBN[bank]])
                rstd_from_ps(PB[bank], PBN[bank], rows, n, float(dim), W["rs"], wn + "rs", W["tmp"], wn + "tmp")
                dst = W["xn"][R, 0:n] if Ct is not None else out
                dn = wn + "xn" if Ct is not None else outname
                stt(dst, src, gcol, W["rs"][R, 0:n], ALU.mult, ALU.mult, [srcname, "gm", "gs", wn + "rs"], [dn])
            else:
                cp("act", W["xn"][R, 0:n], src, [srcname], [wn + "xn"])
            if Ct is not None:
                mm(PB[bank][R, 0:n], Pm[R, R], W["xn"][R, 0:n], True, True, ["cbf", wn + "xn"], [PBN[bank]])
                tt("pool", W["t1"][R, 0:n], W["xn"][R, 0:n], Ct, ALU.mult, [wn + "xn", Ctn], [wn + "t1"])
                tt("dve", W["t2"][R, 0:n], PB[bank][R, 0:n], St, ALU.mult, [PBN[bank], Stn], [wn + "t2"])
                i0, i1 = W["t1"][R, 0:n], W["t2"][R, 0:n]
                if isinstance(out, tuple):
                    tt("dve", out[0], W["t1"][0:64, 0:n], W["t2"][0:64, 0:n], ALU.add, [wn + "t1", wn + "t2"], [outname])
                    tt("dve", out[1], W["t1"][64:128, 0:n], W["t2"][64:128, 0:n], ALU.add, [wn + "t1", wn + "t2"], [outname])
                    return
                if len(out.shape) == 3:
                    i0 = i0.rearrange("p (b q) -> p b q", q=128)
                    i1 = i1.rearrange("p (b q) -> p b q", q=128)
                tt("dve", out, i0, i1, ALU.add, [wn + "t1", wn + "t2"], [outname])

        def chunk_pipeline(es, nch, src_d, pos_d, tokoff, invf, rows, gcol0, pref, stage2, want_rope=True):
            xs = sb(es, [128, 8, N], F32, pref + "xs")
            hh_ = [sb(es, [128, 8, N], BF16, pref + "h%d" % i) for i in range(2)]
            sq = sb(es, [128, 8, N], BF16, pref + "sq")
            rs1 = sb(es, [128, N], F32, pref + "rs1")
            tmp1 = sb(es, [128, N], F32, pref + "tmp1")
            if want_rope:
                Cts = [sb(es, [128, N], F32, pref + "C%d" % i) for i in range(2)]
                Sts = [sb(es, [128, N], F32, pref + "S%d" % i) for i in range(2)]
                RT = rope_tmp(es, N)

            def s1a(c):
                i = c % 2
                t0 = tokoff + c * N
                Sc.dma(xs[:], src_d[:, t0:t0 + N].rearrange("(k p) n -> p k n", p=128), writes=[pref + "xs"])
                if want_rope:
                    rope_tables(RT, pos_d, t0, N, invf, rows, Cts[i], pref + "C%d" % i, Sts[i], pref + "S%d" % i)
                act(sq[:, :, 0:N], xs[:, :, 0:N], AF.Square, [pref + "xs"], [pref + "sq"])
                for k in range(8):
                    mm(PB[0][:, 0:N], ones128, sq[:, k, 0:N], k == 0, k == 7, ["cbf", pref + "sq"], [PBN[0]])
                rstd_from_ps(PB[0], PBN[0], 128, N, float(D), rs1, pref + "rs1", tmp1, pref + "tmp1")

            def s1b(c):
                i = c % 2
                for k in range(8):
                    stt(hh_[i][:, k, 0:N], xs[:, k, 0:N], gm[:, gcol0 + k:gcol0 + k + 1], rs1[:, 0:N], ALU.mult, ALU.mult,
                        [pref + "xs", "gm", pref + "rs1"], [pref + "h%d" % i])
            s1a(0)
            s1b(0)
            for c in range(nch):
                if c + 1 < nch:
                    s1a(c + 1)
                    s1b(c + 1)
                i = c % 2
                if want_rope:
                    stage2(c, hh_[i], pref + "h%d" % i, Cts[i], pref + "C%d" % i, Sts[i], pref + "S%d" % i)
                else:
                    stage2(c, hh_[i], pref + "h%d" % i, None, None, None, None)

        def work_tiles(es, n, pref):
            W = {"sq": sb(es, [128, n], BF16, pref + "sq"), "rs": sb(es, [128, n], F32, pref + "rs"),
                 "tmp": sb(es, [128, n], F32, pref + "tmp"), "xn": sb(es, [128, n], BF16, pref + "xn"),
                 "t1": sb(es, [128, n], F32, pref + "t1"), "t2": sb(es, [128, n], F32, pref + "t2")}
            return W

        def attn_epilogue(psO, psOn, bankB, n, W, wn, writer):
            cp("act", W["osb"][0:65, 0:n], psO[0:65, 0:n], [psOn], [wn + "osb"])
            mm(PB[bankB][0:64, 0:n], sel[0:65, 0:64], W["osb"][0:65, 0:n], True, True, ["cf", wn + "osb"], [PBN[bankB]])
            op("dve", lambda e: e.reciprocal(out=W["rinv"][0:64, 0:n], in_=PB[bankB][0:64, 0:n]), [PBN[bankB]], [wn + "rinv"])
            writer(W["osb"], W["rinv"])

        KmT = sb(top, [128, 4, NMEM], BF16, "KmT")
        Vm = sb(top, [128, 2, 512], BF16, "Vm")
        with scope() as es:
            wk = sb(es, [128, 8, 512], BF16, "wk")
            wv = sb(es, [128, 8, 512], BF16, "wv")
            with scope() as e2:
                load_w(e2, wk, m_wk, D, 512, 0, 0, "wk")
                load_w(e2, wv, m_wv, D, 512, 0, 0, "wv")
            ms = sb(es, [128, 8, NMEM], F32, "ms")
            mh = sb(es, [128, 8, NMEM], BF16, "mh")
            msq = sb(es, [128, 8, NMEM], BF16, "msq")
            W = work_tiles(es, NMEM, "mw")
            Sc.dma(ms[:], memT.rearrange("(k p) n -> p k n", p=128), writes=["ms"])
            norm_chunk(ms, "ms", NMEM, 16, mh, "mh", msq, "msq", 0, W["tmp"], "mwtmp", W["rs"], "mwrs")
            for hh in range(4):
                for k in range(8):
                    mm(PB[1][:, 0:NMEM], wk[:, k, hh * 128:(hh + 1) * 128], mh[:, k, :], k == 0, k == 7, ["wk", "mh"], ["pb1"])
                head_norm_rope(PB[1][:, 0:NMEM], "pb1", 128, NMEM, ones128, 128, gm[:, 41:42], None, None, None, None, None,
                               KmT[:, hh, :], "KmT", 2, W, "mw")
            for blk in range(2):
                for k in range(8):
                    mm(PB[3][:, 0:512], mh[:, k, blk * 128:(blk + 1) * 128], wv[:, k, :], k == 0, k == 7, ["mh", "wv"], ["pb3"])
                cp("act", Vm[:, blk, :], PB[3][:, 0:512], ["pb3"], ["Vm"])

        for hf in range(NH):
            tok0 = hf * T_H
            with scope() as pd:
                NBH = T_H // 128
                qdT = sb(pd, [128, NBH, 4, 128], BF16, "qdT")
                qiT = sb(pd, [128, NBH, 4, 128], BF16, "qiT")
                wabs = sb(pd, [128, NBH, 8], F32, "wabs")
                wsgn = sb(pd, [128, NBH, 8], F32, "wsgn")
                with scope() as es:
                    Wq = sb(es, [128, 8, 1032], BF16, "Wq")
                    for c4 in range(4):
                        with scope() as e2:
                            load_w(e2, Wq, w_in, D, 64, O_QD + c4 * 64, c4 * 128, "Wq")
                            load_w(e2, Wq, w_in, D, 64, O_QD + (4 + c4) * 64, c4 * 128 + 64, "Wq")
                    for q4 in range(2):
                        with scope() as e2:
                            load_w(e2, Wq, w_in, D, 256, O_QI + q4 * 256, 512 + q4 * 256, "Wq")
                    with scope() as e2:
                        load_w(e2, Wq, w_in, D, 8, O_WI, 1024, "Wq")
                    W = work_tiles(es, N, "bw")
                    wtok = sb(es, [128, NB, 8], F32, "wtok")

                    def q_stage2(c, h, hn, Ct, Ctn, St, Stn):
                        b0 = c * NB
                        for c4 in range(4):
                            for k in range(8):
                                mm(PB[1][:, 0:N], Wq[:, k, c4 * 128:(c4 + 1) * 128], h[:, k, :], k == 0, k == 7, ["Wq", hn], ["pb1"])
                            head_norm_rope(PB[1][:, 0:N], "pb1", 128, N, B64, 64, gs[:, 1:2], Ct[:, :], Ctn, St[:, :], Stn, P_part,
                                           qdT[:, b0:b0 + NB, c4, :], "qdT", 2, W, "bw")
                        for c4 in range(4):
                            for k in range(8):
                                mm(PB[3][:, 0:N], Wq[:, k, 512 + c4 * 128:512 + (c4 + 1) * 128], h[:, k, :], k == 0, k == 7, ["Wq", hn], ["pb3"])
                            head_norm_rope(PB[3][:, 0:N], "pb3", 128, N, None, 64, None, Ct[:, :], Ctn, St[:, :], Stn, P_part,
                                           qiT[:, b0:b0 + NB, c4, :], "qiT", 4, W, "bw")
                        for b in range(NB):
                            for k in range(8):
                                mm(PB[5][:, 0:8], h[:, k, b * 128:(b + 1) * 128], Wq[:, k, 1024:1032], k == 0, k == 7, [hn, "Wq"], ["pb5"])
                            cp("act", wtok[:, b, :], PB[5][:, 0:8], ["pb5"], ["wtok"])
                        act(wabs[:, b0:b0 + NB, :], wtok[:], AF.Abs, ["wtok"], ["wabs"], scale=(8 ** -0.5) * (64 ** -0.5))
                        ts("dve", wsgn[:, b0:b0 + NB, :], wtok[:], 0.0, ALU.is_ge, ["wtok"], ["wsgn"], s2=2.0, op1=ALU.mult)
                        ts("dve", wsgn[:, b0:b0 + NB, :], wsgn[:, b0:b0 + NB, :], -1.0, ALU.add, ["wsgn"], ["wsgn"])
                    chunk_pipeline(es, NCH_H, xT_own, pos_own, tok0, invf_part, 128, 0, "q", q_stage2)
                kdT = sb(pd, [128, S], BF16, "kdT")
                kiT_lo = sb(pd, [128, S], BF16, "kiTlo")
                kiT_hi = sb(pd, [128, S], BF16, "kiThi")
                op("pool", lambda e: e.memset(kiT_lo[64:128, :], 0.0), (), ["kiT"])
                op("pool", lambda e: e.memset(kiT_hi[0:64, :], 0.0), (), ["kiT"])
                vd = sb(pd, [128, NKB, 2, 65], BF16, "vd")
                op("pool", lambda e: e.memset(vd[:, :, :, 64:65], 1.0), (), ["vd"])
                with scope() as es:
                    Wk = sb(es, [128, 8, 384], BF16, "Wk1")
                    with scope() as e2:
                        load_w(e2, Wk, w_in, D, 128, O_KD, 0, "Wk1")
                        load_w(e2, Wk, w_in, D, 128, O_VD, 128, "Wk1")
                        load_w(e2, Wk, w_in, D, 64, O_KI, 256, "Wk1")
                        load_w(e2, Wk, w_in, D, 64, O_KI, 320, "Wk1")
                    W = work_tiles(es, N, "aw")

                    def a1_stage2(c, h, hn, Ct, Ctn, St, Stn):
                        t0 = c * N
                        for k in range(8):
                            mm(PB[1][:, 0:N], Wk[:, k, 0:128], h[:, k, :], k == 0, k == 7, ["Wk1", hn], ["pb1"])
                        head_norm_rope(PB[1][:, 0:N], "pb1", 128, N, B64, 64, gm[:, 38:39], Ct[:, :], Ctn, St[:, :], Stn, P_part,
                                       kdT[:, t0:t0 + N], "kdT", 2, W, "aw")
                        for k in range(8):
                            mm(PB[3][:, 0:N], Wk[:, k, 256:384], h[:, k, :], k == 0, k == 7, ["Wk1", hn], ["pb3"])
                        head_norm_rope(PB[3][:, 0:N], "pb3", 128, N, B64, 64, gm[:, 39:40], Ct[:, :], Ctn, St[:, :], Stn, P_part,
                                       (kiT_lo[0:64, t0:t0 + N], kiT_hi[64:128, t0:t0 + N]), "kiT", 4, W, "aw")
                        for b in range(N // 128):
                            kb = (t0 // 128) + b
                            for k in range(8):
                                mm(PB[5][:, 0:128], h[:, k, b * 128:(b + 1) * 128], Wk[:, k, 128:256], k == 0, k == 7, [hn, "Wk1"], ["pb5"])
                            cp("act", vd[:, kb, :, 0:64], PB[5][:, 0:128].rearrange("p (g d) -> p g d", g=2), ["pb5"], ["vd"])
                    chunk_pipeline(es, NCH_ALL, xT_all, pos_all, 0, invf_part, 128, 0, "a", a1_stage2)
                with scope() as es:
                    W = {}
                    W["osb"] = sb(es, [128, 512], F32, "bwosb")
                    W["rinv"] = sb(es, [128, 512], F32, "bwrinv")
                    Dg = sb(es, [128, 8, 128], BF16, "Dg")
                    Isb = sb(es, [128, S], F32, "Isb")
                    junk = sb(es, [128, S], mybir.dt.uint8, "junk")
                    Mall = sb(es, [128, S], BF16, "Mall")
                    Rh = [sb(es, [128, 512], BF16, "Rh%d" % i) for i in range(4)]
                    MT = [sb(es, [128, 4, 128], BF16, "MT%d" % i) for i in range(2)]
                    Eb = [sb(es, [128, 512], BF16, "Eb%d" % i) for i in range(3)]
                    bs = sb(es, [128, 8], F32, "bs")
                    qb0 = tok0 // 128

                    def stage_idx(b):
                        ext = 256 * (qb0 + b + 1)
                        for hh in range(8):
                            ts("dve", Dg[:, hh, :], ident, wsgn[:, b, hh:hh + 1], ALU.mult, ["cbf", "wsgn"], ["Dg"])
                        YB = [0, 1, 3, 4]
                        items = []
                        k0 = 0
                        while k0 < ext:
                            kw = min(512, ext - k0)
                            for hh in range(8):
                                items.append((k0, kw, hh))
                            k0 += kw

                        def ymm(i):
                            k0, kw, hh = items[i]
                            kt = kiT_lo if hh % 2 == 0 else kiT_hi
                            bk = YB[i % 4]
                            mm(PB[bk][:, 0:kw], qiT[:, b, hh // 2, :], kt[:, k0:k0 + kw], True, True, ["qiT", "kiT"], [PBN[bk]])
                        LA = 3
                        for i in range(min(LA, len(items))):
                            ymm(i)
                        for i in range(len(items)):
                            if i + LA < len(items):
                                ymm(i + LA)
                            k0, kw, hh = items[i]
                            bk = YB[i % 4]
                            r = Rh[i % 4]
                            rn = "Rh%d" % (i % 4)
                            if hh % 2 == 0:
                                act(r[:, 0:kw], PB[bk][:, 0:kw], AF.Relu, [PBN[bk], "wabs"], [rn], scale=wabs[:, b, hh:hh + 1])
                            else:
                                ts("dve", r[:, 0:kw], PB[bk][:, 0:kw], 0.0, ALU.max, [PBN[bk], "wabs"], [rn],
                                   s2=wabs[:, b, hh:hh + 1], op1=ALU.mult)
                            mm(PB[2][:, 0:kw], Dg[:, hh, :], r[:, 0:kw], hh == 0, hh == 7, ["Dg", rn], ["pb2"])
                            if hh == 7:
                                cp("act", Isb[:, k0:k0 + kw], PB[2][:, 0:kw], ["pb2"], ["Isb"])

                    def stage_thr(b):
                        ext = 256 * (qb0 + b + 1)
                        op("dve", lambda e: e.tensor_reduce(out=bs[:, 5:6], in_=Isb[:, 0:ext], axis=AX.X, op=ALU.max, apply_absolute_value=True),
                           ["Isb"], ["bs"])
                        tt("dve", Isb[:, ext - 256:ext], Isb[:, ext - 256:ext], mI[:, :], ALU.add, ["Isb", "mI"], ["Isb"])
                        ts("dve", bs[:, 0:1], bs[:, 5:6], -1.0, ALU.mult, ["bs"], ["bs"], s2=-1.0, op1=ALU.add)
                        ts("dve", bs[:, 1:2], bs[:, 5:6], 2.0, ALU.mult, ["bs"], ["bs"], s2=2.0, op1=ALU.add)
                        for it in range(BISECT_ITERS):
                            cst = 2.0 ** -(it + 1)
                            stt(bs[:, 2:3], bs[:, 1:2], cst, bs[:, 0:1], ALU.mult, ALU.add, ["bs"], ["bs"])
                            ts("dve", junk[:, 0:ext], Isb[:, 0:ext], bs[:, 2:3], ALU.is_ge, ["Isb", "bs"], ["junk", "bs"],
                               op1=ALU.add, accum=bs[:, 3:4])
                            ts("dve", bs[:, 4:5], bs[:, 3:4], TOPK - 0.5, ALU.is_ge, ["bs"], ["bs"], s2=cst, op1=ALU.mult)
                            stt(bs[:, 0:1], bs[:, 4:5], bs[:, 1:2], bs[:, 0:1], ALU.mult, ALU.add, ["bs"], ["bs"])

                    def stage_mall(b):
                        ext = 256 * (qb0 + b + 1)
                        ts("dve", Mall[:, 0:ext], Isb[:, 0:ext], bs[:, 0:1], ALU.is_lt, ["Isb", "bs"], ["Mall"], s2=-30000.0, op1=ALU.mult)

                    def stage_attn(b):
                        ext = 256 * (qb0 + b + 1)
                        nkb = ext // 128
                        ngrp = (nkb + 3) // 4

                        def prep(gi):
                            kc = gi * 4
                            nb4 = min(4, nkb - kc)
                            mt = MT[gi % 2]
                            mtn = "MT%d" % (gi % 2)
                            for j in range(nb4):
                                op("pe", lambda e: e.transpose(out=PT[:, j * 128:(j + 1) * 128], in_=Mall[:, (kc + j) * 128:(kc + j + 1) * 128], identity=ident),
                                   ["Mall", "cbf"], ["pt"])
                            cp("act", mt[:, 0:nb4, :], PT[:, 0:nb4 * 128].rearrange("p (a q) -> p a q", a=nb4), ["pt"], [mtn])

                        units = [(kb, g) for kb in range(nkb) for g in range(2)]

                        def emit_S(u):
                            kb, g = units[u]
                            G = slice(g * 64, (g + 1) * 64)
                            bk = 3 + (u % 2)
                            mt = MT[(kb // 4) % 2]
                            mtn = "MT%d" % ((kb // 4) % 2)
                            mm(PB[bk][:, 0:512], kdT[G, kb * 128:(kb + 1) * 128], qdT[G, b, :, :].rearrange("p a q -> p (a q)"),
                               True, False, ["kdT", "qdT"], [PBN[bk]])
                            mm(PB[bk][:, 0:512].rearrange("p (a q) -> p a q", a=4), ident, mt[:, (kb % 4):(kb % 4) + 1, :].to_broadcast([128, 4, 128]),
                               False, True, ["cbf", mtn], [PBN[bk]])

                        def emit_rest(u):
                            kb, g = units[u]
                            bk = 3 + (u % 2)
                            u2 = u % 3
                            act(Eb[u2][:, :], PB[bk][:, 0:512], AF.Exp, [PBN[bk]], ["Eb%d" % u2])
                            mm(PB[5 + g][0:65, 0:512], vd[:, kb, g, :], Eb[u2][:, :], kb == 0, kb == nkb - 1, ["vd", "Eb%d" % u2], [PBN[5 + g]])

                        prep(0)
                        if ngrp > 1:
                            prep(1)
                        emit_S(0)
                        for u in range(len(units)):
                            if u + 1 < len(units):
                                emit_S(u + 1)
                            emit_rest(u)
                            kb, g = units[u]
                            if g == 1 and kb % 4 == 3 and (kb // 4) + 2 < ngrp:
                                prep(kb // 4 + 2)
                        for g in range(2):
                            def writer(osb, rinv, g=g, b=b):
                                for a in range(4):
                                    hd = g * 4 + a
                                    col = b * 128
                                    tt("dve", mixD[(hd % 2) * 64:(hd % 2) * 64 + 64, hd // 2, col:col + 128],
                                       osb[0:64, a * 128:(a + 1) * 128], rinv[0:64, a * 128:(a + 1) * 128], ALU.mult,
                                       ["bwosb", "bwrinv"], ["mixD"])
                            attn_epilogue(PB[5 + g], PBN[5 + g], 3, 512, W, "bw", writer)

                    stage_idx(0)
                    stage_thr(0)
                    stage_mall(0)
                    for b in range(NBH):
                        if b + 1 < NBH:
                            stage_idx(b + 1)
                            stage_thr(b + 1)
                        stage_attn(b)
                        if b + 1 < NBH:
                            stage_mall(b + 1)

            with scope() as pmd:
                mixM = sb(pmd, [128, 4, T_H], BF16, "mixM")
                mT = sb(pmd, [128, 2 * NB, N], BF16, "mT")
                with scope() as es:
                    st2 = sb(es, [128, 2 * NB * N], F32, "cst2")
                    Sc.dma(st2[:], mT_d[:, :], writes=["cst2"])
                    cp("dve", mT[:].rearrange("p a n -> p (a n)"), st2[:], ["cst2"], ["mT"])
                with scope() as pm:
                    ckvnT = sb(pm, [128, S], BF16, "ckvnT")
                    kpeX = sb(pm, [128, S], BF16, "kpeX")
                    cqnT = sb(pm, [128, 2, T_H], BF16, "cqnT")
                    tabC = sb(pm, [128, T_H], BF16, "tabC")
                    tabS = sb(pm, [128, T_H], BF16, "tabS")
                    with scope() as es:
                        Wk = sb(es, [128, 8, 256], BF16, "Wk2")
                        with scope() as e2:
                            load_w(e2, Wk, w_in, D, 128, O_CKV, 0, "Wk2")
                            load_w(e2, Wk, w_in, D, 32, O_KPE, 128, "Wk2")
                            load_w(e2, Wk, w_in, D, 32, O_KPE, 192, "Wk2")
                        W = work_tiles(es, N, "cw")
                        op("pool", lambda e: e.memset(Wk[:, :, 160:192], 0.0), (), ["Wk2"])
                        op("pool", lambda e: e.memset(Wk[:, :, 224:256], 0.0), (), ["Wk2"])

                        def a2_stage2(c, h, hn, Ct, Ctn, St, Stn):
                            t0 = c * N
                            for k in range(8):
                                mm(PB[1][:, 0:N], Wk[:, k, 0:128], h[:, k, :], k == 0, k == 7, ["Wk2", hn], ["pb1"])
                            head_norm_rope(PB[1][:, 0:N], "pb1", 128, N, ones128, 128, gm[:, 34:35], None, None, None, None, None,
                                           ckvnT[:, t0:t0 + N], "ckvnT", 2, W, "cw")
                            for k in range(8):
                                mm(PB[3][:, 0:N], Wk[:, k, 128:256], h[:, k, :], k == 0, k == 7, ["Wk2", hn], ["pb3"])
                            act(kpeX[0:32, t0:t0 + N], PB[3][0:32, 0:N], AF.Square, ["pb3"], ["kpeX"])
                            R = slice(64, 96)
                            ts("dve", W["xn"][R, 0:N], PB[3][R, 0:N], gm[R, 36:37], ALU.mult, ["pb3", "gm"], ["cwxn"])
                            op("pool", lambda e: e.memset(W["xn"][0:64, 0:N], 0.0), (), ["cwxn"])
                            mm(PB[4][0:96, 0:N], P_mla[0:96, 0:96], W["xn"][0:96, 0:N], True, True, ["cbf", "cwxn"], ["pb4"])
                            tt("pool", W["t1"][R, 0:N], W["xn"][R, 0:N], Ct[R, :], ALU.mult, ["cwxn", Ctn], ["cwt1"])
                            tt("dve", W["t2"][R, 0:N], PB[4][R, 0:N], St[R, :], ALU.mult, ["pb4", Stn], ["cwt2"])
                            tt("dve", kpeX[R, t0:t0 + N], W["t1"][R, 0:N], W["t2"][R, 0:N], ALU.add, ["cwt1", "cwt2"], ["kpeX"])
                        chunk_pipeline(es, NCH_ALL, xT_all, pos_all, 0, invf_mla, 96, 0, "c", a2_stage2)
                    with scope() as es:
                        Wc = sb(es, [128, 8, 256], BF16, "Wc")
                        with scope() as e2:
                            load_w(e2, Wc, w_in, D, 256, O_CQ, 0, "Wc")
                        W = work_tiles(es, N, "dw")
                        cqr = sb(es, [128, 2, N], F32, "cqr")
                        cqs = sb(es, [128, 2, N], BF16, "cqs")

                        def l_stage2(c, h, hn, Cf, Cfn, Sf, Sfn):
                            cp("pool", tabC[0:96, c * N:(c + 1) * N], Cf[0:96, :], [Cfn], ["tabC"])
                            cp("pool", tabS[0:96, c * N:(c + 1) * N], Sf[0:96, :], [Sfn], ["tabS"])
                            for r2 in range(2):
                                for k in range(8):
                                    mm(PB[1][:, 0:N], Wc[:, k, r2 * 128:(r2 + 1) * 128], h[:, k, :], k == 0, k == 7, ["Wc", hn], ["pb1"])
                                cp("act", cqr[:, r2, :], PB[1][:, 0:N], ["pb1"], ["cqr"])
                            act(cqs[:, :, :], cqr[:, :, :], AF.Square, ["cqr"], ["cqs"])
                            for r2 in range(2):
                                mm(PB[2][:, 0:N], ones128, cqs[:, r2, :], r2 == 0, r2 == 1, ["cbf", "cqs"], ["pb2"])
                            rstd_from_ps(PB[2], "pb2", 128, N, 256.0, W["rs"], "dwrs", W["tmp"], "dwtmp")
                            for r2 in range(2):
                                stt(cqnT[:, r2, c * N:(c + 1) * N], cqr[:, r2, :], gm[:, 32 + r2:33 + r2], W["rs"][:, 0:N], ALU.mult, ALU.mult,
                                    ["cqr", "gm", "dwrs"], ["cqnT"])
                        chunk_pipeline(es, NCH_H, xT_own, pos_own, tok0, invf_mla, 96, 0, "l", l_stage2)
                    with scope() as es:
                        wuq = sb(es, [128, 2, 768], BF16, "wuq")
                        wukv = sb(es, [128, 1, 1024], BF16, "wukv")
                        with scope() as e2:
                            load_w(e2, wuq, w_uq, 256, 768, 0, 0, "wuq")
                            load_w(e2, wukv, w_ukv, 128, 1024, 0, 0, "wukv")
                        KhT = sb(es, [128, S], BF16, "KhT")
                        Vh = sb(es, [128, NKB, 65], BF16, "Vh")
                        QhT = sb(es, [128, T_H], BF16, "QhT")
                        W = work_tiles(es, N, "ew")
                        W["osb"] = sb(es, [128, 512], F32, "ewosb")
                        W["rinv"] = sb(es, [128, 512], F32, "ewrinv")
                        Eb = [sb(es, [128, N], BF16, "mEb%d" % i) for i in range(3)]
                        op("pool", lambda e: e.memset(Vh[:, :, 64:65], 1.0), (), ["Vh"])
                        for hd in range(8):
                            for c in range(NCH_ALL):
                                t0 = c * N
                                mm(PB[0][0:64, 0:N], wukv[:, 0, hd * 128:hd * 128 + 64], ckvnT[:, t0:t0 + N], True, True, ["wukv", "ckvnT"], ["pb0"])
                                act(W["sq"][0:64, 0:N], PB[0][0:64, 0:N], AF.Square, ["pb0"], ["ewsq"])
                                mm(PB[1][0:96, 0:N], O96[0:64, 0:96], W["sq"][0:64, 0:N], True, False, ["cbf", "ewsq"], ["pb1"])
                                mm(PB[1][0:96, 0:N], O96[0:32, 0:96], kpeX[0:32, t0:t0 + N], False, True, ["cbf", "kpeX"], ["pb1"])
                                rstd_from_ps(PB[1], "pb1", 96, N, 96.0, W["rs"], "ewrs", W["tmp"], "ewtmp")
                                stt(KhT[0:64, t0:t0 + N], PB[0][0:64, 0:N], gm[0:64, 36:37], W["rs"][0:64, 0:N], ALU.mult, ALU.mult,
                                    ["pb0", "gm", "ewrs"], ["KhT"])
                                tt("pool", KhT[64:96, t0:t0 + N], kpeX[64:96, t0:t0 + N], W["rs"][64:96, 0:N], ALU.mult, ["kpeX", "ewrs"], ["KhT"])
                            for kb0 in range(0, NKB, 8):
                                for j in range(8):
                                    kb = kb0 + j
                                    mm(PB[2][:, j * 64:(j + 1) * 64], ckvnT[:, kb * 128:(kb + 1) * 128], wukv[:, 0, hd * 128 + 64:hd * 128 + 128],
                                       True, True, ["ckvnT", "wukv"], ["pb2"])
                                cp("act", Vh[:, kb0:kb0 + 8, 0:64], PB[2][:, 0:512].rearrange("p (a d) -> p a d", a=8), ["pb2"], ["Vh"])
                            for c in range(NCH_H):
                                for r2 in range(2):
                                    mm(PB[0][0:96, 0:N], wuq[:, r2, hd * 96:(hd + 1) * 96], cqnT[:, r2, c * N:(c + 1) * N], r2 == 0, r2 == 1,
                                       ["wuq", "cqnT"], ["pb0"])
                                head_norm_rope(PB[0][0:96, 0:N], "pb0", 96, N, O96, 96, gs[0:96, 0:1], tabC[0:96, c * N:(c + 1) * N], "tabC",
                                               tabS[0:96, c * N:(c + 1) * N], "tabS", P_mla, QhT[0:96, c * N:(c + 1) * N], "QhT", 1, W, "ew")
                            for c in range(NCH_H):
                                j = (tok0 // N) + c
                                ext = 2 * NB * (j + 1)
                                bo = 3 + (c % 2)
                                def emit_S(kb, c=c, j=j):
                                    bs_ = 5 + (kb % 2)
                                    dg = kb >= 2 * NB * j
                                    mm(PB[bs_][:, 0:N], KhT[0:96, kb * 128:(kb + 1) * 128], QhT[0:96, c * N:(c + 1) * N], True, not dg, ["KhT", "QhT"], [PBN[bs_]])
                                    if dg:
                                        mm(PB[bs_][:, 0:N], ident, mT[:, kb - 2 * NB * j, :], False, True, ["cbf", "mT"], [PBN[bs_]])

                                def emit_rest(kb, ext=ext, bo=bo):
                                    bs_ = 5 + (kb % 2)
                                    e_ = Eb[kb % 3]
                                    en = "mEb%d" % (kb % 3)
                                    act(e_[:, :], PB[bs_][:, 0:N], AF.Exp, [PBN[bs_]], [en])
                                    mm(PB[bo][0:65, 0:N], Vh[:, kb, :], e_[:, :], kb == 0, kb == ext - 1, ["Vh", en], [PBN[bo]])
                                emit_S(0)
                                for kb in range(ext):
                                    if kb + 1 < ext:
                                        emit_S(kb + 1)
                                    emit_rest(kb)

                                def writer(osb, rinv, hd=hd, c=c):
                                    tt("dve", mixM[(hd % 2) * 64:(hd % 2) * 64 + 64, hd // 2, c * N:(c + 1) * N], osb[0:64, 0:N], rinv[0:64, 0:N],
                                       ALU.mult, ["ewosb", "ewrinv"], ["mixM"])
                                attn_epilogue(PB[bo], PBN[bo], 2, N, W, "ew", writer)

                if dbg:
                    with scope() as es:
                        mf = sb(es, [128, 8, T_H], F32, "mf")
                        cp("dve", mf[:, 0:4, :], mixM[:], ["mixM"], ["mf"])
                        cp("dve", mf[:, 4:8, :], mixD[:], ["mixD"], ["mf"])
                        Sc.dma(dbg_mixed[:, tok0:tok0 + T_H].rearrange("(k p) n -> p k n", p=128), mf[:], reads=["mf"], is_out=True)

                with scope() as es:
                    wo_ = sb(es, [128, 8, D], BF16, "wo_")
                    wq_ = sb(es, [128, 8, 512], BF16, "wq_")
                    wmo = sb(es, [128, 4, D], BF16, "wmo")
                    with scope() as e2:
                        for q4 in range(4):
                            with scope() as e3:
                                load_w(e3, wo_, w_out, D, 256, q4 * 256, q4 * 256, "wo_")
                        load_w(e2, wq_, m_wq, D, 512, 0, 0, "wq_")
                    with scope() as e2:
                        load_w(e2, wmo, m_wo, 512, D, 0, 0, "wmo")
                    x1 = sb(es, [128, 8, N], F32, "x1")
                    h = sb(es, [128, 8, N], BF16, "h4")
                    W = work_tiles(es, N, "fw")
                    om = sb(es, [128, 4, N], BF16, "om")
                    Eb = [sb(es, [128, N], BF16, "cEb%d" % i) for i in range(2)]
                    actT = sb(es, [128, DFF // 128, N], BF16, "actT")
                    stg = [sb(es, [128, 2048], F32, "fstg%d" % i) for i in range(4)]
                    wsl = [sb(es, [128, 2048], BF16, "fws%d" % i) for i in range(4)]
                    sg = sb(es, [128, N], F32, "sg")
                    nld = [0]

                    def load_slab(src_ap, a_, b_):
                        i = nld[0]
                        nld[0] += 1
                        sn = "fstg%d" % (i % 4)
                        wn_ = "fws%d" % (i % 4)
                        s_ = stg[i % 4][:, 0:a_ * b_].rearrange("p (a b) -> p a b", a=a_)
                        w_ = wsl[i % 4][:, 0:a_ * b_].rearrange("p (a b) -> p a b", a=a_)
                        Sc.dma(s_, src_ap, writes=[sn])
                        cp(("pool", "dve", "act")[i % 3], w_, s_, [sn], [wn_])
                        return w_, wn_

                    sq = actT[:, 0:8, :]
                    for c in range(NCH_H):
                        lt0 = tok0 + c * N
                        Sc.dma(x1[:], xT_own[:, lt0:lt0 + N].rearrange("(k p) n -> p k n", p=128), writes=["x1"])
                        for m in range(8):
                            bk = m % 2
                            for k in range(8):
                                mm(PB[bk][:, 0:N], wo_[:, k, m * 128:(m + 1) * 128], (mixM if k < 4 else mixD)[:, k % 4, c * N:(c + 1) * N], k == 0, k == 7, ["wo_", "mixM", "mixD"], [PBN[bk]])
                            tt("dve", x1[:, m, :], x1[:, m, :], PB[bk][:, 0:N], ALU.add, ["x1", PBN[bk]], ["x1"])
                        norm_chunk(x1, "x1", N, 8, h, "h4", sq, "actT", 2, W["tmp"], "fwtmp", W["rs"], "fwrs")
                        for hh in range(4):
                            for k in range(8):
                                mm(PB[3][:, 0:N], wq_[:, k, hh * 128:(hh + 1) * 128], h[:, k, :], k == 0, k == 7, ["wq_", "h4"], ["pb3"])
                            head_norm_rope(PB[3][:, 0:N], "pb3", 128, N, ones128, 128, gs[:, 2:3], None, None, None, None, None,
                                           W["xn"][:, 0:N], "fwxn2", 4, W, "fw")
                            for blk in range(2):
                                mm(PB[5][:, 0:N], KmT[:, hh, blk * 128:(blk + 1) * 128], W["xn"][:, 0:N], True, True, ["KmT", "fwxn2"], ["pb5"])
                                act(Eb[blk][:, :], PB[5][:, 0:N], AF.Exp, ["pb5"], ["cEb%d" % blk])
                            for blk in range(2):
                                mm(PB[6][:, 0:N], Vm[:, blk, hh * 128:(hh + 1) * 128], Eb[blk][:, :], blk == 0, blk == 1, ["Vm", "cEb%d" % blk], ["pb6"])
                            for blk in range(2):
                                mm(PB[4][:, 0:N], ones128, Eb[blk][:, :], blk == 0, blk == 1, ["cbf", "cEb%d" % blk], ["pb4"])
                            op("dve", lambda e: e.reciprocal(out=W["t1"][:, 0:N], in_=PB[4][:, 0:N]), ["pb4"], ["fwt1"])
                            tt("dve", om[:, hh, :], PB[6][:, 0:N], W["t1"][:, 0:N], ALU.mult, ["pb6", "fwt1"], ["om"])
                        for m in range(8):
                            bk = m % 2
                            for hh in range(4):
                                mm(PB[bk][:, 0:N], wmo[:, hh, m * 128:(m + 1) * 128], om[:, hh, :], hh == 0, hh == 3, ["wmo", "om"], [PBN[bk]])
                            tt("dve", x1[:, m, :], x1[:, m, :], PB[bk][:, 0:N], ALU.add, ["x1", PBN[bk]], ["x1"])
                        norm_chunk(x1, "x1", N, 24, h, "h4", sq, "actT", 2, W["tmp"], "fwtmp", W["rs"], "fwrs")
                        slabs = [(c0, 256) for c0 in range(0, DFF, 256)]
                        for (c0, cw) in slabs:
                            wg, wgn = load_slab(f_wg[:, c0:c0 + cw].rearrange("(k p) c -> p k c", p=128), 8, cw)
                            wu, wun = load_slab(f_wu[:, c0:c0 + cw].rearrange("(k p) c -> p k c", p=128), 8, cw)
                            for f in range(cw // 128):
                                ff = c0 // 128 + f
                                bg = 3 + (ff % 2)
                                bu = 5 + (ff % 2)
                                for k in range(8):
                                    mm(PB[bg][:, 0:N], wg[:, k, f * 128:(f + 1) * 128], h[:, k, :], k == 0, k == 7, [wgn, "h4"], [PBN[bg]])
                                for k in range(8):
                                    mm(PB[bu][:, 0:N], wu[:, k, f * 128:(f + 1) * 128], h[:, k, :], k == 0, k == 7, [wun, "h4"], [PBN[bu]])
                                act(sg[:, :], PB[bg][:, 0:N], AF.Silu, [PBN[bg]], ["sg"])
                                tt("dve", actT[:, ff, :], sg[:, :], PB[bu][:, 0:N], ALU.mult, ["sg", PBN[bu]], ["actT"])
                        for grp in ((0, 1, 2, 3), (4, 5, 6, 7)):
                            rslabs = [(r0, 256) for r0 in range(0, DFF, 256)]
                            for (r0, rw) in rslabs:
                                nf = rw // 128
                                wd, wdn = load_slab(f_wd[r0:r0 + rw, grp[0] * 128:grp[0] * 128 + 512].rearrange("(f p) c -> p f c", p=128), nf, 512)
                                for f in range(nf):
                                    ff = r0 // 128 + f
                                    for mi, m in enumerate(grp):
                                        mm(PB[mi][:, 0:N], wd[:, f, mi * 128:(mi + 1) * 128], actT[:, ff, :], ff == 0, ff == DFF // 128 - 1, [wdn, "actT"], [PBN[mi]])
                            for mi, m in enumerate(grp):
                                tt("dve", x1[:, m, :], x1[:, m, :], PB[mi][:, 0:N], ALU.add, ["x1", PBN[mi]], ["x1"])
                        Sc.dma(outT[:, lt0:lt0 + N].rearrange("(k p) n -> p k n", p=128), x1[:], reads=["x1"], is_out=True)
        Sc.finish()
    return nc, Sc


def _consts(S, NH, half):
    T_OWN = S // 2
    T_H = T_OWN // NH
    N = min(512, T_H)
    NB = N // 128
    cb = np.zeros((128, 6 * 128), np.float32)
    cb[:, 0:128] = 1.0
    cb[0:64, 128:192] = 1.0
    cb[64:128, 192:256] = 1.0
    cb[0:96, 256:352] = 1.0
    Pp = np.zeros((128, 128), np.float32)
    for base in (0, 64):
        for i in range(8):
            Pp[base + 8 + i, base + i] = -1.0
            Pp[base + i, base + 8 + i] = 1.0
    cb[:, 384:512] = Pp
    Pm = np.zeros((128, 128), np.float32)
    for i in range(16):
        Pm[80 + i, 64 + i] = -1.0
        Pm[64 + i, 80 + i] = 1.0
    cb[:, 512:640] = Pm
    cb[:, 640:768] = np.eye(128, dtype=np.float32)
    cf = np.zeros((128, 66), np.float32)
    cf[64, 0:64] = 1.0
    f8 = (THETA ** (-np.arange(0, 16, 2, dtype=np.float32) / np.float32(16))).astype(np.float32)
    f16 = (THETA ** (-np.arange(0, 32, 2, dtype=np.float32) / np.float32(32))).astype(np.float32)
    for base in (0, 64):
        cf[base:base + 8, 64] = f8
        cf[base + 8:base + 16, 64] = f8
    cf[64:80, 65] = f16
    cf[80:96, 65] = f16
    mI = np.zeros((128, 256), np.float32)
    q = np.arange(128)[:, None]
    k = np.arange(256)[None, :]
    qpos = half * 128 + q
    mI[:] = np.where(k <= qpos, 0.0, NEG)
    mT = np.zeros((128, 2 * NB, N), np.float32)
    p = np.arange(128)[:, None]
    for kbp in range(2 * NB):
        for m in range(NB):
            r = np.arange(128)[None, :]
            gb = 2 * m + half
            if kbp < gb:
                v = np.zeros((128, 128), np.float32)
            elif kbp == gb:
                v = np.where(p <= r, 0.0, -30000.0).astype(np.float32)
            else:
                v = np.full((128, 128), -30000.0, np.float32)
            mT[:, kbp, m * 128:(m + 1) * 128] = v
    return cb, cf, mI, mT.reshape(128, 2 * NB * N)


def _gains(inp):
    gm = np.zeros((128, 42), np.float32)

    def col8(v, c0):
        gm[:, c0:c0 + 8] = np.asarray(v, np.float32).reshape(8, 128).T
    col8(inp["norm_mix"][0], 0)
    col8(inp["norm_mem_x"][0], 8)
    col8(inp["norm_mem_kv"][0], 16)
    col8(inp["norm_ffn"][0], 24)
    gm[:, 32:34] = np.asarray(inp["mla_q_a_norm"][0], np.float32).reshape(2, 128).T
    gm[:, 34] = inp["mla_kv_a_norm"][0]
    gm[0:96, 35] = inp["mla_q_norm"][0]
    gm[0:96, 36] = inp["mla_k_norm"][0]
    gm[:, 37] = np.tile(np.asarray(inp["dsa_q_norm"][0], np.float32), 2)
    gm[:, 38] = np.tile(np.asarray(inp["dsa_k_norm"][0], np.float32), 2)
    gm[:, 39] = np.tile(np.asarray(inp["idx_k_norm"][0], np.float32), 2)
    gm[:, 40] = inp["mem_q_norm"][0]
    gm[:, 41] = inp["mem_k_norm"][0]
    return gm


_CACHE = {}


def run(inputs, S, NH, dbg=False):
    inp = {k: np.asarray(v) for k, v in inputs.items()}
    B = inp["x"].shape[0]
    ncores = 2 * B
    key = (S, NH, dbg)
    if key not in _CACHE:
        _CACHE[key] = build(S, NH, dbg)
    nc, Sc = _CACHE[key]
    gm = _gains(inp)
    nblk = S // 128
    in_maps = []
    for c in range(ncores):
        b, half = c // 2, c % 2
        own_blocks = np.arange(half, nblk, 2)
        own_idx = (own_blocks[:, None] * 128 + np.arange(128)[None, :]).reshape(-1)
        xb = np.asarray(inp["x"][b], np.float32)
        cb, cf, mI, mT = _consts(S, NH, half)
        pos = np.asarray(inp["positions"][b], np.int32)
        in_maps.append({
            "xT_all": np.ascontiguousarray(xb.T),
            "xT_own": np.ascontiguousarray(xb[own_idx].T),
            "pos_all": np.ascontiguousarray(pos.reshape(1, S)),
            "pos_own": np.ascontiguousarray(pos[own_idx].reshape(1, S // 2)),
            "memT": np.ascontiguousarray(np.asarray(inp["mem"][b], np.float32).T),
            "w_in": np.ascontiguousarray(inp["w_in"][0], dtype=np.float32),
            "w_uq": np.ascontiguousarray(inp["mla_w_uq"][0], dtype=np.float32),
            "w_ukv": np.ascontiguousarray(inp["mla_w_ukv"][0], dtype=np.float32),
            "w_out": np.ascontiguousarray(inp["w_out"][0], dtype=np.float32),
            "m_wq": np.ascontiguousarray(inp["mem_w_q"][0], dtype=np.float32),
            "m_wk": np.ascontiguousarray(inp["mem_w_k"][0], dtype=np.float32),
            "m_wv": np.ascontiguousarray(inp["mem_w_v"][0], dtype=np.float32),
            "m_wo": np.ascontiguousarray(inp["mem_w_o"][0], dtype=np.float32),
            "f_wg": np.ascontiguousarray(inp["ffn_w_gate"][0], dtype=np.float32),
            "f_wu": np.ascontiguousarray(inp["ffn_w_up"][0], dtype=np.float32),
            "f_wd": np.ascontiguousarray(inp["ffn_w_down"][0], dtype=np.float32),
            "gm": gm, "cb": cb, "cf": cf, "mI": mI, "mT": mT,
        })
    res = run_bass_kernel_spmd(nc, in_maps, core_ids=list(range(ncores)))
    out = np.zeros((B, S, D), np.float32)
    extra = {}
    for c in range(ncores):
        b, half = c // 2, c % 2
        own_blocks = np.arange(half, nblk, 2)
        own_idx = (own_blocks[:, None] * 128 + np.arange(128)[None, :]).reshape(-1)
        out[b, own_idx, :] = np.asarray(res.results[c]["outT"]).T
        if dbg:
            extra[c] = (own_idx, np.asarray(res.results[c]["dbg_mixed"]).T)
    return out, extra


def kernel(**inputs):
    out, _ = run(inputs, 8192, 2)
    return out
```
